# Optimizing a Trainium2 kernel written in Bass

```python
import math
import jax, jax.numpy as jnp
from jax import lax
import numpy as np

D_MODEL = 1024
BATCH = 8
SEQ = 2048
DEPTH = 4

HEAD_DIM = 64
N_HEADS = D_MODEL // HEAD_DIM
MIX_WIDTH = N_HEADS * HEAD_DIM
NSA_HEADS = N_HEADS // 2
NSA_KV = 2
NSA_GROUP = NSA_HEADS // NSA_KV
FOX_HEADS = N_HEADS // 4
SB_HEADS = N_HEADS - NSA_HEADS - FOX_HEADS
ROPE_DIM = HEAD_DIM // 4
ROPE_THETA = 500000.0
CMP_LEN = 32
CMP_STRIDE = 16
CMP_HIDDEN = 2 * HEAD_DIM
SEL_LEN = 64
SEL_TOPN = 16
WINDOW = 512
FORCE_SCORE = 1.0e4
Q_BLOCK = 128
SEL_Q_BLOCK = 64
N_GROUPS = 4
EXPERTS_PER_GROUP = 8
N_EXPERTS = N_GROUPS * EXPERTS_PER_GROUP
EXPERT_TOPK = 2
D_EXPERT = D_MODEL // 2
MOE_CHUNK = 256
EPS = 1e-6
NEG = -1e30

KV_W = NSA_KV * HEAD_DIM
IN_SIZES = (NSA_HEADS * HEAD_DIM, KV_W, KV_W, KV_W, KV_W, KV_W, KV_W, NSA_HEADS * 3,
            FOX_HEADS * HEAD_DIM, FOX_HEADS * HEAD_DIM, FOX_HEADS * HEAD_DIM, FOX_HEADS,
            SB_HEADS * HEAD_DIM, SB_HEADS * HEAD_DIM, SB_HEADS * HEAD_DIM)
D_IN = sum(IN_SIZES)

kernel_name = 'hybrid_nsa_fox_stickbreak_hmoe_adaln'


def rmsnorm(x, g):
    xf = x.astype(jnp.float32)
    y = xf * lax.rsqrt(jnp.mean(xf * xf, axis=-1, keepdims=True) + EPS)
    return (y * g.astype(jnp.float32)).astype(x.dtype)


def rope_partial(x, pos):
    half = ROPE_DIM // 2
    inv = jnp.exp(jnp.arange(half, dtype=jnp.float32) * (-2.0 * math.log(ROPE_THETA) / ROPE_DIM))
    ang = pos.astype(jnp.float32)[:, None] * inv[None, :]
    shp = (pos.shape[0],) + (1,) * (x.ndim - 3) + (half,)
    cos = jnp.cos(ang).reshape(shp).astype(x.dtype)
    sin = jnp.sin(ang).reshape(shp).astype(x.dtype)
    x1 = x[..., :half]
    x2 = x[..., half:ROPE_DIM]
    return jnp.concatenate([x1 * cos - x2 * sin, x2 * cos + x1 * sin, x[..., ROPE_DIM:]], axis=-1)


def masked_softmax(s, m):
    return jax.nn.softmax(jnp.where(m, s, NEG), axis=-1)


def sweep_blocks(fn, n_blocks):
    out = lax.map(fn, jnp.arange(n_blocks))
    out = jnp.moveaxis(out, 0, 1)
    return out.reshape(out.shape[:1] + (-1,) + out.shape[3:])


def compress_blocks(k, blk_idx, pe, w1, w2):
    b, _, g, dh = k.shape
    n = blk_idx.shape[0]
    blk = k[:, blk_idx] + pe[:, None, :]
    flat = jnp.moveaxis(blk, 3, 2).reshape(b, n, g, CMP_LEN * dh)
    return jax.nn.silu(flat @ w1) @ w2


def nsa_mixer(q, kc, vc, ks, vs, kw, vw, gates, pos_k, w1_k, w2_k, pos_v, w1_v, w2_v):
    b, s_len, _ = q.shape
    scale = HEAD_DIM ** -0.5
    pos = jnp.arange(s_len)
    kv_shape = (b, s_len, NSA_KV, HEAD_DIM)
    q = rope_partial(q.reshape(b, s_len, NSA_KV, NSA_GROUP, HEAD_DIM), pos)
    kc, vc, vs, vw = [t.reshape(kv_shape) for t in (kc, vc, vs, vw)]
    ks = rope_partial(ks.reshape(kv_shape), pos)
    kw = rope_partial(kw.reshape(kv_shape), pos)

    n_cmp = (s_len - CMP_LEN) // CMP_STRIDE + 1
    cmp_start = jnp.arange(n_cmp) * CMP_STRIDE
    cmp_end = cmp_start + CMP_LEN - 1
    blk_idx = cmp_start[:, None] + jnp.arange(CMP_LEN)[None, :]
    ck = rope_partial(compress_blocks(kc, blk_idx, pos_k, w1_k, w2_k), cmp_end)
    cv = compress_blocks(vc, blk_idx, pos_v, w1_v, w2_v)
    s_cmp = jnp.einsum('bqghd,bngd->bqghn', q, ck).astype(jnp.float32) * scale
    valid = (cmp_end[None, :] <= pos[:, None])[None, :, None, None, :]
    p_cmp = masked_softmax(s_cmp, valid) * valid
    o_cmp = jnp.einsum('bqghn,bngd->bqghd', p_cmp.astype(cv.dtype), cv)

    n_sel = s_len // SEL_LEN
    sel_start = jnp.arange(n_sel) * SEL_LEN
    cover = ((cmp_start[:, None] < sel_start[None, :] + SEL_LEN) &
             (cmp_start[:, None] + CMP_LEN > sel_start[None, :])).astype(jnp.float32)
    imp = jnp.einsum('bqghn,nj->bqgj', p_cmp, cover)
    cur = pos // SEL_LEN
    jj = jnp.arange(n_sel)
    forced = (jj[None, :] == 0) | (jj[None, :] == cur[:, None]) | (jj[None, :] == cur[:, None] - 1)
    causal_blk = sel_start[None, :] <= pos[:, None]
    score = jnp.where(forced[None, :, None, :], FORCE_SCORE,
                      jnp.where(causal_blk[None, :, None, :], imp, -1.0))
    n_top = min(SEL_TOPN, n_sel)
    _, sel_idx = lax.top_k(score, n_top)
    ksb = ks.reshape(b, n_sel, SEL_LEN, NSA_KV, HEAD_DIM).transpose(0, 3, 1, 2, 4)
    vsb = vs.reshape(b, n_sel, SEL_LEN, NSA_KV, HEAD_DIM).transpose(0, 3, 1, 2, 4)
    b_ix = jnp.arange(b)[:, None, None, None]
    g_ix = jnp.arange(NSA_KV)[None, None, :, None]

    def sel_block(i):
        t0 = i * SEL_Q_BLOCK
        qi = lax.dynamic_slice_in_dim(q, t0, SEL_Q_BLOCK, axis=1)
        ii = lax.dynamic_slice_in_dim(sel_idx, t0, SEL_Q_BLOCK, axis=1)
        kg = ksb[b_ix, g_ix, ii]
        vg = vsb[b_ix, g_ix, ii]
        s = jnp.einsum('bqghd,bqgnld->bqghnl', qi, kg).astype(jnp.float32) * scale
        tq = t0 + jnp.arange(SEL_Q_BLOCK)
        kpos = ii[..., None] * SEL_LEN + jnp.arange(SEL_LEN)
        m = (kpos <= tq[None, :, None, None, None])[:, :, :, None]
        p = masked_softmax(s.reshape(s.shape[:4] + (-1,)), m.reshape(m.shape[:4] + (-1,))).reshape(s.shape)
        return jnp.einsum('bqghnl,bqgnld->bqghd', p.astype(vg.dtype), vg)

    o_sel = sweep_blocks(sel_block, s_len // SEL_Q_BLOCK)

    span = WINDOW + Q_BLOCK
    kwp = jnp.pad(kw, ((0, 0), (WINDOW, 0), (0, 0), (0, 0)))
    vwp = jnp.pad(vw, ((0, 0), (WINDOW, 0), (0, 0), (0, 0)))

    def win_block(i):
        t0 = i * Q_BLOCK
        qi = lax.dynamic_slice_in_dim(q, t0, Q_BLOCK, axis=1)
        ki = lax.dynamic_slice_in_dim(kwp, t0, span, axis=1)
        vi = lax.dynamic_slice_in_dim(vwp, t0, span, axis=1)
        s = jnp.einsum('bqghd,bkgd->bqghk', qi, ki).astype(jnp.float32) * scale
        tq = t0 + jnp.arange(Q_BLOCK)
        tk = t0 - WINDOW + jnp.arange(span)
        m = (tk[None, :] <= tq[:, None]) & (tk[None, :] > tq[:, None] - WINDOW) & (tk[None, :] >= 0)
        p = masked_softmax(s, m[None, :, None, None, :])
        return jnp.einsum('bqghk,bkgd->bqghd', p.astype(vi.dtype), vi)

    o_win = sweep_blocks(win_block, s_len // Q_BLOCK)

    g = jax.nn.sigmoid(gates.astype(jnp.float32)).reshape(b, s_len, NSA_KV, NSA_GROUP, 3).astype(q.dtype)
    o = g[..., 0:1] * o_cmp + g[..., 1:2] * o_sel + g[..., 2:3] * o_win
    return o.reshape(b, s_len, NSA_HEADS * HEAD_DIM)


def fox_mixer(q, k, v, f_logit, b_forget):
    b, s_len, _ = q.shape
    scale = HEAD_DIM ** -0.5
    hs = (b, s_len, FOX_HEADS, HEAD_DIM)
    q, k, v = q.reshape(hs), k.reshape(hs), v.reshape(hs)
    cumf = jnp.cumsum(jax.nn.log_sigmoid(f_logit.astype(jnp.float32) + b_forget.astype(jnp.float32)),
                      axis=1).transpose(0, 2, 1)
    tk = jnp.arange(s_len)

    def blk(i):
        t0 = i * Q_BLOCK
        qi = lax.dynamic_slice_in_dim(q, t0, Q_BLOCK, axis=1)
        ci = lax.dynamic_slice_in_dim(cumf, t0, Q_BLOCK, axis=2)
        s = (jnp.einsum('bqhd,bkhd->bhqk', qi, k).astype(jnp.float32) * scale
             + ci[..., :, None] - cumf[:, :, None, :])
        tq = t0 + jnp.arange(Q_BLOCK)
        p = masked_softmax(s, tk[None, :] <= tq[:, None])
        return jnp.einsum('bhqk,bkhd->bqhd', p.astype(v.dtype), v)

    o = sweep_blocks(blk, s_len // Q_BLOCK)
    return o.reshape(b, s_len, FOX_HEADS * HEAD_DIM)


def stick_breaking_mixer(q, k, v):
    b, s_len, _ = q.shape
    scale = HEAD_DIM ** -0.5
    hs = (b, s_len, SB_HEADS, HEAD_DIM)
    q, k, v = q.reshape(hs), k.reshape(hs), v.reshape(hs)
    tk = jnp.arange(s_len)

    def blk(i):
        t0 = i * Q_BLOCK
        qi = lax.dynamic_slice_in_dim(q, t0, Q_BLOCK, axis=1)
        z = jnp.einsum('bqhd,bkhd->bhqk', qi, k).astype(jnp.float32) * scale
        tq = t0 + jnp.arange(Q_BLOCK)
        m = tk[None, :] < tq[:, None]
        l = jnp.where(m, jax.nn.log_sigmoid(-z), 0.0)
        rest = lax.cumsum(l, axis=3, reverse=True) - l
        a = jnp.where(m, jnp.exp(jax.nn.log_sigmoid(z) + rest), 0.0)
        return jnp.einsum('bhqk,bkhd->bqhd', a.astype(v.dtype), v)

    o = sweep_blocks(blk, s_len // Q_BLOCK)
    return o.reshape(b, s_len, SB_HEADS * HEAD_DIM)


def hybrid_mixer(h, w_in, b_forget, cmp_pos_k, cmp_w1_k, cmp_w2_k, cmp_pos_v, cmp_w1_v, cmp_w2_v,
                 out_norm_g, w_out):
    b, s_len, _ = h.shape
    u = h @ w_in
    pts = []
    acc = 0
    for sz in IN_SIZES[:-1]:
        acc += sz
        pts.append(acc)
    (qa, kca, vca, ksa, vsa, kwa, vwa, ga, qb, kb, vb, fb, qc, kcc, vcc) = jnp.split(u, pts, axis=-1)
    o_a = nsa_mixer(qa, kca, vca, ksa, vsa, kwa, vwa, ga,
                    cmp_pos_k, cmp_w1_k, cmp_w2_k, cmp_pos_v, cmp_w1_v, cmp_w2_v)
    o_b = fox_mixer(qb, kb, vb, fb, b_forget)
    o_c = stick_breaking_mixer(qc, kcc, vcc)
    o = jnp.concatenate([o_a, o_b, o_c], axis=-1).reshape(b, s_len, N_HEADS, HEAD_DIM)
    o = rmsnorm(o, out_norm_g.reshape(N_HEADS, HEAD_DIM))
    return o.reshape(b, s_len, MIX_WIDTH) @ w_out


def hier_moe(h, wg, bg, we, be, w1, w3, w2):
    b, s_len, d = h.shape
    t = b * s_len
    xt = h.reshape(t, d)
    gp = jax.nn.softmax((xt @ wg).astype(jnp.float32) + bg.astype(jnp.float32), axis=-1)
    pg, gi = lax.top_k(gp, 1)
    el = ((xt @ we).astype(jnp.float32) + be.astype(jnp.float32)).reshape(t, N_GROUPS, EXPERTS_PER_GROUP)
    el_sel = el[jnp.arange(t), gi[:, 0]]
    pe, ei = lax.top_k(jax.nn.softmax(el_sel, axis=-1), EXPERT_TOPK)
    pe = pe / jnp.sum(pe, axis=-1, keepdims=True)
    wts = (pg * pe).reshape(-1)
    eid = (gi * EXPERTS_PER_GROUP + ei).reshape(-1)
    tok = jnp.repeat(jnp.arange(t, dtype=jnp.int32), EXPERT_TOPK)
    n_assign = t * EXPERT_TOPK
    order = jnp.argsort(eid)
    e_sorted = eid[order]
    counts = jnp.zeros((N_EXPERTS,), jnp.int32).at[eid].add(1)
    starts = jnp.cumsum(counts) - counts
    padded = ((counts + MOE_CHUNK - 1) // MOE_CHUNK) * MOE_CHUNK
    pends = jnp.cumsum(padded)
    pstarts = pends - padded
    dest = pstarts[e_sorted] + (jnp.arange(n_assign) - starts[e_sorted])
    n_chunks = -(-(n_assign + N_EXPERTS * (MOE_CHUNK - 1)) // MOE_CHUNK)
    p_rows = n_chunks * MOE_CHUNK
    buf_tok = jnp.full((p_rows,), t, jnp.int32).at[dest].set(tok[order])
    buf_w = jnp.zeros((p_rows,), jnp.float32).at[dest].set(wts[order])
    chunk_e = jnp.clip(jnp.searchsorted(pends, jnp.arange(n_chunks, dtype=jnp.int32) * MOE_CHUNK,
                                        side='right'), 0, N_EXPERTS - 1)
    xpad = jnp.concatenate([xt, jnp.zeros((1, d), xt.dtype)], axis=0)
    xs = xpad[buf_tok].reshape(n_chunks, MOE_CHUNK, d)

    def expert_chunk(args):
        xc, e = args
        return (jax.nn.silu(xc @ w1[e]) * (xc @ w3[e])) @ w2[e]

    ys = lax.map(expert_chunk, (xs, chunk_e)).reshape(p_rows, d)
    out = jnp.zeros((t + 1, d), ys.dtype).at[buf_tok].add(ys * buf_w[:, None].astype(ys.dtype))
    return out[:t].reshape(b, s_len, d)


def setup_inputs(seed: int = 0) -> dict:
    key = jax.random.key(seed)
    ks = jax.random.split(key, 24)
    L, D = DEPTH, D_MODEL

    def nrm(k, shape, s):
        return s * jax.random.normal(k, shape, jnp.float32)

    return {
        'x': nrm(ks[0], (BATCH, SEQ, D), 1.0),
        'c': nrm(ks[1], (BATCH, D), 1.0),
        'norm1_g': 1.0 + nrm(ks[2], (L, D), 0.02),
        'norm2_g': 1.0 + nrm(ks[3], (L, D), 0.02),
        'ada_w': nrm(ks[4], (L, D, 6 * D), 0.5 * D ** -0.5),
        'ada_b': nrm(ks[5], (L, 6 * D), 0.02),
        'w_in': nrm(ks[6], (L, D, D_IN), D ** -0.5),
        'b_forget': 2.0 + nrm(ks[7], (L, FOX_HEADS), 0.1),
        'cmp_pos_k': nrm(ks[8], (L, CMP_LEN, HEAD_DIM), 0.1),
        'cmp_w1_k': nrm(ks[9], (L, CMP_LEN * HEAD_DIM, CMP_HIDDEN), (CMP_LEN * HEAD_DIM) ** -0.5),
        'cmp_w2_k': nrm(ks[10], (L, CMP_HIDDEN, HEAD_DIM), CMP_HIDDEN ** -0.5),
        'cmp_pos_v': nrm(ks[11], (L, CMP_LEN, HEAD_DIM), 0.1),
        'cmp_w1_v': nrm(ks[12], (L, CMP_LEN * HEAD_DIM, CMP_HIDDEN), (CMP_LEN * HEAD_DIM) ** -0.5),
        'cmp_w2_v': nrm(ks[13], (L, CMP_HIDDEN, HEAD_DIM), CMP_HIDDEN ** -0.5),
        'out_norm_g': 1.0 + nrm(ks[14], (L, MIX_WIDTH), 0.02),
        'w_out': nrm(ks[15], (L, MIX_WIDTH, D), MIX_WIDTH ** -0.5),
        'router_group_w': nrm(ks[16], (L, D, N_GROUPS), D ** -0.5),
        'router_group_b': nrm(ks[17], (L, N_GROUPS), 0.01),
        'router_expert_w': nrm(ks[18], (L, D, N_EXPERTS), D ** -0.5),
        'router_expert_b': nrm(ks[19], (L, N_EXPERTS), 0.01),
        'expert_w1': nrm(ks[20], (L, N_EXPERTS, D, D_EXPERT), D ** -0.5),
        'expert_w3': nrm(ks[21], (L, N_EXPERTS, D, D_EXPERT), D ** -0.5),
        'expert_w2': nrm(ks[22], (L, N_EXPERTS, D_EXPERT, D), D_EXPERT ** -0.5),
        'final_g': 1.0 + nrm(ks[23], (D,), 0.02),
    }


def reference(x, c, norm1_g, norm2_g, ada_w, ada_b, w_in, b_forget, cmp_pos_k, cmp_w1_k, cmp_w2_k,
              cmp_pos_v, cmp_w1_v, cmp_w2_v, out_norm_g, w_out, router_group_w, router_group_b,
              router_expert_w, router_expert_b, expert_w1, expert_w3, expert_w2, final_g):
    cond = jax.nn.silu(c)
    for l in range(DEPTH):
        mod = cond @ ada_w[l] + ada_b[l]
        sh1, sc1, g1, sh2, sc2, g2 = jnp.split(mod[:, None, :], 6, axis=-1)
        h = rmsnorm(x, norm1_g[l]) * (1.0 + sc1) + sh1
        x = x + g1 * hybrid_mixer(h, w_in[l], b_forget[l], cmp_pos_k[l], cmp_w1_k[l], cmp_w2_k[l],
                                  cmp_pos_v[l], cmp_w1_v[l], cmp_w2_v[l], out_norm_g[l], w_out[l])
        h = rmsnorm(x, norm2_g[l]) * (1.0 + sc2) + sh2
        x = x + g2 * hier_moe(h, router_group_w[l], router_group_b[l], router_expert_w[l],
                              router_expert_b[l], expert_w1[l], expert_w3[l], expert_w2[l])
    return rmsnorm(x, final_g)
```

```python
import math
import os
from contextlib import ExitStack
import numpy as np
import concourse.bass as bass
import concourse.mybir as mybir
from concourse.bass_utils import run_bass_kernel_spmd

F32 = mybir.dt.float32
BF16 = mybir.dt.bfloat16
AF = mybir.ActivationFunctionType
ALU = mybir.AluOpType
AX = mybir.AxisListType

S_LEN = 2048
D = 1024
NB = 16
KC = 8
DEPTH = 4
D_IN = 2844
NEGB = -30000.0
VW = 72
EPS = 1e-6

EPOCH = 30000
NSEM_ENG = 8
DMA_POOL = {"sp": 12, "act": 4, "pool": 12}
COMPUTE = ("pe", "dve", "act", "pool")
QUEUES = ("pe", "dve", "act", "pool", "sp")


class Sched:
    def __init__(self, nc, stack):
        self.nc = nc
        self.q = {e: [] for e in QUEUES}
        self.cnt = {e: 0 for e in COMPUTE}
        self.sems = {}
        for e in COMPUTE:
            self.sems[e] = [stack.enter_context(nc.semaphore(f"s_{e}{i}")) for i in range(NSEM_ENG)]
        self.dsems = {}
        self.dcnt = {}
        for qn, n in DMA_POOL.items():
            self.dsems[qn] = [stack.enter_context(nc.semaphore(f"d_{qn}{i}")) for i in range(n)]
            self.dcnt[qn] = 0
        self.seen = {e: {} for e in QUEUES}
        self.last_w = {}
        self.readers = {}
        self.all_dma_tokens = {}
        self.n_ops = 0

    def _need(self, eng, tok, same_ok):
        sem, val, prod = tok
        if prod == eng and same_ok:
            return None
        if self.seen[eng].get(sem.name, 0) >= val:
            return None
        return tok

    def _deps(self, eng, r, w, same_raw=True):
        toks = []
        for k in r:
            t = self.last_w.get(k)
            if t is not None:
                n = self._need(eng, t, same_ok=(not same_raw))
                if n:
                    toks.append(n)
            if isinstance(k, str) and k.startswith("bk"):
                for t in self.readers.get(k, {}).values():
                    n = self._need(eng, t, same_ok=True)
                    if n:
                        toks.append(n)
        for k in w:
            t = self.last_w.get(k)
            if t is not None:
                n = self._need(eng, t, same_ok=(not os.environ.get('STRICT_SYNC')) or (not same_raw))
                if n:
                    toks.append(n)
            for t in self.readers.get(k, {}).values():
                n = self._need(eng, t, same_ok=(not os.environ.get('STRICT_SYNC')) or (not same_raw))
                if n:
                    toks.append(n)
        best = {}
        for sem, val, prod in toks:
            if sem.name not in best or best[sem.name][1] < val:
                best[sem.name] = (sem, val, prod)
        return list(best.values())

    def _record(self, eng, tok, r, w):
        for k in w:
            self.last_w[k] = tok
            self.readers[k] = {}
        for k in r:
            if k in w:
                continue
            rk = eng if tok[2] in COMPUTE else tok[0].name
            self.readers.setdefault(k, {})[rk] = tok

    def op(self, eng, fn, r=(), w=(), same_raw=True):
        waits = self._deps(eng, r, w, same_raw)
        g = self.cnt[eng]
        self.cnt[eng] = g + 1
        sem = self.sems[eng][g // EPOCH]
        val = g % EPOCH + 1
        tok = (sem, val, eng)
        for (s, v, p) in waits:
            self.seen[eng][s.name] = max(self.seen[eng].get(s.name, 0), v)
        self.n_ops += 1

        def emit(e, waits=waits, fn=fn, sem=sem):
            for (s, v, p) in waits:
                e.wait_ge(s, v)
            fn(e).then_inc(sem, 1)
        self.q[eng].append(emit)
        self._record(eng, tok, r, w)
        return tok

    def dma(self, qn, out, in_, r=(), w=(), **kw):
        n = self.dcnt[qn]
        self.dcnt[qn] = n + 1
        pool = self.dsems[qn]
        sem = pool[n % len(pool)]
        val = 16 * (n // len(pool) + 1)
        waits = self._deps(qn, r, w, same_raw=True)
        if val > 16 and self.seen[qn].get(sem.name, 0) < val - 16:
            waits = [t for t in waits if t[0].name != sem.name] + [(sem, val - 16, "dma")]
        for (s, v, p) in waits:
            self.seen[qn][s.name] = max(self.seen[qn].get(s.name, 0), v)
        tok = (sem, val, "dma")
        self.n_ops += 1

        def emit(e, waits=waits, sem=sem, out=out, in_=in_, kw=kw):
            for (s, v, p) in waits:
                e.wait_ge(s, v)
            e.dma_start(out=out, in_=in_, **kw).then_inc(sem, 16)
        self.q[qn].append(emit)
        self._record(qn, tok, r, w)
        self.all_dma_tokens[sem.name] = (sem, val, "dma")
        return tok

    def barrier(self):
        toks = []
        for e in COMPUTE:
            g = self.cnt[e]
            if g > 0:
                toks.append((self.sems[e][(g - 1) // EPOCH], (g - 1) % EPOCH + 1, e))
        toks += list(self.all_dma_tokens.values())
        for e in QUEUES:
            ws = [t for t in toks if t[2] != e and self.seen[e].get(t[0].name, 0) < t[1]]
            for (s, v, p) in ws:
                self.seen[e][s.name] = v

            def emit(eo, ws=ws):
                for (s, v, p) in ws:
                    eo.wait_ge(s, v)
            self.q[e].append(emit)
        self.last_w = {}
        self.readers = {}

    def emit_all(self):
        nc = self.nc
        with nc.Block() as block:
            @block.tensor
            def _(e):
                for f in self.q["pe"]:
                    f(e)

            @block.vector
            def _(e):
                for f in self.q["dve"]:
                    f(e)

            @block.scalar
            def _(e):
                for f in self.q["act"]:
                    f(e)

            @block.gpsimd
            def _(e):
                for f in self.q["pool"]:
                    f(e)

            @block.sync
            def _(e):
                for f in self.q["sp"]:
                    f(e)


class Arena:
    def __init__(self, t, nbytes):
        self.t = t
        self.n = nbytes
        self.off = 0

    def reset(self):
        self.off = 0

    def alloc(self, free_shape, dt):
        esz = 4 if dt == F32 else 2
        n_el = int(np.prod(free_shape))
        size = n_el * esz
        off = (self.off + 63) // 64 * 64
        assert off + size <= self.n, f"arena overflow {off + size} > {self.n}"
        self.off = off + size
        v = self.t[:, off // 4:(off + size) // 4]
        if dt != F32:
            v = v.bitcast(dt)
        if len(free_shape) == 2:
            v = v.rearrange("p (a b) -> p a b", a=free_shape[0])
        elif len(free_shape) == 3:
            v = v.rearrange("p (a b c) -> p a b c", a=free_shape[0], b=free_shape[1])
        elif len(free_shape) == 4:
            v = v.rearrange("p (a b c d) -> p a b c d", a=free_shape[0], b=free_shape[1], c=free_shape[2])
        return v


def _col_perm():
    o = {}
    acc = 0
    sizes = [("qa", 512), ("kca", 128), ("vca", 128), ("ksa", 128), ("vsa", 128), ("kwa", 128), ("vwa", 128),
             ("ga", 24), ("qb", 256), ("kb", 256), ("vb", 256), ("fb", 4), ("qc", 256), ("kcc", 256), ("vcc", 256)]
    for n, s in sizes:
        o[n] = acc
        acc += s
    assert acc == D_IN
    perm = []
    for j in range(4):
        perm += list(range(o["qa"] + j * 64, o["qa"] + j * 64 + 64))
        perm += list(range(o["qa"] + (4 + j) * 64, o["qa"] + (4 + j) * 64 + 64))
    perm += list(range(o["ksa"], o["ksa"] + 128)) + list(range(o["kwa"], o["kwa"] + 128))
    perm += list(range(o["vsa"], o["vsa"] + 128)) + list(range(o["vwa"], o["vwa"] + 128))
    perm += list(range(o["kca"], o["kca"] + 128)) + list(range(o["vca"], o["vca"] + 128))
    perm += list(range(o["qb"], o["qb"] + 256)) + list(range(o["kb"], o["kb"] + 256))
    perm += list(range(o["qc"], o["qc"] + 256)) + list(range(o["kcc"], o["kcc"] + 256))
    perm += list(range(o["vb"], o["vb"] + 256)) + list(range(o["vcc"], o["vcc"] + 256))
    perm += list(range(o["fb"], o["fb"] + 4)) + list(range(o["ga"], o["ga"] + 24))
    assert len(perm) == D_IN and len(set(perm)) == D_IN
    return np.array(perm)


PIECES = [(0, 512), (512, 512), (1024, 256), (1280, 512), (1792, 512), (2304, 512), (2816, 28)]

CF = {}
_o = 0
for _n, _w in [("identf", 128), ("onesf", 128), ("triincl", 128), ("cos", 128), ("sin", 128), ("cosc", 8), ("sinc", 8),
               ("A", 512), ("Bc", 512)]:
    CF[_n] = (_o, _w)
    _o += _w
NCF = _o
CB = {}
_o = 0
for _n, _w in [("ident", 128), ("ones", 128), ("tri", 128), ("antitri", 128), ("strictpos", 128), ("trige", 128),
               ("cmpmask", 2048), ("eblk", 2048), ("cover1", 36)]:
    CB[_n] = (_o, _w)
    _o += _w
NCB = _o


def _host_consts():
    p = np.arange(128)
    cf = np.zeros((128, NCF), np.float32)
    cb = np.zeros((128, NCB), np.float32)

    def setf(n, a):
        o, w = CF[n]
        cf[:, o:o + w] = a.reshape(128, w)

    def setb(n, a):
        o, w = CB[n]
        cb[:, o:o + w] = a.reshape(128, w)
    eye = np.eye(128, dtype=np.float32)
    setf("identf", eye)
    setf("onesf", np.ones((128, 128), np.float32))
    setf("triincl", (p[:, None] <= p[None, :]).astype(np.float32))
    half = 8
    inv = np.exp(np.arange(half, dtype=np.float32) * np.float32(-2.0 * math.log(500000.0) / 16)).astype(np.float32)
    pos = (np.arange(NB)[None, :] * 128 + p[:, None]).astype(np.float32)
    ang = pos[:, :, None] * inv[None, None, :]
    setf("cos", np.cos(ang).astype(np.float32))
    setf("sin", np.sin(ang).astype(np.float32))
    posc = (16 * p + 31).astype(np.float32)
    angc = posc[:, None] * inv[None, :]
    setf("cosc", np.cos(angc).astype(np.float32))
    setf("sinc", np.sin(angc).astype(np.float32))
    q = np.arange(NB)[None, :] * 128 + p[:, None]
    cur = q // 64
    j = np.arange(32)[None, None, :]
    A = np.zeros((128, NB, 32), np.float32)
    Bc = np.zeros((128, NB, 32), np.float32)
    causal = (64 * j <= q[:, :, None])
    A[causal] = 1.0
    Bc[~causal] = -1.0
    f0 = (j == 0) & np.ones_like(causal)
    f1 = (j == cur[:, :, None])
    f2 = (j == cur[:, :, None] - 1)
    for fm, val in ((f0, 1.0e4), (f2, 1.0e4 + 64.0), (f1, 1.0e4 + 128.0)):
        A[fm] = 0.0
        Bc[fm] = val
    setf("A", A)
    setf("Bc", Bc)
    setb("ident", eye)
    setb("ones", np.ones((128, 128), np.float32))
    kk = p[:, None]
    qq = p[None, :]
    setb("tri", np.where(kk > qq, NEGB, 0.0).astype(np.float32))
    setb("antitri", np.where(kk <= qq, NEGB, 0.0).astype(np.float32))
    setb("strictpos", np.where(kk >= qq, -NEGB, 0.0).astype(np.float32))
    setb("trige", (p[:, None] >= p[None, :]).astype(np.float32))
    n = p[:, None, None]
    qabs = np.arange(NB)[None, :, None] * 128 + p[None, None, :]
    setb("cmpmask", np.where((16 * n + 31 > qabs) | (n >= 127), NEGB, 0.0).astype(np.float32))
    kb = np.arange(NB)[None, :, None]
    kp = p[None, None, :]
    jj = p[:, None, None]
    setb("eblk", ((jj == 2 * kb + kp // 64) & (jj < 32)).astype(np.float32))
    cover = np.zeros((128, 36), np.float32)
    nn = np.arange(127)[:, None]
    js = np.arange(32)[None, :]
    cover[:127, :32] = ((16 * nn < 64 * js + 64) & (16 * nn + 32 > 64 * js)).astype(np.float32)
    cover[:127, 32] = 1.0
    setb("cover1", cover)
    return cf, cb


def build(depth=DEPTH, upto="all", dbg=(), lite=False):
    nc = bass.Bass("TRN2", target_bir_lowering=False)
    dram = {}

    def din(name, shape, dt=F32):
        dram[name] = nc.dram_tensor(name, list(shape), dt, kind="ExternalInput").ap()
        return dram[name]

    NL = 1 if lite else DEPTH
    x_d = din("x", [S_LEN, D])
    c_d = din("c_fm", [128, KC])
    adaw_d = din("ada_w", [NL, D, 6 * D])
    adab_d = din("ada_b", [NL, 6 * D])
    gcol_d = din("gcols", [128, DEPTH * 3 * KC])
    win_d = din("w_in_p", [NL, D, D_IN])
    bfb_d = din("bfb", [128, DEPTH * 4])
    w1k_d = din("cmp_w1_k", [NL, 2048, 128])
    w1v_d = din("cmp_w1_v", [NL, 2048, 128])
    w2k_d = din("cmp_w2_k", [NL, 128, 64])
    w2v_d = din("cmp_w2_v", [NL, 128, 64])
    pek_d = din("pekT", [NL, 64, 32])
    pev_d = din("pevT", [NL, 64, 32])
    wout_d = din("w_out", [NL, D, D])
    wr_d = din("wr", [NL, D, 36])
    br_d = din("brb", [128, DEPTH * 36])
    ne_l, ne_e = (1, 1) if lite else (DEPTH, 32)
    ew1_d = din("expert_w1", [ne_l, ne_e, D, 512])
    ew3_d = din("expert_w3", [ne_l, ne_e, D, 512])
    ew2_d = din("expert_w2", [ne_l, ne_e, 512, D])
    fg_d = din("fgb", [128, D])
    cf_d = din("cf", [128, NCF])
    cb_d = din("cb", [128, NCB])
    out_d = nc.dram_tensor("out", [S_LEN, D], F32, kind="ExternalOutput").ap()
    ut_d = nc.dram_tensor("ut_scr", [16, 128, S_LEN], BF16, kind="Internal").ap()
    v_d = nc.dram_tensor("v_scr", [3, 128, NB * 4 * VW], BF16, kind="Internal").ap()
    dbg_out = {}

    with ExitStack() as st:
        S = Sched(nc, st)

        def sb(name, shape, dt):
            return st.enter_context(nc.sbuf_tensor(name, list(shape), dt))
        banks = [st.enter_context(nc.psum_tensor(f"bank{i}", [128, 512], F32)) for i in range(8)]
        BK = [f"bk{i}" for i in range(8)]

        x_sb = sb("x_sb", [128, NB, D], F32)
        hT = sb("hT", [128, KC, S_LEN], BF16)
        cf = sb("cf_sb", [128, NCF], F32)
        cbt = sb("cb_sb", [128, NCB], BF16)
        g1b = sb("g1b", [128, D], F32)
        g2b = sb("g2b", [128, D], F32)
        modT = sb("modT", [128, 48], F32)
        AB = sb("AB", [128, 4, KC], F32)
        gcol = sb("gcol", [128, DEPTH * 3 * KC], F32)
        bfb = sb("bfb_sb", [128, DEPTH * 4], F32)
        brb = sb("brb_sb", [128, DEPTH * 36], F32)
        cond_bf = sb("cond_bf", [128, KC], BF16)
        small = sb("small", [128, 256], F32)
        ssq = sb("ssq", [128, NB], F32)
        rstd = sb("rstd", [128, NB], F32)
        ones1 = sb("ones1", [1, 128], F32)
        one11 = sb("one11", [1, 1], F32)
        ARENA_BYTES = 84 * 1024
        arena_t = sb("arena", [128, ARENA_BYTES // 4], F32)
        AR = Arena(arena_t, ARENA_BYTES)

        def cfv(n):
            o, w = CF[n]
            return cf[:, o:o + w]

        def cbv(n):
            o, w = CB[n]
            return cbt[:, o:o + w]
        ident = cbv("ident")
        ones_bf = cbv("ones")
        tri_b = cbv("tri")
        antitri_b = cbv("antitri")
        strictpos_b = cbv("strictpos")
        trige_b = cbv("trige")
        cmpmask = cbv("cmpmask").rearrange("p (a b) -> p a b", a=NB)
        eblk = cbv("eblk").rearrange("p (a b) -> p a b", a=NB)
        cover1 = cbv("cover1")
        identf = cfv("identf")
        onesf = cfv("onesf")
        triincl = cfv("triincl")
        cos_t = cfv("cos").rearrange("p (a b) -> p a b", a=NB)
        sin_t = cfv("sin").rearrange("p (a b) -> p a b", a=NB)
        cosc = cfv("cosc")
        sinc = cfv("sinc")
        A_t = cfv("A").rearrange("p (a b) -> p a b", a=NB)
        Bc_t = cfv("Bc").rearrange("p (a b) -> p a b", a=NB)

        def mm(out, lhsT, rhs, start, stop, r, w, sgc=False):
            S.op("pe", lambda e: e.matmul(out, lhsT=lhsT, rhs=rhs, start=start, stop=stop, skip_group_check=sgc),
                 r=r, w=w, same_raw=False)

        def tp(out, in_, r, w, idn=None):
            idn_ = ident if idn is None else idn
            S.op("pe", lambda e: e.transpose(out=out, in_=in_, identity=idn_), r=r, w=w, same_raw=False)

        def act(out, in_, func, r, w, bias=None, scale=None, accum=None):
            kw = {}
            if bias is not None:
                kw["bias"] = bias
            if scale is not None:
                kw["scale"] = scale
            if accum is not None:
                kw["accum_out"] = accum
            S.op("act", lambda e: e.activation(out=out, in_=in_, func=func, **kw), r=r, w=w)

        def tt(eng, out, in0, in1, op, r, w):
            S.op(eng, lambda e: e.tensor_tensor(out=out, in0=in0, in1=in1, op=op), r=r, w=w)

        def ts(eng, out, in0, s1, op0, r, w, s2=None, op1=None):
            if op1 is None:
                S.op(eng, lambda e: e.tensor_scalar(out=out, in0=in0, scalar1=s1, scalar2=None, op0=op0), r=r, w=w)
            else:
                S.op(eng, lambda e: e.tensor_scalar(out=out, in0=in0, scalar1=s1, scalar2=s2, op0=op0, op1=op1), r=r, w=w)

        def stt(eng, out, in0, scalar, in1, op0, op1, r, w):
            S.op(eng, lambda e: e.scalar_tensor_tensor(out=out, in0=in0, scalar=scalar, in1=in1, op0=op0, op1=op1), r=r, w=w)

        def cp(eng, out, in_, r, w):
            if eng == "act":
                S.op("act", lambda e: e.copy(out=out, in_=in_), r=r, w=w)
            else:
                S.op(eng, lambda e: e.tensor_copy(out=out, in_=in_), r=r, w=w)

        def recip(out, in_, r, w):
            S.op("dve", lambda e: e.reciprocal(out=out, in_=in_), r=r, w=w)

        def memset(eng, ap, val, w):
            S.op(eng, lambda e: e.memset(ap, val), w=w)

        def dump(name, ap, shape, r, dt=F32):
            if name not in dbg:
                return
            if not isinstance(ap, bass.AP):
                ap = ap[:]
            d = nc.dram_tensor("dbg_" + name, list(shape), dt, kind="ExternalOutput").ap()
            dbg_out[name] = d
            S.dma("sp", d, ap, r=r)

        XK = [f"x{tb}" for tb in range(NB)]
        HK = [f"hT{tb}" for tb in range(NB)]

        S.dma("sp", cf[:], cf_d, w=["cf"])
        S.dma("pool", cbt[:], cb_d, w=["cb"])
        S.dma("sp", gcol[:], gcol_d, w=["gcol"])
        S.dma("sp", bfb[:], bfb_d, w=["bfb"])
        S.dma("sp", brb[:], br_d, w=["brb"])
        memset("pool", ones1[:], 1.0, ["ones1"])
        memset("pool", one11[:], 1.0, ["one11"])
        for tb in range(NB):
            S.dma("sp" if tb % 2 == 0 else "act", x_sb[:, tb, :], x_d[tb * 128:(tb + 1) * 128, :], w=[XK[tb]])
        ctmp = small[:, 0:8]
        ctmp2 = small[:, 8:16]
        S.dma("sp", ctmp, c_d, w=["ctmp"])
        act(ctmp2, ctmp, AF.Exp, r=["ctmp"], w=["ctmp2"], scale=-1.0)
        ts("dve", ctmp2, ctmp2, 1.0, ALU.add, r=["ctmp2"], w=["ctmp2"])
        recip(ctmp2, ctmp2, r=["ctmp2"], w=["ctmp2"])
        tt("dve", cond_bf[:], ctmp, ctmp2, ALU.mult, r=["ctmp", "ctmp2"], w=["cond"])

        def layer_mod(l):
            AR.reset()
            adab = [AR.alloc([KC, 512], BF16) for _ in range(2)]
            brow = [AR.alloc([512], F32) for _ in range(2)]
            rowp = [AR.alloc([512], F32) for _ in range(2)]
            for j in range(12):
                i = j % 2
                S.dma("pool", adab[i], adaw_d[l, :, j * 512:(j + 1) * 512].rearrange("(kc p) n -> p kc n", p=128),
                      w=[f"adab{i}"])
                S.dma("sp", brow[i][0:1, :], adab_d[l:l + 1, j * 512:(j + 1) * 512], w=[f"brow{i}"])
                bk = 0 + i
                for kc in range(KC):
                    mm(banks[bk][0:1, :], cond_bf[:, kc:kc + 1], adab[i][:, kc, :], kc == 0, kc == KC - 1,
                       r=["cond", f"adab{i}"], w=[BK[bk]])
                tt("dve", rowp[i][0:1, :], banks[bk][0:1, :], brow[i][0:1, :], ALU.add, r=[BK[bk], f"brow{i}"], w=[f"rowp{i}"])
                if j in (4, 5, 10, 11):
                    dst = g1b if j < 6 else g2b
                    off = (j % 2) * 512
                    mm(banks[2 + i][:, :], ones1[0:1, :], rowp[i][0:1, :], True, True, r=["ones1", f"rowp{i}"], w=[BK[2 + i]])
                    cp("act", dst[:, off:off + 512], banks[2 + i][:, :], r=[BK[2 + i]], w=["g1b" if j < 6 else "g2b"])
                for ii in range(4):
                    cidx = j * 4 + ii
                    mm(banks[4][:, cidx:cidx + 1], rowp[i][0:1, ii * 128:(ii + 1) * 128], one11[0:1, 0:1], True, True,
                       r=[f"rowp{i}", "one11"], w=[BK[4]])
            cp("dve", modT[:], banks[4][:, 0:48], r=[BK[4]], w=["modT"])
            g1c = gcol[:, (l * 3 + 0) * KC:(l * 3 + 1) * KC]
            g2c = gcol[:, (l * 3 + 1) * KC:(l * 3 + 2) * KC]
            stt("dve", AB[:, 0, :], modT[:, 8:16], 1.0, g1c, ALU.add, ALU.mult, r=["modT", "gcol"], w=["AB"])
            cp("dve", AB[:, 1, :], modT[:, 0:8], r=["modT"], w=["AB"])
            stt("dve", AB[:, 2, :], modT[:, 32:40], 1.0, g2c, ALU.add, ALU.mult, r=["modT", "gcol"], w=["AB"])
            cp("dve", AB[:, 3, :], modT[:, 24:32], r=["modT"], w=["AB"])

        def norm_to_hT(which):
            junk = AR.alloc([D], BF16)
            xn = [AR.alloc([D], BF16) for _ in range(2)]
            for tb in range(NB):
                act(junk, x_sb[:, tb, :], AF.Square, r=[XK[tb]], w=["junk", "ssq"], accum=ssq[:, tb:tb + 1])
            act(rstd[:], ssq[:], AF.Ln, r=["ssq"], w=["rstd"], scale=1.0 / D, bias=EPS)
            act(rstd[:], rstd[:], AF.Exp, r=["rstd"], w=["rstd"], scale=-0.5)
            for tb in range(NB):
                i = tb % 2
                ts("dve", xn[i], x_sb[:, tb, :], rstd[:, tb:tb + 1], ALU.mult, r=[XK[tb], "rstd"], w=[f"xn{i}"])
                bk = 6 + i
                bb = banks[bk][:].bitcast(BF16)
                for kc in range(KC):
                    tp(bb[:, kc * 128:(kc + 1) * 128], xn[i][:, kc * 128:(kc + 1) * 128], r=[f"xn{i}", "cb"], w=[BK[bk]])
                for kc in range(KC):
                    act(hT[:, kc, tb * 128:(tb + 1) * 128], bb[:, kc * 128:(kc + 1) * 128], AF.Identity,
                        r=[BK[bk], "AB"], w=[HK[tb]], scale=AB[:, 2 * which, kc:kc + 1], bias=AB[:, 2 * which + 1, kc:kc + 1])

        def in_proj(l, st_):
            wbuf = [AR.alloc([KC, 512], BF16) for _ in range(2)]
            ut32 = [AR.alloc([512], F32) for _ in range(2)]
            ub = [AR.alloc([512], BF16) for _ in range(2)]
            rtmp = [AR.alloc([4, 12, 8], F32) for _ in range(2)]
            stage = [AR.alloc([4, 512], BF16) for _ in range(2)]
            fmst = [AR.alloc([512], BF16) for _ in range(2)]
            vst = [AR.alloc([4, VW], BF16) for _ in range(4)]
            for i in range(4):
                memset("pool", vst[i], 1.0, [f"vst{i}"])
            fraw, sig = st_["fraw"], st_["sig"]
            wcnt = [0]

            def load_w(pi):
                c0, w = PIECES[pi]
                i = wcnt[0] % 2
                wcnt[0] += 1
                S.dma("pool", wbuf[i][:, :, 0:w], win_d[l, :, c0:c0 + w].rearrange("(kc p) n -> p kc n", p=128),
                      w=[f"wbuf{i}"])
                return i

            def rope(src32, dstb, nheads, tb, ri):
                sv = src32.rearrange("p (h d) -> p h d", h=nheads)
                dv = dstb.rearrange("p (h d) -> p h d", h=nheads)
                x1 = sv[:, :, 0:8]
                x2 = sv[:, :, 8:16]
                cs = cos_t[:, tb, :].unsqueeze(1).to_broadcast([128, nheads, 8])
                sn = sin_t[:, tb, :].unsqueeze(1).to_broadcast([128, nheads, 8])
                t = rtmp[ri]
                k = [f"rtmp{ri}"]
                rsk = []
                cp("act", dstb, src32, r=[f"ut32{ri}"], w=[f"ub{ri}"])
                if "0" not in rsk:
                    tt("dve", t[:, 0, 0:nheads, :], x1, cs, ALU.mult, r=[f"ut32{ri}", "cf"], w=k)
                    tt("dve", t[:, 1, 0:nheads, :], x2, sn, ALU.mult, r=[f"ut32{ri}", "cf"], w=k)
                    tt("dve", t[:, 2, 0:nheads, :], x2, cs, ALU.mult, r=[f"ut32{ri}", "cf"], w=k)
                    tt("dve", t[:, 3, 0:nheads, :], x1, sn, ALU.mult, r=[f"ut32{ri}", "cf"], w=k)
                if "1" not in rsk:
                    tt("dve", dv[:, :, 0:8], t[:, 0, 0:nheads, :], t[:, 1, 0:nheads, :], ALU.subtract, r=k, w=[f"ub{ri}"])
                    tt("dve", dv[:, :, 8:16], t[:, 2, 0:nheads, :], t[:, 3, 0:nheads, :], ALU.add, r=k, w=[f"ub{ri}"])

            def tm_piece(pi, handler):
                wi = load_w(pi)
                c0, w = PIECES[pi]
                for tb in range(NB):
                    bk = tb % 2
                    for kc in range(KC):
                        mm(banks[bk][:, 0:w], hT[:, kc, tb * 128:(tb + 1) * 128], wbuf[wi][:, kc, 0:w], kc == 0, kc == KC - 1,
                           r=[HK[tb], f"wbuf{wi}"], w=[BK[bk]])
                    handler(tb, bk)

            def fm_piece(pi, slot0, scales):
                wi = load_w(pi)
                c0, w = PIECES[pi]
                cnt = 0
                for m in range(w // 128):
                    for t4 in range(4):
                        bk = cnt % 2
                        si = cnt % 2
                        cnt += 1
                        for kc in range(KC):
                            mm(banks[bk][:, :], wbuf[wi][:, kc, m * 128:(m + 1) * 128], hT[:, kc, t4 * 512:(t4 + 1) * 512],
                               kc == 0, kc == KC - 1, r=HK[t4 * 4:t4 * 4 + 4] + [f"wbuf{wi}"], w=[BK[bk]])
                        if scales[m] == 1.0:
                            cp("act", fmst[si], banks[bk][:, :], r=[BK[bk]], w=[f"fmst{si}"])
                        else:
                            S.op("act", lambda e, si=si, bk=bk, sc=scales[m]: e.mul(out=fmst[si], in_=banks[bk][:, :], mul=sc),
                                 r=[BK[bk]], w=[f"fmst{si}"])
                        S.dma("sp", ut_d[slot0 + m, :, t4 * 512:(t4 + 1) * 512], fmst[si], r=[f"fmst{si}"], w=[f"ut{slot0 + m}"])

            def h_p1(tb, bk):
                ri = tb % 2
                S.op("act", lambda e: e.mul(out=ut32[ri], in_=banks[bk][:, :], mul=0.125), r=[BK[bk]], w=[f"ut32{ri}"])
                rope(ut32[ri], ub[ri], 8, tb, ri)
                tbk = 2 + ri
                bb = banks[tbk][:].bitcast(BF16)
                for j in range(4):
                    tp(bb[:, j * 128:(j + 1) * 128], ub[ri][:, j * 128:(j + 1) * 128], r=[f"ub{ri}", "cb"], w=[BK[tbk]])
                sgi = (tb // 4) % 2
                cp("act", stage[sgi][:, :, (tb % 4) * 128:(tb % 4 + 1) * 128],
                   bb[:, 0:512].rearrange("p (s t) -> p s t", s=4), r=[BK[tbk]], w=[f"stage{sgi}"])
                if tb % 4 == 3:
                    t4 = tb // 4
                    S.dma("sp", ut_d[0:4, :, t4 * 512:(t4 + 1) * 512].rearrange("s p t -> p s t"), stage[sgi],
                          r=[f"stage{sgi}"], w=["ut0", "ut1", "ut2", "ut3"])
            ipn = int(upto[6:]) if (upto.startswith("inproj") and len(upto) > 6) else 99
            tm_piece(0, h_p1)
            if ipn <= 1:
                return

            import os as _os
            _skip = _os.environ.get("P2SKIP", "").split(",")

            def h_p2(tb, bk):
                ri = tb % 2
                cp("act", ut32[ri][:, 0:256], banks[bk][:, 0:256], r=[BK[bk]], w=[f"ut32{ri}"])
                vi = tb % 4
                if "vcopy" not in _skip:
                    cp("dve", vst[vi][:, :, 0:64], banks[bk][:, 256:512].rearrange("p (h d) -> p h d", h=4), r=[BK[bk]], w=[f"vst{vi}"])
                if "vdma" not in _skip:
                    S.dma("sp", v_d[0, :, tb * 4 * VW:(tb + 1) * 4 * VW], vst[vi].rearrange("p h d -> p (h d)"), r=[f"vst{vi}"], w=["v0"])
                if "rope" not in _skip:
                    rope(ut32[ri][:, 0:256], ub[ri][:, 0:256], 4, tb, ri)
                else:
                    cp("dve", ub[ri][:, 0:256], ut32[ri][:, 0:256], r=[f"ut32{ri}"], w=[f"ub{ri}"])
                if "tp" in _skip:
                    return
                tbk = 2 + ri
                bb = banks[tbk][:].bitcast(BF16)
                for j in range(2):
                    tp(bb[:, j * 128:(j + 1) * 128], ub[ri][:, j * 128:(j + 1) * 128], r=[f"ub{ri}", "cb"], w=[BK[tbk]])
                sgi = (tb // 4) % 2
                cp("act", stage[sgi][:, 0:2, (tb % 4) * 128:(tb % 4 + 1) * 128],
                   bb[:, 0:256].rearrange("p (s t) -> p s t", s=2), r=[BK[tbk]], w=[f"stage{sgi}"])
                if tb % 4 == 3 and "sdma" not in _skip:
                    t4 = tb // 4
                    S.dma("sp", ut_d[4:6, :, t4 * 512:(t4 + 1) * 512].rearrange("s p t -> p s t"), stage[sgi][:, 0:2, :],
                          r=[f"stage{sgi}"], w=["ut4", "ut5"])
            tm_piece(1, h_p2)
            if ipn <= 2:
                return

            fm_piece(2, 6, [1.0, 1.0])
            if ipn <= 3:
                return
            fm_piece(3, 8, [0.125, 0.125, 1.0, 1.0])
            fm_piece(4, 12, [-0.125, -0.125, 1.0, 1.0])
            if ipn <= 5:
                return

            def h_p6(tb, bk):
                for gi in range(2):
                    vi = (2 * tb + gi) % 4
                    cp("dve" if gi == 0 else "act", vst[vi][:, :, 0:64],
                       banks[bk][:, gi * 256:(gi + 1) * 256].rearrange("p (h d) -> p h d", h=4), r=[BK[bk]], w=[f"vst{vi}"])
                    S.dma("sp", v_d[1 + gi, :, tb * 4 * VW:(tb + 1) * 4 * VW], vst[vi].rearrange("p h d -> p (h d)"),
                          r=[f"vst{vi}"], w=[f"v{1 + gi}"])
            tm_piece(5, h_p6)
            if ipn <= 6:
                return

            def h_p7(tb, bk):
                cp("dve", fraw[:, tb, :], banks[bk][:, 0:4], r=[BK[bk]], w=["fraw"])
                cp("dve", sig[:, tb, :], banks[bk][:, 4:28], r=[BK[bk]], w=["sig"])
            tm_piece(6, h_p7)
            act(sig[:], sig[:], AF.Exp, r=["sig"], w=["sig"], scale=-1.0)
            ts("dve", sig[:], sig[:], 1.0, ALU.add, r=["sig"], w=["sig"])
            recip(sig[:], sig[:], r=["sig"], w=["sig"])

        def load_wout(l, wo, stg, kc0, nkc):
            for k in range(nkc):
                kc = kc0 + k
                S.dma("act", stg, wout_d[l, kc * 128:(kc + 1) * 128, :], w=["wostg"])
                ong = gcol[:, (l * 3 + 2) * KC + kc:(l * 3 + 2) * KC + kc + 1]
                stt("dve", wo[:, k, :], stg, ong, g1b[:], ALU.mult, ALU.mult, r=["wostg", "gcol", "g1b"], w=["wo"])

        def out_proj_block(tb, otile_ap, nkc, wo, otk, res):
            i = res["cnt"] % 2
            res["cnt"] += 1
            oT = res["oT"][i]
            tbk = 5
            bb = banks[tbk][:].bitcast(BF16)
            for k in range(nkc):
                tp(bb[:, k * 128:(k + 1) * 128], otile_ap[:, k * 128:(k + 1) * 128], r=[otk, "cb"], w=[BK[tbk]])
            cp("act", oT[:, 0:nkc, :], bb[:, 0:nkc * 128].rearrange("p (k t) -> p k t", k=nkc), r=[BK[tbk]], w=[f"oT{i}"])
            for half in range(2):
                bk = 6 + half
                for k in range(nkc):
                    mm(banks[bk][:, :], oT[:, k, :], wo[:, k, half * 512:(half + 1) * 512], k == 0, k == nkc - 1,
                       r=[f"oT{i}", "wo"], w=[BK[bk]])
                tt("dve", x_sb[:, tb, half * 512:(half + 1) * 512], x_sb[:, tb, half * 512:(half + 1) * 512], banks[bk][:, :],
                   ALU.add, r=[BK[bk], XK[tb]], w=[XK[tb]])

        def head_norm_store(src_ap, nblk, width, dst_fn, rkeys, wkey, scl):
            jk = scl["junk64"]
            sq = scl["sq"]
            for b in range(nblk):
                act(jk, src_ap(b), AF.Square, r=rkeys, w=["junk64", "hn_sq"], accum=sq[:, b:b + 1])
            act(sq[:, nblk:2 * nblk], sq[:, 0:nblk], AF.Ln, r=["hn_sq"], w=["hn_sq"], scale=1.0 / 64, bias=EPS)
            act(sq[:, 2 * nblk:3 * nblk], sq[:, nblk:2 * nblk], AF.Exp, r=["hn_sq"], w=["hn_sq"], scale=-0.5)
            for b in range(nblk):
                ts("dve", dst_fn(b), src_ap(b), sq[:, 2 * nblk + b:2 * nblk + b + 1], ALU.mult, r=rkeys + ["hn_sq"], w=[wkey])

        def fox_attention(l, st_, wo, res):
            qk = st_["qk"]
            V = st_["V"]
            fraw = st_["fraw"]
            PT = [AR.alloc([512], BF16) for _ in range(2)]
            otile = [AR.alloc([4, 256], BF16) for _ in range(2)]
            cpos = AR.alloc([NB, 4], F32)
            cend = AR.alloc([4, NB], F32)
            ncrow = [AR.alloc([512], BF16) for _ in range(2)]
            spf = AR.alloc([NB, 4], F32)
            scl = {"junk64": AR.alloc([64], F32), "sq": AR.alloc([16], F32)}
            for s_ in range(4):
                S.dma("sp", qk[:, s_, :], ut_d[8 + s_, :, :], r=[f"ut{8 + s_}"], w=["qk"])
            S.dma("act", V.rearrange("p a h d -> p (a h d)"), v_d[1, :, :], r=["v1"], w=["V"])
            bf_ = bfb[:, l * 4:(l + 1) * 4]
            tt("dve", spf[:], fraw[:], bf_.unsqueeze(1).to_broadcast([128, NB, 4]), ALU.add, r=["fraw", "bfb"], w=["spf"])
            act(spf[:], spf[:], AF.Exp, r=["spf"], w=["spf"], scale=-1.0)
            act(spf[:], spf[:], AF.Ln, r=["spf"], w=["spf"], bias=1.0)
            spf2 = spf.rearrange("p a h -> p (a h)")
            S.op("pe", lambda e: e.matmul(banks[4][:, 0:64], lhsT=triincl, rhs=spf2, start=True, stop=True),
                 r=["spf", "cf"], w=[BK[4]], same_raw=False)
            S.op("pe", lambda e: e.matmul(banks[4][:, 64:128], lhsT=onesf, rhs=spf2, start=True, stop=True),
                 r=["spf", "cf"], w=[BK[4]], same_raw=False)
            tot = banks[4][:, 64:128].rearrange("p (a h) -> p a h", a=NB)
            cp("dve", cend[:, :, 0], tot[:, 0, :], r=[BK[4]], w=["cend"])
            for tb in range(1, NB):
                tt("dve", cend[:, :, tb], cend[:, :, tb - 1], tot[:, tb, :], ALU.add, r=[BK[4], "cend"], w=["cend"])
            win_ = banks[4][:, 0:64].rearrange("p (a h) -> p a h", a=NB)
            cp("dve", cpos[:, 0, :], win_[:, 0, :], r=[BK[4]], w=["cpos"])
            for tb in range(1, NB):
                tt("dve", cpos[:, tb, :], win_[:, tb, :], cend[:, :, tb - 1], ALU.add, r=[BK[4], "cend"], w=["cpos"])
            dump("cpos", cpos, [128, NB, 4], r=["cpos"])
            it = 0
            for Q in range(4):
                oi = Q % 2
                for h in range(4):
                    hp = (h % 2) * 64
                    sq_, sk_ = h // 2, 2 + h // 2
                    obk = 2 + (it % 2)
                    nkb = 4 * Q + 4
                    nci = (Q * 4 + h) % 2
                    ts("dve", ncrow[nci][0:1, :].rearrange("p (a b) -> p a b", a=4),
                       cend[0:1, h, 4 * Q:4 * Q + 4].unsqueeze(2).to_broadcast([1, 4, 128]), -1.0, ALU.mult,
                       r=["cend"], w=[f"ncrow{nci}"])
                    for kb in range(nkb):
                        c0b = max(0, kb - 4 * Q)
                        c0 = c0b * 128
                        n = 512 - c0
                        sbk = it % 2
                        pi_ = it % 2
                        it += 1
                        diag = kb >= 4 * Q
                        mm(banks[sbk][:, 0:n], qk[hp:hp + 64, sk_, kb * 128:(kb + 1) * 128],
                           qk[hp:hp + 64, sq_, Q * 512 + c0:(Q + 1) * 512], True, False, r=["qk"], w=[BK[sbk]])
                        mm(banks[sbk][:, 0:n], ones_bf[0:1, :], ncrow[nci][0:1, c0:512], False, not diag,
                           r=["cb", f"ncrow{nci}"], w=[BK[sbk]])
                        if diag:
                            mm(banks[sbk][:, 0:128], ident, tri_b, False, True, r=["cb"], w=[BK[sbk]])
                        act(PT[pi_][:, c0:512], banks[sbk][:, 0:n], AF.Exp, r=[BK[sbk], "cpos"], w=[f"PT{pi_}"],
                            bias=cpos[:, kb, h:h + 1])
                        for qbl in range(c0b, 4):
                            mm(banks[obk][:, qbl * 65:(qbl + 1) * 65], PT[pi_][:, qbl * 128:(qbl + 1) * 128], V[:, kb, h, 0:65],
                               (kb == 0 and qbl == 0), kb == 4 * Q + qbl, r=[f"PT{pi_}", "V"], w=[BK[obk]], sgc=True)
                    ob = banks[obk]
                    rs = scl["sq"][:, 12:16]
                    for qbl in range(4):
                        cp("dve", rs[:, qbl:qbl + 1], ob[:, qbl * 65 + 64:qbl * 65 + 65], r=[BK[obk]], w=["fx_rs"])
                    recip(rs, rs, r=["fx_rs"], w=["fx_rs"])
                    onrm = st_["onrm"]
                    for qbl in range(4):
                        ts("dve", onrm[:, qbl, :], ob[:, qbl * 65:qbl * 65 + 64], rs[:, qbl:qbl + 1], ALU.mult,
                           r=[BK[obk], "fx_rs"], w=["onrm"])
                    head_norm_store(lambda b: onrm[:, b, :], 4, 64, lambda b: otile[oi][:, b, h * 64:(h + 1) * 64],
                                    ["onrm"], f"otile{oi}", scl)
                for qbl in range(4):
                    out_proj_block(4 * Q + qbl, otile[oi][:, qbl, :], 2, wo, f"otile{oi}", res)

        def sb_attention(l, st_, wo, res):
            qk = st_["qk"]
            V = st_["V"]
            Et = [AR.alloc([512], F32) for _ in range(2)]
            SPt = [AR.alloc([512], BF16) for _ in range(2)]
            AT = [AR.alloc([512], BF16) for _ in range(2)]
            SPsum = AR.alloc([512], BF16)
            otile = [AR.alloc([4, 256], BF16) for _ in range(2)]
            scl = {"junk64": AR.alloc([64], F32), "sq": AR.alloc([16], F32)}
            for s_ in range(4):
                S.dma("sp", qk[:, s_, :], ut_d[12 + s_, :, :], r=[f"ut{12 + s_}"], w=["qk"])
            S.dma("act", V.rearrange("p a h d -> p (a h d)"), v_d[2, :, :], r=["v2"], w=["V"])
            it = 0
            for Q in range(4):
                oi = Q % 2
                for h in range(4):
                    hp = (h % 2) * 64
                    sq_, sk_ = h // 2, 2 + h // 2
                    obk = 4 + (it % 2)
                    memset("pool", SPsum, 0.0, ["SPsum"])
                    first = True
                    for kb in range(4 * Q + 3, -1, -1):
                        c0b = max(0, kb - 4 * Q)
                        c0 = c0b * 128
                        n = 512 - c0
                        i2 = it % 2
                        it += 1
                        zbk = 0 + i2
                        cbk = 2 + i2
                        diag = kb >= 4 * Q
                        kT = qk[hp:hp + 64, sk_, kb * 128:(kb + 1) * 128]
                        nq = qk[hp:hp + 64, sq_, Q * 512 + c0:(Q + 1) * 512]
                        mm(banks[zbk][:, 0:n], kT, nq, True, not diag, r=["qk"], w=[BK[zbk]])
                        if diag:
                            mm(banks[zbk][:, 0:128], ident, strictpos_b, False, True, r=["cb"], w=[BK[zbk]])
                        act(Et[i2][:, 0:n], banks[zbk][:, 0:n], AF.Exp, r=[BK[zbk]], w=[f"Et{i2}"], scale=-1.0)
                        act(SPt[i2][:, 0:n], Et[i2][:, 0:n], AF.Ln, r=[f"Et{i2}"], w=[f"SPt{i2}"], bias=1.0)
                        mm(banks[cbk][:, 0:n], trige_b, SPt[i2][:, 0:n], True, False, r=[f"SPt{i2}", "cb"], w=[BK[cbk]])
                        if not first:
                            mm(banks[cbk][:, 0:n], ones_bf, SPsum[:, c0:512], False, False, r=["SPsum", "cb"], w=[BK[cbk]])
                        mm(banks[cbk][:, 0:n], kT, nq, False, not diag, r=["qk"], w=[BK[cbk]])
                        if diag:
                            mm(banks[cbk][:, 0:128], ident, strictpos_b, False, True, r=["cb"], w=[BK[cbk]])
                        act(AT[i2][:, 0:n], banks[cbk][:, 0:n], AF.Exp, r=[BK[cbk]], w=[f"AT{i2}"], scale=-1.0)
                        tt("dve", SPsum[:, c0:512], SPsum[:, c0:512], SPt[i2][:, 0:n], ALU.add, r=["SPsum", f"SPt{i2}"], w=["SPsum"])
                        for qbl in range(c0b, 4):
                            mm(banks[obk][:, qbl * 64:(qbl + 1) * 64], AT[i2][:, qbl * 128 - c0:(qbl + 1) * 128 - c0], V[:, kb, h, 0:64],
                               first and qbl == 3, kb == 0, r=[f"AT{i2}", "V"], w=[BK[obk]], sgc=True)
                        first = False
                    ob = banks[obk]
                    head_norm_store(lambda b: ob[:, b * 64:(b + 1) * 64], 4, 64, lambda b: otile[oi][:, b, h * 64:(h + 1) * 64],
                                    [BK[obk]], f"otile{oi}", scl)
                for qbl in range(4):
                    out_proj_block(4 * Q + qbl, otile[oi][:, qbl, :], 2, wo, f"otile{oi}", res)

        def nsa_attention(l, st_, wo, res):
            qk = st_["qk"]
            V = st_["V"]
            sig = st_["sig"]
            W1 = AR.alloc([32, 128], BF16)
            w2d = AR.alloc([128], BF16)
            peT = AR.alloc([32], BF16)
            hidS = AR.alloc([128], BF16)
            cktok = AR.alloc([128], BF16)
            ckT = AR.alloc([128], BF16)
            CVX = AR.alloc([2, 100], BF16)
            csm = AR.alloc([64], F32)
            htmp = AR.alloc([3, 128], F32)
            ctmp_ = AR.alloc([6, 8], F32)
            ET = [AR.alloc([512], BF16) for _ in range(2)]
            PT = [AR.alloc([512], BF16) for _ in range(2)]
            selT = AR.alloc([4, 128], BF16)
            selb = AR.alloc([32], BF16)
            OACC = AR.alloc([4, 64], F32)
            sc32 = AR.alloc([4, 32], F32)
            m8 = AR.alloc([16], F32)
            nsm = AR.alloc([32], F32)
            otile = [AR.alloc([512], BF16) for _ in range(2)]
            scl = {"junk64": AR.alloc([64], F32), "sq": AR.alloc([16], F32)}
            for s_ in range(8):
                S.dma("sp", qk[:, s_, :], ut_d[s_, :, :], r=[f"ut{s_}"], w=["qk"])
            S.dma("act", V.rearrange("p a h d -> p (a h d)"), v_d[0, :, :], r=["v0"], w=["V"])
            memset("pool", CVX, 0.0, ["CVX"])
            memset("pool", ckT, 0.0, ["ckT"])
            for g in range(2):
                cp("pool", CVX[:, g, 0:36], cover1, r=["cb"], w=["CVX"])

            for kind in range(2):
                w1_d = (w1k_d, w1v_d)[kind]
                w2_d = (w2k_d, w2v_d)[kind]
                pe_d = (pek_d, pev_d)[kind]
                slot = 6 + kind
                for hf in range(2):
                    S.dma("pool", W1[hf * 64:(hf + 1) * 64, :, :], w1_d[l].rearrange("(l d) h -> d l h", d=64), w=["W1"])
                    S.dma("pool", w2d[:, hf * 64:(hf + 1) * 64], w2_d[l], w=["w2d"])
                S.dma("pool", peT[0:64, :], pe_d[l], w=["peT"])
                bb = 4
                for l_ in range(32):
                    mm(banks[bb][:, 0:1], W1[0:64, l_, :], peT[0:64, l_:l_ + 1], l_ == 0, l_ == 31, r=["W1", "peT"], w=[BK[bb]])
                cp("dve", csm[:, 0:1], banks[bb][:, 0:1], r=[BK[bb]], w=["csm"])
                ts("dve", csm[:, 1:2], csm[:, 0:1], -1.0, ALU.mult, r=["csm"], w=["csm"])
                for g in range(2):
                    hb = 5
                    src = qk[g * 64:(g + 1) * 64, slot, :].rearrange("p (n s) -> p n s", s=16)
                    for l_ in range(32):
                        rhs = src[:, 0:127, l_] if l_ < 16 else src[:, 1:128, l_ - 16]
                        mm(banks[hb][:, 0:127], W1[g * 64:(g + 1) * 64, l_, :], rhs, l_ == 0, l_ == 31, r=["W1", "qk"], w=[BK[hb]])
                    act(htmp[:, 0, 0:127], banks[hb][:, 0:127], AF.Exp, r=[BK[hb], "csm"], w=["htmp"], scale=-1.0, bias=csm[:, 1:2])
                    ts("dve", htmp[:, 0, 0:127], htmp[:, 0, 0:127], 1.0, ALU.add, r=["htmp"], w=["htmp"])
                    recip(htmp[:, 0, 0:127], htmp[:, 0, 0:127], r=["htmp"], w=["htmp"])
                    stt("dve", hidS[:, 0:127], banks[hb][:, 0:127], csm[:, 0:1], htmp[:, 0, 0:127], ALU.add, ALU.mult,
                        r=[BK[hb], "csm", "htmp"], w=["hidS"])
                    ob_ = 6
                    mm(banks[ob_][0:127, 0:64], hidS[:, 0:127], w2d[:, 0:64], True, True, r=["hidS", "w2d"], w=[BK[ob_]])
                    if kind == 0:
                        srcp = banks[ob_][0:127, 0:64]
                        x1, x2 = srcp[:, 0:8], srcp[:, 8:16]
                        c_, s__ = cosc[0:127, :], sinc[0:127, :]
                        t_ = ctmp_
                        tt("dve", t_[0:127, 0, :], x1, c_, ALU.mult, r=[BK[ob_], "cf"], w=["ctmp_"])
                        tt("dve", t_[0:127, 1, :], x2, s__, ALU.mult, r=[BK[ob_], "cf"], w=["ctmp_"])
                        tt("dve", t_[0:127, 2, :], x2, c_, ALU.mult, r=[BK[ob_], "cf"], w=["ctmp_"])
                        tt("dve", t_[0:127, 3, :], x1, s__, ALU.mult, r=[BK[ob_], "cf"], w=["ctmp_"])
                        tt("dve", cktok[0:127, g * 64:g * 64 + 8], t_[0:127, 0, :], t_[0:127, 1, :], ALU.subtract, r=["ctmp_"], w=["cktok"])
                        tt("dve", cktok[0:127, g * 64 + 8:g * 64 + 16], t_[0:127, 2, :], t_[0:127, 3, :], ALU.add, r=["ctmp_"], w=["cktok"])
                        cp("dve", cktok[0:127, g * 64 + 16:g * 64 + 64], srcp[:, 16:64], r=[BK[ob_]], w=["cktok"])
                    else:
                        cp("dve", CVX[0:127, g, 36:100], banks[ob_][0:127, 0:64], r=[BK[ob_]], w=["CVX"])
                if kind == 0:
                    tbk = 7
                    bbv = banks[tbk][:].bitcast(BF16)
                    tp(bbv[:, 0:127], cktok[0:127, :], r=["cktok", "cb"], w=[BK[tbk]], idn=ident[0:127, 0:127])
                    cp("dve", ckT[:, 0:127], bbv[:, 0:127], r=[BK[tbk]], w=["ckT"])
            import os as _os
            nstop = _os.environ.get("NSASTOP", "")
            if nstop == "cmpr":
                return
            dump("ckT", ckT, [128, 128], r=["ckT"], dt=BF16)
            dump("CVX", CVX, [128, 2, 100], r=["CVX"], dt=BF16)

            it = 0
            nqb = int(_os.environ.get("NSAQB", "16"))
            qbl_ = [int(v) for v in _os.environ["NSAQBLIST"].split(",")] if _os.environ.get("NSAQBLIST") else list(range(nqb))
            for qb in qbl_:
                oi = qb % 2
                for g in range(2):
                    gp = g * 64
                    qrhs = qk[gp:gp + 64, 0:4, qb * 128:(qb + 1) * 128]
                    nv = 128
                    i2 = it % 2
                    it += 1
                    sbk = 0 + i2
                    mm(banks[sbk][0:nv, :], ckT[gp:gp + 64, 0:nv], qrhs, True, False, r=["ckT", "qk"], w=[BK[sbk]])
                    for h in range(4):
                        mm(banks[sbk][0:nv, h * 128:(h + 1) * 128], ident[0:nv, 0:nv], cmpmask[0:nv, qb, :], False, h == 3,
                           r=["cb"], w=[BK[sbk]])
                    act(ET[i2][0:nv, :], banks[sbk][0:nv, :], AF.Exp, r=[BK[sbk]], w=[f"ET{i2}"])
                    rbk = 2
                    for h in range(4):
                        mm(banks[rbk][:, h * 100:(h + 1) * 100], ET[i2][0:nv, h * 128:(h + 1) * 128], CVX[0:nv, g, :], h == 0, h == 3,
                           r=[f"ET{i2}", "CVX"], w=[BK[rbk]], sgc=True)
                    R = banks[rbk][:, 0:400].rearrange("p (h c) -> p h c", h=4)
                    rinv = nsm[:, 0:4]
                    gfac = nsm[:, 4:8]
                    ts("dve", rinv, R[:, :, 32], 1e-30, ALU.add, r=[BK[rbk]], w=["nsm"])
                    recip(rinv, rinv, r=["nsm"], w=["nsm"])
                    sg = sig[:, qb, g * 12:(g + 1) * 12].rearrange("p (h b) -> p h b", b=3)
                    tt("dve", gfac, rinv, sg[:, :, 0], ALU.mult, r=["nsm", "sig"], w=["nsm"])
                    imp = sc32[:, 0, :]
                    ts("dve", imp, R[:, 0, 0:32], rinv[:, 0:1], ALU.mult, r=[BK[rbk], "nsm"], w=["sc32"])
                    for h in range(1, 4):
                        stt("dve", imp, R[:, h, 0:32], rinv[:, h:h + 1], imp, ALU.mult, ALU.add, r=[BK[rbk], "nsm", "sc32"], w=["sc32"])
                    for h in range(4):
                        ts("dve", OACC[:, h, :], R[:, h, 36:100], gfac[:, h:h + 1], ALU.mult, r=[BK[rbk], "nsm"], w=["OACC"])
                    if nstop == "cmp" and qb == int(_os.environ.get("NSASTOPQB", "0")) and g == int(_os.environ.get("NSASTOPG", "0")):
                        return
                    score = sc32[:, 1, :]
                    sc2 = sc32[:, 2, :]
                    tt("dve", score, imp, A_t[:, qb, :], ALU.mult, r=["sc32", "cf"], w=["sc32"])
                    tt("dve", score, score, Bc_t[:, qb, :], ALU.add, r=["sc32", "cf"], w=["sc32"])
                    S.op("dve", lambda e: e.max(out=m8[:, 0:8], in_=score), r=["sc32"], w=["m8"])
                    S.op("dve", lambda e: e.match_replace(out=sc2, in_to_replace=m8[:, 0:8], in_values=score, imm_value=-1.0e9),
                         r=["sc32", "m8"], w=["sc32"])
                    S.op("dve", lambda e: e.max(out=m8[:, 8:16], in_=sc2), r=["sc32"], w=["m8"])
                    ts("dve", sc32[:, 3, :], score, m8[:, 15:16], ALU.is_ge, r=["sc32", "m8"], w=["sc32"], s2=-NEGB, op1=ALU.mult)
                    ts("dve", selb, sc32[:, 3, :], NEGB, ALU.add, r=["sc32"], w=["selb"])
                    tbk = 3
                    bbv = banks[tbk][:].bitcast(BF16)
                    tp(bbv[0:32, 0:128], selb, r=["selb", "cb"], w=[BK[tbk]])
                    cp("dve", selT[0:32, :, :], bbv[0:32, 0:128].unsqueeze(1).to_broadcast([32, 4, 128]), r=[BK[tbk]], w=["selT"])
                    if "sel" in dbg and qb == 9 and g == 1:
                        dump("selb", selb, [128, 32], r=["selb"], dt=BF16)
                        dump("imp", sc32, [128, 4, 32], r=["sc32"])
                    if nstop == "selc" and qb == int(_os.environ.get("NSASTOPQB", "0")) and g == int(_os.environ.get("NSASTOPG", "0")):
                        return
                    for br in (1, 2):
                        kslot = 4 if br == 1 else 5
                        vh = (0 if br == 1 else 2) + g
                        kb0 = 0 if br == 1 else max(0, qb - 4)
                        obk = 4 + (br - 1)
                        for kb in range(kb0, qb + 1):
                            i2 = it % 2
                            it += 1
                            sbk = 0 + i2
                            last_plain = not (br == 1 or kb == qb or (br == 2 and kb == qb - 4))
                            mm(banks[sbk][:, :], qk[gp:gp + 64, kslot, kb * 128:(kb + 1) * 128], qrhs, True, last_plain,
                               r=["qk"], w=[BK[sbk]])
                            if br == 1:
                                mm(banks[sbk][:, :], eblk[0:32, kb, :], selT[0:32, :, :], False, kb != qb, r=["cb", "selT"], w=[BK[sbk]])
                            if kb == qb:
                                for h in range(4):
                                    mm(banks[sbk][:, h * 128:(h + 1) * 128], ident, tri_b, False, h == 3, r=["cb"], w=[BK[sbk]])
                            if br == 2 and kb == qb - 4 and not _os.environ.get("NOANTI"):
                                for h in range(4):
                                    mm(banks[sbk][:, h * 128:(h + 1) * 128], ident, antitri_b, False, h == 3, r=["cb"], w=[BK[sbk]])
                            act(PT[i2], banks[sbk][:, :], AF.Exp, r=[BK[sbk]], w=[f"PT{i2}"])
                            for h in range(4):
                                mm(banks[obk][:, h * 65:(h + 1) * 65], PT[i2][:, h * 128:(h + 1) * 128], V[:, kb, vh, 0:65],
                                   kb == kb0 and h == 0, kb == qb, r=[f"PT{i2}", "V"], w=[BK[obk]], sgc=True)
                        O = banks[obk][:, 0:260].rearrange("p (h c) -> p h c", h=4)
                        rv = nsm[:, 8 + 8 * (br - 1):12 + 8 * (br - 1)]
                        gf = nsm[:, 12 + 8 * (br - 1):16 + 8 * (br - 1)]
                        cp("dve", rv, O[:, :, 64], r=[BK[obk]], w=["nsm"])
                        recip(rv, rv, r=["nsm"], w=["nsm"])
                        tt("dve", gf, rv, sg[:, :, br], ALU.mult, r=["nsm", "sig"], w=["nsm"])
                        for h in range(4):
                            stt("dve", OACC[:, h, :], O[:, h, 0:64], gf[:, h:h + 1], OACC[:, h, :], ALU.mult, ALU.add,
                                r=[BK[obk], "nsm", "OACC"], w=["OACC"])
                    if nstop == "br" and qb == int(_os.environ.get("NSASTOPQB", "0")) and g == int(_os.environ.get("NSASTOPG", "0")):
                        return
                    head_norm_store(lambda b: OACC[:, b, :], 4, 64, lambda b: otile[oi][:, (g * 4 + b) * 64:(g * 4 + b + 1) * 64],
                                    ["OACC"], f"otile{oi}", scl)
                if "otile" in dbg and qb == 9:
                    dump("otile", otile[oi], [128, 512], r=[f"otile{oi}"], dt=BF16)
                out_proj_block(qb, otile[oi], 4, wo, f"otile{oi}", res)

        def moe(l):
            AR.reset()
            W1e = [AR.alloc([KC, 512], BF16) for _ in range(2)]
            W3e = [AR.alloc([KC, 512], BF16) for _ in range(2)]
            W2e = [AR.alloc([4, D], BF16) for _ in range(2)]
            G = [AR.alloc([4, 512], BF16) for _ in range(2)]
            St = [AR.alloc([512], BF16) for _ in range(2)]
            wr_bf = AR.alloc([KC, 36], BF16)
            lg = AR.alloc([NB, 36], F32)
            Wg = AR.alloc([NB, 32], F32)
            elm = AR.alloc([NB, 32], F32)
            gtmp = AR.alloc([6, NB, 4], F32)
            m8 = AR.alloc([NB, 8], F32)
            ptmp = AR.alloc([8, NB], F32)
            eq = AR.alloc([NB, 32], F32)
            norm_to_hT(1)
            S.dma("pool", wr_bf, wr_d[l].rearrange("(kc p) n -> p kc n", p=128), w=["wr_bf"])
            for tb in range(NB):
                bk = tb % 2
                for kc in range(KC):
                    mm(banks[bk][:, 0:36], hT[:, kc, tb * 128:(tb + 1) * 128], wr_bf[:, kc, :], kc == 0, kc == KC - 1,
                       r=[HK[tb], "wr_bf"], w=[BK[bk]])
                tt("dve", lg[:, tb, :], banks[bk][:, 0:36], brb[:, l * 36:(l + 1) * 36], ALU.add, r=[BK[bk], "brb"], w=["lg"])
            gl = lg[:, :, 0:4]
            el = lg[:, :, 4:36]
            gmax = ptmp[:, 0, :]
            S.op("dve", lambda e: e.tensor_reduce(out=gmax, in_=gl, axis=AX.X, op=ALU.max), r=["lg"], w=["ptmp"])
            gmb = gmax.unsqueeze(2).to_broadcast([128, NB, 4])
            tt("dve", gtmp[:, 0, :, :], gl, gmb, ALU.is_ge, r=["lg", "ptmp"], w=["gtmp"])
            tt("dve", gtmp[:, 1, :, :], gl, gmb, ALU.subtract, r=["lg", "ptmp"], w=["gtmp"])
            act(gtmp[:, 1, :, :], gtmp[:, 1, :, :], AF.Exp, r=["gtmp"], w=["gtmp"])
            gs = ptmp[:, 1, :]
            S.op("dve", lambda e: e.tensor_reduce(out=gs, in_=gtmp[:, 1, :, :], axis=AX.X, op=ALU.add), r=["gtmp"], w=["ptmp"])
            pg = ptmp[:, 2, :]
            recip(pg, gs, r=["ptmp"], w=["ptmp"])
            ts("dve", gtmp[:, 2, :, :], gtmp[:, 0, :, :], 1.0e9, ALU.mult, r=["gtmp"], w=["gtmp"], s2=-1.0e9, op1=ALU.add)
            tt("dve", elm.rearrange("p a (g e) -> p a g e", g=4), el.rearrange("p a (g e) -> p a g e", g=4),
               gtmp[:, 2, :, :].unsqueeze(3).to_broadcast([128, NB, 4, 8]), ALU.add, r=["lg", "gtmp"], w=["elm"])
            for tb in range(NB):
                S.op("dve", lambda e, tb=tb: e.max(out=m8[:, tb, :], in_=elm[:, tb, :]), r=["elm"], w=["m8"])
            l1 = m8[:, :, 0]
            l2 = m8[:, :, 1]
            d21 = ptmp[:, 3, :]
            tt("dve", d21, l2, l1, ALU.subtract, r=["m8"], w=["ptmp"])
            act(d21, d21, AF.Exp, r=["ptmp"], w=["ptmp"])
            ts("dve", d21, d21, 1.0, ALU.add, r=["ptmp"], w=["ptmp"])
            p1 = ptmp[:, 4, :]
            recip(p1, d21, r=["ptmp"], w=["ptmp"])
            wA = ptmp[:, 5, :]
            wB = ptmp[:, 6, :]
            tt("dve", wA, p1, pg, ALU.mult, r=["ptmp"], w=["ptmp"])
            tt("dve", wB, pg, wA, ALU.subtract, r=["ptmp"], w=["ptmp"])
            tt("dve", eq[:], elm[:], l1.unsqueeze(2).to_broadcast([128, NB, 32]), ALU.is_equal, r=["elm", "m8"], w=["eq"])
            tt("dve", Wg[:], eq[:], wA.unsqueeze(2).to_broadcast([128, NB, 32]), ALU.mult, r=["eq", "ptmp"], w=["Wg"])
            tt("dve", eq[:], elm[:], l2.unsqueeze(2).to_broadcast([128, NB, 32]), ALU.is_equal, r=["elm", "m8"], w=["eq"])
            tt("dve", eq[:], eq[:], wB.unsqueeze(2).to_broadcast([128, NB, 32]), ALU.mult, r=["eq", "ptmp"], w=["eq"])
            tt("dve", Wg[:], Wg[:], eq[:], ALU.add, r=["eq", "Wg"], w=["Wg"])
            dump("Wg", Wg, [128, NB, 32], r=["Wg"])
            if upto == "router":
                return
            it = 0
            for e_ in range(32):
                i = e_ % 2
                S.dma("pool", W1e[i], ew1_d[l, e_].rearrange("(kc p) n -> p kc n", p=128), w=[f"W1e{i}"])
                S.dma("pool", W3e[i], ew3_d[l, e_].rearrange("(kc p) n -> p kc n", p=128), w=[f"W3e{i}"])
                S.dma("pool", W2e[i], ew2_d[l, e_].rearrange("(hc p) n -> p hc n", p=128), w=[f"W2e{i}"])
                for hc in range(4):
                    tt("pool", W2e[i][:, hc, :], W2e[i][:, hc, :], g2b[:], ALU.mult, r=[f"W2e{i}", "g2b"], w=[f"W2e{i}"])
                for t4 in range(4):
                    gi = it % 2
                    it += 1
                    for hc in range(4):
                        b1 = 0 + hc % 2
                        b3 = 2 + hc % 2
                        si = hc % 2
                        for kc in range(KC):
                            mm(banks[b1][:, :], W1e[i][:, kc, hc * 128:(hc + 1) * 128], hT[:, kc, t4 * 512:(t4 + 1) * 512],
                               kc == 0, kc == KC - 1, r=HK[4 * t4:4 * t4 + 4] + [f"W1e{i}"], w=[BK[b1]])
                        for kc in range(KC):
                            mm(banks[b3][:, :], W3e[i][:, kc, hc * 128:(hc + 1) * 128], hT[:, kc, t4 * 512:(t4 + 1) * 512],
                               kc == 0, kc == KC - 1, r=HK[4 * t4:4 * t4 + 4] + [f"W3e{i}"], w=[BK[b3]])
                        act(St[si], banks[b1][:, :], AF.Silu, r=[BK[b1]], w=[f"St{si}"])
                        tt("dve", G[gi][:, hc, :], St[si], banks[b3][:, :], ALU.mult, r=[f"St{si}", BK[b3]], w=[f"G{gi}"])
                    for tbl in range(4):
                        tb = 4 * t4 + tbl
                        for half in range(2):
                            yb = 4 + (2 * tbl + half) % 4
                            for hc in range(4):
                                mm(banks[yb][:, :], G[gi][:, hc, tbl * 128:(tbl + 1) * 128], W2e[i][:, hc, half * 512:(half + 1) * 512],
                                   hc == 0, hc == 3, r=[f"G{gi}", f"W2e{i}"], w=[BK[yb]])
                            xs = x_sb[:, tb, half * 512:(half + 1) * 512]
                            stt("dve", xs, banks[yb][:, :], Wg[:, tb, e_:e_ + 1], xs, ALU.mult, ALU.add,
                                r=[BK[yb], "Wg", XK[tb]], w=[XK[tb]])

        done = False
        for l in range(depth):
            layer_mod(l)
            if upto == "mod":
                dump("modT", modT, [128, 48], r=["modT"])
                dump("g1b", g1b, [128, D], r=["g1b"])
                dump("g2b", g2b, [128, D], r=["g2b"])
                dump("AB", AB, [128, 4, KC], r=["AB"])
                break
            S.barrier()
            AR.reset()
            fraw = AR.alloc([NB, 4], F32)
            sig = AR.alloc([NB, 24], F32)
            st_ = {"fraw": fraw, "sig": sig}
            mark = AR.off
            norm_to_hT(0)
            if upto == "norm":
                dump("hT", hT, [128, KC, S_LEN], r=HK, dt=BF16)
                break
            in_proj(l, st_)
            if upto.startswith("inproj"):
                S.barrier()
                if "ut" in dbg:
                    d = nc.dram_tensor("dbg_ut", [16, 128, S_LEN], BF16, kind="ExternalOutput").ap()
                    dbg_out["ut"] = d
                    for s_ in range(16):
                        S.dma("sp", d[s_], ut_d[s_], r=[f"ut{s_}"])
                    d2 = nc.dram_tensor("dbg_v", [3, 128, NB * 4 * VW], BF16, kind="ExternalOutput").ap()
                    dbg_out["v"] = d2
                    for s_ in range(3):
                        S.dma("sp", d2[s_], v_d[s_], r=[f"v{s_}"])
                dump("sig", sig, [128, NB, 24], r=["sig"])
                dump("fraw", fraw, [128, NB, 4], r=["fraw"])
                break
            S.barrier()
            AR.off = mark
            wo = AR.alloc([4, D], BF16)
            wstg = AR.alloc([D], F32)
            st_["qk"] = AR.alloc([8, S_LEN], BF16)
            st_["V"] = AR.alloc([NB, 4, VW], BF16)
            st_["onrm"] = AR.alloc([4, 64], F32)
            res = {"cnt": 0, "oT": [AR.alloc([4, 128], BF16) for _ in range(2)]}
            mark2 = AR.off
            load_wout(l, wo, wstg, 4, 2)
            fox_attention(l, st_, wo, res)
            if upto == "fox":
                break
            S.barrier()
            AR.off = mark2
            load_wout(l, wo, wstg, 6, 2)
            sb_attention(l, st_, wo, res)
            if upto == "sb":
                break
            S.barrier()
            AR.off = mark2
            load_wout(l, wo, wstg, 0, 4)
            nsa_attention(l, st_, wo, res)
            if upto == "nsa":
                break
            S.barrier()
            moe(l)
            if upto in ("router", "moe1"):
                break
            S.barrier()
        else:
            done = True

        if done:
            AR.reset()
            junk = AR.alloc([D], BF16)
            fgb = AR.alloc([D], F32)
            ob = [AR.alloc([D], F32) for _ in range(2)]
            S.dma("sp", fgb, fg_d, w=["fgb"])
            for tb in range(NB):
                act(junk, x_sb[:, tb, :], AF.Square, r=[XK[tb]], w=["junk", "ssq"], accum=ssq[:, tb:tb + 1])
            act(rstd[:], ssq[:], AF.Ln, r=["ssq"], w=["rstd"], scale=1.0 / D, bias=EPS)
            act(rstd[:], rstd[:], AF.Exp, r=["rstd"], w=["rstd"], scale=-0.5)
            for tb in range(NB):
                i = tb % 2
                stt("dve", ob[i], x_sb[:, tb, :], rstd[:, tb:tb + 1], fgb, ALU.mult, ALU.mult, r=[XK[tb], "rstd", "fgb"], w=[f"ob{i}"])
                S.dma("sp", out_d[tb * 128:(tb + 1) * 128, :], ob[i], r=[f"ob{i}"], w=["out"])
        else:
            S.barrier()
            for tb in range(NB):
                S.dma("sp", out_d[tb * 128:(tb + 1) * 128, :], x_sb[:, tb, :], r=[XK[tb]], w=["out"])
        S.barrier()
        S.emit_all()
    return nc, dbg_out, S


def prep_inputs(inputs):
    f = lambda a: np.ascontiguousarray(np.asarray(a, dtype=np.float32))
    L = DEPTH
    perm = _col_perm()
    cf, cb = _host_consts()
    shared = {
        "ada_w": f(inputs["ada_w"]),
        "ada_b": f(inputs["ada_b"]),
        "w_in_p": f(np.asarray(inputs["w_in"])[:, :, perm]),
        "cmp_w1_k": f(inputs["cmp_w1_k"]), "cmp_w1_v": f(inputs["cmp_w1_v"]),
        "cmp_w2_k": f(inputs["cmp_w2_k"]), "cmp_w2_v": f(inputs["cmp_w2_v"]),
        "pekT": f(np.asarray(inputs["cmp_pos_k"]).transpose(0, 2, 1)),
        "pevT": f(np.asarray(inputs["cmp_pos_v"]).transpose(0, 2, 1)),
        "w_out": f(inputs["w_out"]),
        "wr": f(np.concatenate([np.asarray(inputs["router_group_w"]), np.asarray(inputs["router_expert_w"])], axis=2)),
        "expert_w1": f(inputs["expert_w1"]), "expert_w3": f(inputs["expert_w3"]), "expert_w2": f(inputs["expert_w2"]),
        "cf": cf, "cb": cb,
    }
    g = np.stack([np.asarray(inputs["norm1_g"]), np.asarray(inputs["norm2_g"]), np.asarray(inputs["out_norm_g"])], axis=1)
    shared["gcols"] = f(g.reshape(L, 3, KC, 128).transpose(3, 0, 1, 2).reshape(128, L * 3 * KC))
    shared["bfb"] = f(np.broadcast_to(np.asarray(inputs["b_forget"]).reshape(1, L * 4), (128, L * 4)))
    br = np.concatenate([np.asarray(inputs["router_group_b"]), np.asarray(inputs["router_expert_b"])], axis=1)
    shared["brb"] = f(np.broadcast_to(br.reshape(1, L * 36), (128, L * 36)))
    shared["fgb"] = f(np.broadcast_to(np.asarray(inputs["final_g"]).reshape(1, D), (128, D)))
    xs = np.asarray(inputs["x"], dtype=np.float32)
    cs = np.asarray(inputs["c"], dtype=np.float32)
    in_maps = []
    for b in range(8):
        m = dict(shared)
        m["x"] = f(xs[b])
        m["c_fm"] = f(cs[b].reshape(KC, 128).T)
        in_maps.append(m)
    return in_maps


def kernel(**inputs):
    in_maps = prep_inputs(inputs)
    nc, _, _ = build()
    res = run_bass_kernel_spmd(nc, in_maps, core_ids=list(range(8)))
    return np.stack([np.asarray(r["out"], dtype=np.float32) for r in res.results], axis=0)
```

```python
import math
from contextlib import ExitStack
import numpy as np
import concourse.bass as bass
import concourse.mybir as mybir
from concourse.bass_utils import run_bass_kernel_spmd

F32 = mybir.dt.float32
BF16 = mybir.dt.bfloat16
AF = mybir.ActivationFunctionType
ALU = mybir.AluOpType
AX = mybir.AxisListType

S_LEN = 2048
D = 1024
NB = 16
KC = 8
DEPTH = 4
D_IN = 2844
NEGB = -30000.0
VW = 72
EPS = 1e-6

EPOCH = 30000
NSEM_ENG = 8
DMA_POOL = {"sp": 12, "act": 4, "pool": 12}
COMPUTE = ("pe", "dve", "act", "pool")
QUEUES = ("pe", "dve", "act", "pool", "sp")


class Sched:
    def __init__(self, nc, stack):
        self.nc = nc
        self.q = {e: [] for e in QUEUES}
        self.cnt = {e: 0 for e in COMPUTE}
        self.sems = {}
        for e in COMPUTE:
            self.sems[e] = [stack.enter_context(nc.semaphore(f"s_{e}{i}")) for i in range(NSEM_ENG)]
        self.dsems = {}
        self.dcnt = {}
        for qn, n in DMA_POOL.items():
            self.dsems[qn] = [stack.enter_context(nc.semaphore(f"d_{qn}{i}")) for i in range(n)]
            self.dcnt[qn] = 0
        self.seen = {e: {} for e in QUEUES}
        self.last_w = {}
        self.readers = {}
        self.all_dma_tokens = {}
        self.n_ops = 0

    def _need(self, eng, tok, same_ok):
        sem, val, prod = tok
        if prod == eng and same_ok:
            return None
        if self.seen[eng].get(sem.name, 0) >= val:
            return None
        return tok

    def _deps(self, eng, r, w, same_raw=True):
        toks = []
        for k in r:
            t = self.last_w.get(k)
            if t is not None:
                n = self._need(eng, t, same_ok=(not same_raw))
                if n:
                    toks.append(n)
            if isinstance(k, str) and k.startswith("bk"):
                for t in self.readers.get(k, {}).values():
                    n = self._need(eng, t, same_ok=True)
                    if n:
                        toks.append(n)
        for k in w:
            t = self.last_w.get(k)
            if t is not None:
                n = self._need(eng, t, same_ok=True)
                if n:
                    toks.append(n)
            for t in self.readers.get(k, {}).values():
                n = self._need(eng, t, same_ok=True)
                if n:
                    toks.append(n)
        best = {}
        for sem, val, prod in toks:
            if sem.name not in best or best[sem.name][1] < val:
                best[sem.name] = (sem, val, prod)
        return list(best.values())

    def _record(self, eng, tok, r, w):
        for k in w:
            self.last_w[k] = tok
            self.readers[k] = {}
        for k in r:
            if k in w:
                continue
            rk = eng if tok[2] in COMPUTE else tok[0].name
            self.readers.setdefault(k, {})[rk] = tok

    def op(self, eng, fn, r=(), w=(), same_raw=True):
        waits = self._deps(eng, r, w, same_raw)
        g = self.cnt[eng]
        self.cnt[eng] = g + 1
        sem = self.sems[eng][g // EPOCH]
        val = g % EPOCH + 1
        tok = (sem, val, eng)
        for (s, v, p) in waits:
            self.seen[eng][s.name] = max(self.seen[eng].get(s.name, 0), v)
        self.n_ops += 1

        def emit(e, waits=waits, fn=fn, sem=sem):
            for (s, v, p) in waits:
                e.wait_ge(s, v)
            fn(e).then_inc(sem, 1)
        self.q[eng].append(emit)
        self._record(eng, tok, r, w)
        return tok

    def dma(self, qn, out, in_, r=(), w=(), **kw):
        n = self.dcnt[qn]
        self.dcnt[qn] = n + 1
        pool = self.dsems[qn]
        sem = pool[n % len(pool)]
        val = 16 * (n // len(pool) + 1)
        waits = self._deps(qn, r, w, same_raw=True)
        if val > 16 and self.seen[qn].get(sem.name, 0) < val - 16:
            waits = [t for t in waits if t[0].name != sem.name] + [(sem, val - 16, "dma")]
        for (s, v, p) in waits:
            self.seen[qn][s.name] = max(self.seen[qn].get(s.name, 0), v)
        tok = (sem, val, "dma")
        self.n_ops += 1

        def emit(e, waits=waits, sem=sem, out=out, in_=in_, kw=kw):
            for (s, v, p) in waits:
                e.wait_ge(s, v)
            e.dma_start(out=out, in_=in_, **kw).then_inc(sem, 16)
        self.q[qn].append(emit)
        self._record(qn, tok, r, w)
        self.all_dma_tokens[sem.name] = (sem, val, "dma")
        return tok

    def barrier(self):
        toks = []
        for e in COMPUTE:
            g = self.cnt[e]
            if g > 0:
                toks.append((self.sems[e][(g - 1) // EPOCH], (g - 1) % EPOCH + 1, e))
        toks += list(self.all_dma_tokens.values())
        for e in QUEUES:
            ws = [t for t in toks if t[2] != e and self.seen[e].get(t[0].name, 0) < t[1]]
            for (s, v, p) in ws:
                self.seen[e][s.name] = v

            def emit(eo, ws=ws):
                for (s, v, p) in ws:
                    eo.wait_ge(s, v)
            self.q[e].append(emit)
        self.last_w = {}
        self.readers = {}

    def emit_all(self):
        nc = self.nc
        with nc.Block() as block:
            @block.tensor
            def _(e):
                for f in self.q["pe"]:
                    f(e)

            @block.vector
            def _(e):
                for f in self.q["dve"]:
                    f(e)

            @block.scalar
            def _(e):
                for f in self.q["act"]:
                    f(e)

            @block.gpsimd
            def _(e):
                for f in self.q["pool"]:
                    f(e)

            @block.sync
            def _(e):
                for f in self.q["sp"]:
                    f(e)


class Arena:
    def __init__(self, t, nbytes):
        self.t = t
        self.n = nbytes
        self.off = 0

    def reset(self):
        self.off = 0

    def alloc(self, free_shape, dt):
        esz = 4 if dt == F32 else 2
        n_el = int(np.prod(free_shape))
        size = n_el * esz
        off = (self.off + 63) // 64 * 64
        assert off + size <= self.n, f"arena overflow {off + size} > {self.n}"
        self.off = off + size
        v = self.t[:, off // 4:(off + size) // 4]
        if dt != F32:
            v = v.bitcast(dt)
        if len(free_shape) == 2:
            v = v.rearrange("p (a b) -> p a b", a=free_shape[0])
        elif len(free_shape) == 3:
            v = v.rearrange("p (a b c) -> p a b c", a=free_shape[0], b=free_shape[1])
        elif len(free_shape) == 4:
            v = v.rearrange("p (a b c d) -> p a b c d", a=free_shape[0], b=free_shape[1], c=free_shape[2])
        return v


def _col_perm():
    o = {}
    acc = 0
    sizes = [("qa", 512), ("kca", 128), ("vca", 128), ("ksa", 128), ("vsa", 128), ("kwa", 128), ("vwa", 128),
             ("ga", 24), ("qb", 256), ("kb", 256), ("vb", 256), ("fb", 4), ("qc", 256), ("kcc", 256), ("vcc", 256)]
    for n, s in sizes:
        o[n] = acc
        acc += s
    assert acc == D_IN
    perm = []
    for j in range(4):
        perm += list(range(o["qa"] + j * 64, o["qa"] + j * 64 + 64))
        perm += list(range(o["qa"] + (4 + j) * 64, o["qa"] + (4 + j) * 64 + 64))
    perm += list(range(o["ksa"], o["ksa"] + 128)) + list(range(o["kwa"], o["kwa"] + 128))
    perm += list(range(o["vsa"], o["vsa"] + 128)) + list(range(o["vwa"], o["vwa"] + 128))
    perm += list(range(o["kca"], o["kca"] + 128)) + list(range(o["vca"], o["vca"] + 128))
    perm += list(range(o["qb"], o["qb"] + 256)) + list(range(o["kb"], o["kb"] + 256))
    perm += list(range(o["qc"], o["qc"] + 256)) + list(range(o["kcc"], o["kcc"] + 256))
    perm += list(range(o["vb"], o["vb"] + 256)) + list(range(o["vcc"], o["vcc"] + 256))
    perm += list(range(o["fb"], o["fb"] + 4)) + list(range(o["ga"], o["ga"] + 24))
    assert len(perm) == D_IN and len(set(perm)) == D_IN
    return np.array(perm)


PIECES = [(0, 512), (512, 512), (1024, 256), (1280, 512), (1792, 512), (2304, 512), (2816, 28)]

CF = {}
_o = 0
for _n, _w in [("identf", 128), ("onesf", 128), ("triincl", 128), ("cos", 128), ("sin", 128), ("cosc", 8), ("sinc", 8),
               ("A", 512), ("Bc", 512)]:
    CF[_n] = (_o, _w)
    _o += _w
NCF = _o
CB = {}
_o = 0
for _n, _w in [("ident", 128), ("ones", 128), ("tri", 128), ("antitri", 128), ("strictpos", 128), ("trige", 128),
               ("cmpmask", 2048), ("eblk", 2048), ("cover1", 36)]:
    CB[_n] = (_o, _w)
    _o += _w
NCB = _o


def _host_consts():
    p = np.arange(128)
    cf = np.zeros((128, NCF), np.float32)
    cb = np.zeros((128, NCB), np.float32)

    def setf(n, a):
        o, w = CF[n]
        cf[:, o:o + w] = a.reshape(128, w)

    def setb(n, a):
        o, w = CB[n]
        cb[:, o:o + w] = a.reshape(128, w)
    eye = np.eye(128, dtype=np.float32)
    setf("identf", eye)
    setf("onesf", np.ones((128, 128), np.float32))
    setf("triincl", (p[:, None] <= p[None, :]).astype(np.float32))
    half = 8
    inv = np.exp(np.arange(half, dtype=np.float32) * np.float32(-2.0 * math.log(500000.0) / 16)).astype(np.float32)
    pos = (np.arange(NB)[None, :] * 128 + p[:, None]).astype(np.float32)
    ang = pos[:, :, None] * inv[None, None, :]
    setf("cos", np.cos(ang).astype(np.float32))
    setf("sin", np.sin(ang).astype(np.float32))
    posc = (16 * p + 31).astype(np.float32)
    angc = posc[:, None] * inv[None, :]
    setf("cosc", np.cos(angc).astype(np.float32))
    setf("sinc", np.sin(angc).astype(np.float32))
    q = np.arange(NB)[None, :] * 128 + p[:, None]
    cur = q // 64
    j = np.arange(32)[None, None, :]
    A = np.zeros((128, NB, 32), np.float32)
    Bc = np.zeros((128, NB, 32), np.float32)
    causal = (64 * j <= q[:, :, None])
    A[causal] = 1.0
    Bc[~causal] = -1.0
    f0 = (j == 0) & np.ones_like(causal)
    f1 = (j == cur[:, :, None])
    f2 = (j == cur[:, :, None] - 1)
    for fm, val in ((f0, 1.0e4), (f2, 1.0e4 + 64.0), (f1, 1.0e4 + 128.0)):
        A[fm] = 0.0
        Bc[fm] = val
    setf("A", A)
    setf("Bc", Bc)
    setb("ident", eye)
    setb("ones", np.ones((128, 128), np.float32))
    kk = p[:, None]
    qq = p[None, :]
    setb("tri", np.where(kk > qq, NEGB, 0.0).astype(np.float32))
    setb("antitri", np.where(kk <= qq, NEGB, 0.0).astype(np.float32))
    setb("strictpos", np.where(kk >= qq, -NEGB, 0.0).astype(np.float32))
    setb("trige", (p[:, None] >= p[None, :]).astype(np.float32))
    n = p[:, None, None]
    qabs = np.arange(NB)[None, :, None] * 128 + p[None, None, :]
    setb("cmpmask", np.where((16 * n + 31 > qabs) | (n >= 127), NEGB, 0.0).astype(np.float32))
    kb = np.arange(NB)[None, :, None]
    kp = p[None, None, :]
    jj = p[:, None, None]
    setb("eblk", ((jj == 2 * kb + kp // 64) & (jj < 32)).astype(np.float32))
    cover = np.zeros((128, 36), np.float32)
    nn = np.arange(127)[:, None]
    js = np.arange(32)[None, :]
    cover[:127, :32] = ((16 * nn < 64 * js + 64) & (16 * nn + 32 > 64 * js)).astype(np.float32)
    cover[:127, 32] = 1.0
    setb("cover1", cover)
    return cf, cb


def build(depth=DEPTH, upto="all", dbg=(), lite=False):
    nc = bass.Bass("TRN2", target_bir_lowering=False)
    dram = {}

    def din(name, shape, dt=F32):
        dram[name] = nc.dram_tensor(name, list(shape), dt, kind="ExternalInput").ap()
        return dram[name]

    NL = 1 if lite else DEPTH
    x_d = din("x", [S_LEN, D])
    c_d = din("c_fm", [128, KC])
    adaw_d = din("ada_w", [NL, D, 6 * D])
    adab_d = din("ada_b", [NL, 6 * D])
    gcol_d = din("gcols", [128, DEPTH * 3 * KC])
    win_d = din("w_in_p", [NL, D, D_IN])
    bfb_d = din("bfb", [128, DEPTH * 4])
    w1k_d = din("cmp_w1_k", [NL, 2048, 128])
    w1v_d = din("cmp_w1_v", [NL, 2048, 128])
    w2k_d = din("cmp_w2_k", [NL, 128, 64])
    w2v_d = din("cmp_w2_v", [NL, 128, 64])
    pek_d = din("pekT", [NL, 64, 32])
    pev_d = din("pevT", [NL, 64, 32])
    wout_d = din("w_out", [NL, D, D])
    wr_d = din("wr", [NL, D, 36])
    br_d = din("brb", [128, DEPTH * 36])
    ne_l, ne_e = (1, 1) if lite else (DEPTH, 32)
    ew1_d = din("expert_w1", [ne_l, ne_e, D, 512])
    ew3_d = din("expert_w3", [ne_l, ne_e, D, 512])
    ew2_d = din("expert_w2", [ne_l, ne_e, 512, D])
    fg_d = din("fgb", [128, D])
    cf_d = din("cf", [128, NCF])
    cb_d = din("cb", [128, NCB])
    out_d = nc.dram_tensor("out", [S_LEN, D], F32, kind="ExternalOutput").ap()
    ut_d = nc.dram_tensor("ut_scr", [16, 128, S_LEN], BF16, kind="Internal").ap()
    v_d = nc.dram_tensor("v_scr", [3, 128, NB * 4 * VW], BF16, kind="Internal").ap()
    dbg_out = {}

    with ExitStack() as st:
        S = Sched(nc, st)

        def sb(name, shape, dt):
            return st.enter_context(nc.sbuf_tensor(name, list(shape), dt))
        banks = [st.enter_context(nc.psum_tensor(f"bank{i}", [128, 512], F32)) for i in range(8)]
        BK = [f"bk{i}" for i in range(8)]

        x_sb = sb("x_sb", [128, NB, D], F32)
        hT = sb("hT", [128, KC, S_LEN], BF16)
        cf = sb("cf_sb", [128, NCF], F32)
        cbt = sb("cb_sb", [128, NCB], BF16)
        g1b = sb("g1b", [128, D], F32)
        g2b = sb("g2b", [128, D], F32)
        modT = sb("modT", [128, 48], F32)
        AB = sb("AB", [128, 4, KC], F32)
        gcol = sb("gcol", [128, DEPTH * 3 * KC], F32)
        bfb = sb("bfb_sb", [128, DEPTH * 4], F32)
        brb = sb("brb_sb", [128, DEPTH * 36], F32)
        cond_bf = sb("cond_bf", [128, KC], BF16)
        small = sb("small", [128, 256], F32)
        ssq = sb("ssq", [128, NB], F32)
        rstd = sb("rstd", [128, NB], F32)
        ones1 = sb("ones1", [1, 128], F32)
        one11 = sb("one11", [1, 1], F32)
        ARENA_BYTES = 84 * 1024
        arena_t = sb("arena", [128, ARENA_BYTES // 4], F32)
        AR = Arena(arena_t, ARENA_BYTES)

        def cfv(n):
            o, w = CF[n]
            return cf[:, o:o + w]

        def cbv(n):
            o, w = CB[n]
            return cbt[:, o:o + w]
        ident = cbv("ident")
        ones_bf = cbv("ones")
        tri_b = cbv("tri")
        antitri_b = cbv("antitri")
        strictpos_b = cbv("strictpos")
        trige_b = cbv("trige")
        cmpmask = cbv("cmpmask").rearrange("p (a b) -> p a b", a=NB)
        eblk = cbv("eblk").rearrange("p (a b) -> p a b", a=NB)
        cover1 = cbv("cover1")
        identf = cfv("identf")
        onesf = cfv("onesf")
        triincl = cfv("triincl")
        cos_t = cfv("cos").rearrange("p (a b) -> p a b", a=NB)
        sin_t = cfv("sin").rearrange("p (a b) -> p a b", a=NB)
        cosc = cfv("cosc")
        sinc = cfv("sinc")
        A_t = cfv("A").rearrange("p (a b) -> p a b", a=NB)
        Bc_t = cfv("Bc").rearrange("p (a b) -> p a b", a=NB)

        def mm(out, lhsT, rhs, start, stop, r, w, sgc=False):
            S.op("pe", lambda e: e.matmul(out, lhsT=lhsT, rhs=rhs, start=start, stop=stop, skip_group_check=sgc),
                 r=r, w=w, same_raw=False)

        def tp(out, in_, r, w, idn=None):
            idn_ = ident if idn is None else idn
            S.op("pe", lambda e: e.transpose(out=out, in_=in_, identity=idn_), r=r, w=w, same_raw=False)

        def act(out, in_, func, r, w, bias=None, scale=None, accum=None):
            kw = {}
            if bias is not None:
                kw["bias"] = bias
            if scale is not None:
                kw["scale"] = scale
            if accum is not None:
                kw["accum_out"] = accum
            S.op("act", lambda e: e.activation(out=out, in_=in_, func=func, **kw), r=r, w=w)

        def tt(eng, out, in0, in1, op, r, w):
            S.op(eng, lambda e: e.tensor_tensor(out=out, in0=in0, in1=in1, op=op), r=r, w=w)

        def ts(eng, out, in0, s1, op0, r, w, s2=None, op1=None):
            if op1 is None:
                S.op(eng, lambda e: e.tensor_scalar(out=out, in0=in0, scalar1=s1, scalar2=None, op0=op0), r=r, w=w)
            else:
                S.op(eng, lambda e: e.tensor_scalar(out=out, in0=in0, scalar1=s1, scalar2=s2, op0=op0, op1=op1), r=r, w=w)

        def stt(eng, out, in0, scalar, in1, op0, op1, r, w):
            S.op(eng, lambda e: e.scalar_tensor_tensor(out=out, in0=in0, scalar=scalar, in1=in1, op0=op0, op1=op1), r=r, w=w)

        def cp(eng, out, in_, r, w):
            if eng == "act":
                S.op("act", lambda e: e.copy(out=out, in_=in_), r=r, w=w)
            else:
                S.op(eng, lambda e: e.tensor_copy(out=out, in_=in_), r=r, w=w)

        def recip(out, in_, r, w):
            S.op("dve", lambda e: e.reciprocal(out=out, in_=in_), r=r, w=w)

        def memset(eng, ap, val, w):
            S.op(eng, lambda e: e.memset(ap, val), w=w)

        def dump(name, ap, shape, r, dt=F32):
            if name not in dbg:
                return
            if not isinstance(ap, bass.AP):
                ap = ap[:]
            d = nc.dram_tensor("dbg_" + name, list(shape), dt, kind="ExternalOutput").ap()
            dbg_out[name] = d
            S.dma("sp", d, ap, r=r)

        XK = [f"x{tb}" for tb in range(NB)]
        HK = [f"hT{tb}" for tb in range(NB)]

        S.dma("sp", cf[:], cf_d, w=["cf"])
        S.dma("pool", cbt[:], cb_d, w=["cb"])
        S.dma("sp", gcol[:], gcol_d, w=["gcol"])
        S.dma("sp", bfb[:], bfb_d, w=["bfb"])
        S.dma("sp", brb[:], br_d, w=["brb"])
        memset("pool", ones1[:], 1.0, ["ones1"])
        memset("pool", one11[:], 1.0, ["one11"])
        for tb in range(NB):
            S.dma("sp" if tb % 2 == 0 else "act", x_sb[:, tb, :], x_d[tb * 128:(tb + 1) * 128, :], w=[XK[tb]])
        ctmp = small[:, 0:8]
        ctmp2 = small[:, 8:16]
        S.dma("sp", ctmp, c_d, w=["ctmp"])
        act(ctmp2, ctmp, AF.Exp, r=["ctmp"], w=["ctmp2"], scale=-1.0)
        ts("dve", ctmp2, ctmp2, 1.0, ALU.add, r=["ctmp2"], w=["ctmp2"])
        recip(ctmp2, ctmp2, r=["ctmp2"], w=["ctmp2"])
        tt("dve", cond_bf[:], ctmp, ctmp2, ALU.mult, r=["ctmp", "ctmp2"], w=["cond"])

        def layer_mod(l):
            AR.reset()
            adab = [AR.alloc([KC, 512], BF16) for _ in range(2)]
            brow = [AR.alloc([512], F32) for _ in range(2)]
            rowp = [AR.alloc([512], F32) for _ in range(2)]
            for j in range(12):
                i = j % 2
                S.dma("pool", adab[i], adaw_d[l, :, j * 512:(j + 1) * 512].rearrange("(kc p) n -> p kc n", p=128),
                      w=[f"adab{i}"])
                S.dma("sp", brow[i][0:1, :], adab_d[l:l + 1, j * 512:(j + 1) * 512], w=[f"brow{i}"])
                bk = 0 + i
                for kc in range(KC):
                    mm(banks[bk][0:1, :], cond_bf[:, kc:kc + 1], adab[i][:, kc, :], kc == 0, kc == KC - 1,
                       r=["cond", f"adab{i}"], w=[BK[bk]])
                tt("dve", rowp[i][0:1, :], banks[bk][0:1, :], brow[i][0:1, :], ALU.add, r=[BK[bk], f"brow{i}"], w=[f"rowp{i}"])
                if j in (4, 5, 10, 11):
                    dst = g1b if j < 6 else g2b
                    off = (j % 2) * 512
                    mm(banks[2 + i][:, :], ones1[0:1, :], rowp[i][0:1, :], True, True, r=["ones1", f"rowp{i}"], w=[BK[2 + i]])
                    cp("act", dst[:, off:off + 512], banks[2 + i][:, :], r=[BK[2 + i]], w=["g1b" if j < 6 else "g2b"])
                for ii in range(4):
                    cidx = j * 4 + ii
                    mm(banks[4][:, cidx:cidx + 1], rowp[i][0:1, ii * 128:(ii + 1) * 128], one11[0:1, 0:1], True, True,
                       r=[f"rowp{i}", "one11"], w=[BK[4]])
            cp("dve", modT[:], banks[4][:, 0:48], r=[BK[4]], w=["modT"])
            g1c = gcol[:, (l * 3 + 0) * KC:(l * 3 + 1) * KC]
            g2c = gcol[:, (l * 3 + 1) * KC:(l * 3 + 2) * KC]
            stt("dve", AB[:, 0, :], modT[:, 8:16], 1.0, g1c, ALU.add, ALU.mult, r=["modT", "gcol"], w=["AB"])
            cp("dve", AB[:, 1, :], modT[:, 0:8], r=["modT"], w=["AB"])
            stt("dve", AB[:, 2, :], modT[:, 32:40], 1.0, g2c, ALU.add, ALU.mult, r=["modT", "gcol"], w=["AB"])
            cp("dve", AB[:, 3, :], modT[:, 24:32], r=["modT"], w=["AB"])

        def norm_to_hT(which):
            junk = AR.alloc([D], BF16)
            xn = [AR.alloc([D], BF16) for _ in range(2)]
            for tb in range(NB):
                act(junk, x_sb[:, tb, :], AF.Square, r=[XK[tb]], w=["junk", "ssq"], accum=ssq[:, tb:tb + 1])
            act(rstd[:], ssq[:], AF.Ln, r=["ssq"], w=["rstd"], scale=1.0 / D, bias=EPS)
            act(rstd[:], rstd[:], AF.Exp, r=["rstd"], w=["rstd"], scale=-0.5)
            for tb in range(NB):
                i = tb % 2
                ts("dve", xn[i], x_sb[:, tb, :], rstd[:, tb:tb + 1], ALU.mult, r=[XK[tb], "rstd"], w=[f"xn{i}"])
                bk = 6 + i
                bb = banks[bk][:].bitcast(BF16)
                for kc in range(KC):
                    tp(bb[:, kc * 128:(kc + 1) * 128], xn[i][:, kc * 128:(kc + 1) * 128], r=[f"xn{i}", "cb"], w=[BK[bk]])
                for kc in range(KC):
                    act(hT[:, kc, tb * 128:(tb + 1) * 128], bb[:, kc * 128:(kc + 1) * 128], AF.Identity,
                        r=[BK[bk], "AB"], w=[HK[tb]], scale=AB[:, 2 * which, kc:kc + 1], bias=AB[:, 2 * which + 1, kc:kc + 1])

        def in_proj(l, st_):
            wbuf = [AR.alloc([KC, 512], BF16) for _ in range(2)]
            ut32 = [AR.alloc([512], F32) for _ in range(2)]
            ub = [AR.alloc([512], BF16) for _ in range(2)]
            rtmp = [AR.alloc([4, 12, 8], F32) for _ in range(2)]
            stage = [AR.alloc([4, 512], BF16) for _ in range(2)]
            fmst = [AR.alloc([512], BF16) for _ in range(2)]
            vst = [AR.alloc([4, VW], BF16) for _ in range(4)]
            for i in range(4):
                memset("pool", vst[i], 1.0, [f"vst{i}"])
            fraw, sig = st_["fraw"], st_["sig"]
            wcnt = [0]

            def load_w(pi):
                c0, w = PIECES[pi]
                i = wcnt[0] % 2
                wcnt[0] += 1
                S.dma("pool", wbuf[i][:, :, 0:w], win_d[l, :, c0:c0 + w].rearrange("(kc p) n -> p kc n", p=128),
                      w=[f"wbuf{i}"])
                return i

            def rope(src32, dstb, nheads, tb, ri):
                sv = src32.rearrange("p (h d) -> p h d", h=nheads)
                dv = dstb.rearrange("p (h d) -> p h d", h=nheads)
                x1 = sv[:, :, 0:8]
                x2 = sv[:, :, 8:16]
                cs = cos_t[:, tb, :].unsqueeze(1).to_broadcast([128, nheads, 8])
                sn = sin_t[:, tb, :].unsqueeze(1).to_broadcast([128, nheads, 8])
                t = rtmp[ri]
                k = [f"rtmp{ri}"]
                rsk = []
                cp("act", dstb, src32, r=[f"ut32{ri}"], w=[f"ub{ri}"])
                if "0" not in rsk:
                    tt("dve", t[:, 0, 0:nheads, :], x1, cs, ALU.mult, r=[f"ut32{ri}", "cf"], w=k)
                    tt("dve", t[:, 1, 0:nheads, :], x2, sn, ALU.mult, r=[f"ut32{ri}", "cf"], w=k)
                    tt("dve", t[:, 2, 0:nheads, :], x2, cs, ALU.mult, r=[f"ut32{ri}", "cf"], w=k)
                    tt("dve", t[:, 3, 0:nheads, :], x1, sn, ALU.mult, r=[f"ut32{ri}", "cf"], w=k)
                if "1" not in rsk:
                    tt("dve", dv[:, :, 0:8], t[:, 0, 0:nheads, :], t[:, 1, 0:nheads, :], ALU.subtract, r=k, w=[f"ub{ri}"])
                    tt("dve", dv[:, :, 8:16], t[:, 2, 0:nheads, :], t[:, 3, 0:nheads, :], ALU.add, r=k, w=[f"ub{ri}"])

            def tm_piece(pi, handler):
                wi = load_w(pi)
                c0, w = PIECES[pi]
                for tb in range(NB):
                    bk = tb % 2
                    for kc in range(KC):
                        mm(banks[bk][:, 0:w], hT[:, kc, tb * 128:(tb + 1) * 128], wbuf[wi][:, kc, 0:w], kc == 0, kc == KC - 1,
                           r=[HK[tb], f"wbuf{wi}"], w=[BK[bk]])
                    handler(tb, bk)

            def fm_piece(pi, slot0, scales):
                wi = load_w(pi)
                c0, w = PIECES[pi]
                cnt = 0
                for m in range(w // 128):
                    for t4 in range(4):
                        bk = cnt % 2
                        si = cnt % 2
                        cnt += 1
                        for kc in range(KC):
                            mm(banks[bk][:, :], wbuf[wi][:, kc, m * 128:(m + 1) * 128], hT[:, kc, t4 * 512:(t4 + 1) * 512],
                               kc == 0, kc == KC - 1, r=HK[t4 * 4:t4 * 4 + 4] + [f"wbuf{wi}"], w=[BK[bk]])
                        if scales[m] == 1.0:
                            cp("act", fmst[si], banks[bk][:, :], r=[BK[bk]], w=[f"fmst{si}"])
                        else:
                            S.op("act", lambda e, si=si, bk=bk, sc=scales[m]: e.mul(out=fmst[si], in_=banks[bk][:, :], mul=sc),
                                 r=[BK[bk]], w=[f"fmst{si}"])
                        S.dma("sp", ut_d[slot0 + m, :, t4 * 512:(t4 + 1) * 512], fmst[si], r=[f"fmst{si}"], w=[f"ut{slot0 + m}"])

            def h_p1(tb, bk):
                ri = tb % 2
                S.op("act", lambda e: e.mul(out=ut32[ri], in_=banks[bk][:, :], mul=0.125), r=[BK[bk]], w=[f"ut32{ri}"])
                rope(ut32[ri], ub[ri], 8, tb, ri)
                tbk = 2 + ri
                bb = banks[tbk][:].bitcast(BF16)
                for j in range(4):
                    tp(bb[:, j * 128:(j + 1) * 128], ub[ri][:, j * 128:(j + 1) * 128], r=[f"ub{ri}", "cb"], w=[BK[tbk]])
                sgi = (tb // 4) % 2
                cp("act", stage[sgi][:, :, (tb % 4) * 128:(tb % 4 + 1) * 128],
                   bb[:, 0:512].rearrange("p (s t) -> p s t", s=4), r=[BK[tbk]], w=[f"stage{sgi}"])
                if tb % 4 == 3:
                    t4 = tb // 4
                    S.dma("sp", ut_d[0:4, :, t4 * 512:(t4 + 1) * 512].rearrange("s p t -> p s t"), stage[sgi],
                          r=[f"stage{sgi}"], w=["ut0", "ut1", "ut2", "ut3"])
            ipn = int(upto[6:]) if (upto.startswith("inproj") and len(upto) > 6) else 99
            tm_piece(0, h_p1)
            if ipn <= 1:
                return

            import os as _os
            _skip = _os.environ.get("P2SKIP", "").split(",")

            def h_p2(tb, bk):
                ri = tb % 2
                cp("act", ut32[ri][:, 0:256], banks[bk][:, 0:256], r=[BK[bk]], w=[f"ut32{ri}"])
                vi = tb % 4
                if "vcopy" not in _skip:
                    cp("dve", vst[vi][:, :, 0:64], banks[bk][:, 256:512].rearrange("p (h d) -> p h d", h=4), r=[BK[bk]], w=[f"vst{vi}"])
                if "vdma" not in _skip:
                    S.dma("sp", v_d[0, :, tb * 4 * VW:(tb + 1) * 4 * VW], vst[vi].rearrange("p h d -> p (h d)"), r=[f"vst{vi}"], w=["v0"])
                if "rope" not in _skip:
                    rope(ut32[ri][:, 0:256], ub[ri][:, 0:256], 4, tb, ri)
                else:
                    cp("dve", ub[ri][:, 0:256], ut32[ri][:, 0:256], r=[f"ut32{ri}"], w=[f"ub{ri}"])
                if "tp" in _skip:
                    return
                tbk = 2 + ri
                bb = banks[tbk][:].bitcast(BF16)
                for j in range(2):
                    tp(bb[:, j * 128:(j + 1) * 128], ub[ri][:, j * 128:(j + 1) * 128], r=[f"ub{ri}", "cb"], w=[BK[tbk]])
                sgi = (tb // 4) % 2
                cp("act", stage[sgi][:, 0:2, (tb % 4) * 128:(tb % 4 + 1) * 128],
                   bb[:, 0:256].rearrange("p (s t) -> p s t", s=2), r=[BK[tbk]], w=[f"stage{sgi}"])
                if tb % 4 == 3 and "sdma" not in _skip:
                    t4 = tb // 4
                    S.dma("sp", ut_d[4:6, :, t4 * 512:(t4 + 1) * 512].rearrange("s p t -> p s t"), stage[sgi][:, 0:2, :],
                          r=[f"stage{sgi}"], w=["ut4", "ut5"])
            tm_piece(1, h_p2)
            if ipn <= 2:
                return

            fm_piece(2, 6, [1.0, 1.0])
            if ipn <= 3:
                return
            fm_piece(3, 8, [0.125, 0.125, 1.0, 1.0])
            fm_piece(4, 12, [-0.125, -0.125, 1.0, 1.0])
            if ipn <= 5:
                return

            def h_p6(tb, bk):
                for gi in range(2):
                    vi = (2 * tb + gi) % 4
                    cp("dve" if gi == 0 else "act", vst[vi][:, :, 0:64],
                       banks[bk][:, gi * 256:(gi + 1) * 256].rearrange("p (h d) -> p h d", h=4), r=[BK[bk]], w=[f"vst{vi}"])
                    S.dma("sp", v_d[1 + gi, :, tb * 4 * VW:(tb + 1) * 4 * VW], vst[vi].rearrange("p h d -> p (h d)"),
                          r=[f"vst{vi}"], w=[f"v{1 + gi}"])
            tm_piece(5, h_p6)
            if ipn <= 6:
                return

            def h_p7(tb, bk):
                cp("dve", fraw[:, tb, :], banks[bk][:, 0:4], r=[BK[bk]], w=["fraw"])
                cp("dve", sig[:, tb, :], banks[bk][:, 4:28], r=[BK[bk]], w=["sig"])
            tm_piece(6, h_p7)
            act(sig[:], sig[:], AF.Exp, r=["sig"], w=["sig"], scale=-1.0)
            ts("dve", sig[:], sig[:], 1.0, ALU.add, r=["sig"], w=["sig"])
            recip(sig[:], sig[:], r=["sig"], w=["sig"])

        def load_wout(l, wo, stg, kc0, nkc):
            for k in range(nkc):
                kc = kc0 + k
                S.dma("act", stg, wout_d[l, kc * 128:(kc + 1) * 128, :], w=["wostg"])
                ong = gcol[:, (l * 3 + 2) * KC + kc:(l * 3 + 2) * KC + kc + 1]
                stt("dve", wo[:, k, :], stg, ong, g1b[:], ALU.mult, ALU.mult, r=["wostg", "gcol", "g1b"], w=["wo"])

        def out_proj_block(tb, otile_ap, nkc, wo, otk, res):
            i = res["cnt"] % 2
            res["cnt"] += 1
            oT = res["oT"][i]
            tbk = 5
            bb = banks[tbk][:].bitcast(BF16)
            for k in range(nkc):
                tp(bb[:, k * 128:(k + 1) * 128], otile_ap[:, k * 128:(k + 1) * 128], r=[otk, "cb"], w=[BK[tbk]])
            cp("act", oT[:, 0:nkc, :], bb[:, 0:nkc * 128].rearrange("p (k t) -> p k t", k=nkc), r=[BK[tbk]], w=[f"oT{i}"])
            for half in range(2):
                bk = 6 + half
                for k in range(nkc):
                    mm(banks[bk][:, :], oT[:, k, :], wo[:, k, half * 512:(half + 1) * 512], k == 0, k == nkc - 1,
                       r=[f"oT{i}", "wo"], w=[BK[bk]])
                tt("dve", x_sb[:, tb, half * 512:(half + 1) * 512], x_sb[:, tb, half * 512:(half + 1) * 512], banks[bk][:, :],
                   ALU.add, r=[BK[bk], XK[tb]], w=[XK[tb]])

        def head_norm_store(src_ap, nblk, width, dst_fn, rkeys, wkey, scl):
            jk = scl["junk64"]
            sq = scl["sq"]
            for b in range(nblk):
                act(jk, src_ap(b), AF.Square, r=rkeys, w=["junk64", "hn_sq"], accum=sq[:, b:b + 1])
            act(sq[:, nblk:2 * nblk], sq[:, 0:nblk], AF.Ln, r=["hn_sq"], w=["hn_sq"], scale=1.0 / 64, bias=EPS)
            act(sq[:, 2 * nblk:3 * nblk], sq[:, nblk:2 * nblk], AF.Exp, r=["hn_sq"], w=["hn_sq"], scale=-0.5)
            for b in range(nblk):
                ts("dve", dst_fn(b), src_ap(b), sq[:, 2 * nblk + b:2 * nblk + b + 1], ALU.mult, r=rkeys + ["hn_sq"], w=[wkey])

        def fox_attention(l, st_, wo, res):
            qk = st_["qk"]
            V = st_["V"]
            fraw = st_["fraw"]
            PT = [AR.alloc([512], BF16) for _ in range(2)]
            otile = [AR.alloc([4, 256], BF16) for _ in range(2)]
            cpos = AR.alloc([NB, 4], F32)
            cend = AR.alloc([4, NB], F32)
            FB = AR.alloc([4, NB, NB], F32)
            spf = AR.alloc([NB, 4], F32)
            scl = {"junk64": AR.alloc([64], F32), "sq": AR.alloc([16], F32)}
            for s_ in range(4):
                S.dma("sp", qk[:, s_, :], ut_d[8 + s_, :, :], r=[f"ut{8 + s_}"], w=["qk"])
            S.dma("act", V.rearrange("p a h d -> p (a h d)"), v_d[1, :, :], r=["v1"], w=["V"])
            bf_ = bfb[:, l * 4:(l + 1) * 4]
            tt("dve", spf[:], fraw[:], bf_.unsqueeze(1).to_broadcast([128, NB, 4]), ALU.add, r=["fraw", "bfb"], w=["spf"])
            act(spf[:], spf[:], AF.Exp, r=["spf"], w=["spf"], scale=-1.0)
            act(spf[:], spf[:], AF.Ln, r=["spf"], w=["spf"], bias=1.0)
            spf2 = spf.rearrange("p a h -> p (a h)")
            S.op("pe", lambda e: e.matmul(banks[4][:, 0:64], lhsT=triincl, rhs=spf2, start=True, stop=True),
                 r=["spf", "cf"], w=[BK[4]], same_raw=False)
            S.op("pe", lambda e: e.matmul(banks[4][:, 64:128], lhsT=onesf, rhs=spf2, start=True, stop=True),
                 r=["spf", "cf"], w=[BK[4]], same_raw=False)
            tot = banks[4][:, 64:128].rearrange("p (a h) -> p a h", a=NB)
            cp("dve", cend[:, :, 0], tot[:, 0, :], r=[BK[4]], w=["cend"])
            for tb in range(1, NB):
                tt("dve", cend[:, :, tb], cend[:, :, tb - 1], tot[:, tb, :], ALU.add, r=[BK[4], "cend"], w=["cend"])
            win_ = banks[4][:, 0:64].rearrange("p (a h) -> p a h", a=NB)
            cp("dve", cpos[:, 0, :], win_[:, 0, :], r=[BK[4]], w=["cpos"])
            for tb in range(1, NB):
                tt("dve", cpos[:, tb, :], win_[:, tb, :], cend[:, :, tb - 1], ALU.add, r=[BK[4], "cend"], w=["cpos"])
            for h in range(4):
                for kb in range(NB):
                    ts("dve", FB[:, h, kb, :], cend[:, h, :], cpos[:, kb, h:h + 1], ALU.subtract, r=["cend", "cpos"], w=["FB"],
                       s2=-1.0, op1=ALU.mult)
            dump("cpos", cpos, [128, NB, 4], r=["cpos"])
            it = 0
            for Q in range(4):
                oi = Q % 2
                for h in range(4):
                    hp = (h % 2) * 64
                    sq_, sk_ = h // 2, 2 + h // 2
                    obk = 2 + (it % 2)
                    nkb = 4 * Q + 4
                    steps = []
                    for kb in range(nkb):
                        c0b = max(0, kb - 4 * Q)
                        steps.append((kb, c0b, c0b * 128, 512 - c0b * 128, it % 2, kb >= 4 * Q))
                        it += 1

                    def stA(p):
                        kb, c0b, c0, n, sbk, diag = p
                        mm(banks[sbk][:, 0:n], qk[hp:hp + 64, sk_, kb * 128:(kb + 1) * 128],
                           qk[hp:hp + 64, sq_, Q * 512 + c0:(Q + 1) * 512], True, not diag, r=["qk"], w=[BK[sbk]])
                        if diag:
                            mm(banks[sbk][:, 0:128], ident, tri_b, False, True, r=["cb"], w=[BK[sbk]])
                        for qbl in range(c0b, 4):
                            qb = 4 * Q + qbl
                            act(PT[sbk][:, qbl * 128:(qbl + 1) * 128], banks[sbk][:, qbl * 128 - c0:(qbl + 1) * 128 - c0], AF.Exp,
                                r=[BK[sbk], "FB"], w=[f"PT{sbk}"], bias=FB[:, h, kb, qb:qb + 1])

                    def stB(p):
                        kb, c0b, c0, n, sbk, diag = p
                        for qbl in range(c0b, 4):
                            mm(banks[obk][:, qbl * 65:(qbl + 1) * 65], PT[sbk][:, qbl * 128:(qbl + 1) * 128], V[:, kb, h, 0:65],
                               (kb == 0 and qbl == 0), kb == 4 * Q + qbl, r=[f"PT{sbk}", "V"], w=[BK[obk]], sgc=True)
                    stA(steps[0])
                    for j in range(nkb):
                        if j + 1 < nkb:
                            stA(steps[j + 1])
                        stB(steps[j])
                    ob = banks[obk]
                    rs = scl["sq"][:, 12:16]
                    for qbl in range(4):
                        cp("dve", rs[:, qbl:qbl + 1], ob[:, qbl * 65 + 64:qbl * 65 + 65], r=[BK[obk]], w=["fx_rs"])
                    recip(rs, rs, r=["fx_rs"], w=["fx_rs"])
                    onrm = st_["onrm"]
                    for qbl in range(4):
                        ts("dve", onrm[:, qbl, :], ob[:, qbl * 65:qbl * 65 + 64], rs[:, qbl:qbl + 1], ALU.mult,
                           r=[BK[obk], "fx_rs"], w=["onrm"])
                    head_norm_store(lambda b: onrm[:, b, :], 4, 64, lambda b: otile[oi][:, b, h * 64:(h + 1) * 64],
                                    ["onrm"], f"otile{oi}", scl)
                for qbl in range(4):
                    out_proj_block(4 * Q + qbl, otile[oi][:, qbl, :], 2, wo, f"otile{oi}", res)

        def sb_attention(l, st_, wo, res):
            qk = st_["qk"]
            V = st_["V"]
            Et = [AR.alloc([512], F32) for _ in range(2)]
            SPt = [AR.alloc([512], BF16) for _ in range(2)]
            AT = [AR.alloc([512], BF16) for _ in range(2)]
            SPsum = AR.alloc([512], BF16)
            otile = [AR.alloc([4, 256], BF16) for _ in range(2)]
            scl = {"junk64": AR.alloc([64], F32), "sq": AR.alloc([16], F32)}
            for s_ in range(4):
                S.dma("sp", qk[:, s_, :], ut_d[12 + s_, :, :], r=[f"ut{12 + s_}"], w=["qk"])
            S.dma("act", V.rearrange("p a h d -> p (a h d)"), v_d[2, :, :], r=["v2"], w=["V"])
            it = 0
            for Q in range(4):
                oi = Q % 2
                for h in range(4):
                    hp = (h % 2) * 64
                    sq_, sk_ = h // 2, 2 + h // 2
                    obk = 4 + (it % 2)
                    memset("pool", SPsum, 0.0, ["SPsum"])
                    first = True
                    for kb in range(4 * Q + 3, -1, -1):
                        c0b = max(0, kb - 4 * Q)
                        c0 = c0b * 128
                        n = 512 - c0
                        i2 = it % 2
                        it += 1
                        zbk = 0 + i2
                        cbk = 2 + i2
                        diag = kb >= 4 * Q
                        kT = qk[hp:hp + 64, sk_, kb * 128:(kb + 1) * 128]
                        nq = qk[hp:hp + 64, sq_, Q * 512 + c0:(Q + 1) * 512]
                        mm(banks[zbk][:, 0:n], kT, nq, True, not diag, r=["qk"], w=[BK[zbk]])
                        if diag:
                            mm(banks[zbk][:, 0:128], ident, strictpos_b, False, True, r=["cb"], w=[BK[zbk]])
                        act(Et[i2][:, 0:n], banks[zbk][:, 0:n], AF.Exp, r=[BK[zbk]], w=[f"Et{i2}"], scale=-1.0)
                        act(SPt[i2][:, 0:n], Et[i2][:, 0:n], AF.Ln, r=[f"Et{i2}"], w=[f"SPt{i2}"], bias=1.0)
                        mm(banks[cbk][:, 0:n], trige_b, SPt[i2][:, 0:n], True, False, r=[f"SPt{i2}", "cb"], w=[BK[cbk]])
                        if not first:
                            mm(banks[cbk][:, 0:n], ones_bf, SPsum[:, c0:512], False, False, r=["SPsum", "cb"], w=[BK[cbk]])
                        mm(banks[cbk][:, 0:n], kT, nq, False, not diag, r=["qk"], w=[BK[cbk]])
                        if diag:
                            mm(banks[cbk][:, 0:128], ident, strictpos_b, False, True, r=["cb"], w=[BK[cbk]])
                        act(AT[i2][:, 0:n], banks[cbk][:, 0:n], AF.Exp, r=[BK[cbk]], w=[f"AT{i2}"], scale=-1.0)
                        tt("dve", SPsum[:, c0:512], SPsum[:, c0:512], SPt[i2][:, 0:n], ALU.add, r=["SPsum", f"SPt{i2}"], w=["SPsum"])
                        for qbl in range(c0b, 4):
                            mm(banks[obk][:, qbl * 64:(qbl + 1) * 64], AT[i2][:, qbl * 128 - c0:(qbl + 1) * 128 - c0], V[:, kb, h, 0:64],
                               first and qbl == 3, kb == 0, r=[f"AT{i2}", "V"], w=[BK[obk]], sgc=True)
                        first = False
                    ob = banks[obk]
                    head_norm_store(lambda b: ob[:, b * 64:(b + 1) * 64], 4, 64, lambda b: otile[oi][:, b, h * 64:(h + 1) * 64],
                                    [BK[obk]], f"otile{oi}", scl)
                for qbl in range(4):
                    out_proj_block(4 * Q + qbl, otile[oi][:, qbl, :], 2, wo, f"otile{oi}", res)

        def nsa_attention(l, st_, wo, res):
            qk = st_["qk"]
            V = st_["V"]
            sig = st_["sig"]
            W1 = AR.alloc([32, 128], BF16)
            w2d = AR.alloc([128], BF16)
            peT = AR.alloc([32], BF16)
            hidS = AR.alloc([128], BF16)
            cktok = AR.alloc([128], BF16)
            ckT = AR.alloc([128], BF16)
            CVX = AR.alloc([2, 100], BF16)
            csm = AR.alloc([64], F32)
            htmp = AR.alloc([3, 128], F32)
            ctmp_ = AR.alloc([6, 8], F32)
            ET = [AR.alloc([512], BF16) for _ in range(2)]
            PT = [AR.alloc([512], BF16) for _ in range(2)]
            selT = AR.alloc([4, 128], BF16)
            selb = AR.alloc([32], BF16)
            OACC = AR.alloc([4, 64], F32)
            sc32 = AR.alloc([4, 32], F32)
            m8 = AR.alloc([16], F32)
            nsm = AR.alloc([32], F32)
            otile = [AR.alloc([512], BF16) for _ in range(2)]
            scl = {"junk64": AR.alloc([64], F32), "sq": AR.alloc([16], F32)}
            for s_ in range(8):
                S.dma("sp", qk[:, s_, :], ut_d[s_, :, :], r=[f"ut{s_}"], w=["qk"])
            S.dma("act", V.rearrange("p a h d -> p (a h d)"), v_d[0, :, :], r=["v0"], w=["V"])
            memset("pool", CVX, 0.0, ["CVX"])
            memset("pool", ckT, 0.0, ["ckT"])
            for g in range(2):
                cp("pool", CVX[:, g, 0:36], cover1, r=["cb"], w=["CVX"])

            for kind in range(2):
                w1_d = (w1k_d, w1v_d)[kind]
                w2_d = (w2k_d, w2v_d)[kind]
                pe_d = (pek_d, pev_d)[kind]
                slot = 6 + kind
                for hf in range(2):
                    S.dma("pool", W1[hf * 64:(hf + 1) * 64, :, :], w1_d[l].rearrange("(l d) h -> d l h", d=64), w=["W1"])
                    S.dma("pool", w2d[:, hf * 64:(hf + 1) * 64], w2_d[l], w=["w2d"])
                S.dma("pool", peT[0:64, :], pe_d[l], w=["peT"])
                bb = 4
                for l_ in range(32):
                    mm(banks[bb][:, 0:1], W1[0:64, l_, :], peT[0:64, l_:l_ + 1], l_ == 0, l_ == 31, r=["W1", "peT"], w=[BK[bb]])
                cp("dve", csm[:, 0:1], banks[bb][:, 0:1], r=[BK[bb]], w=["csm"])
                ts("dve", csm[:, 1:2], csm[:, 0:1], -1.0, ALU.mult, r=["csm"], w=["csm"])
                for g in range(2):
                    hb = 5
                    src = qk[g * 64:(g + 1) * 64, slot, :].rearrange("p (n s) -> p n s", s=16)
                    for l_ in range(32):
                        rhs = src[:, 0:127, l_] if l_ < 16 else src[:, 1:128, l_ - 16]
                        mm(banks[hb][:, 0:127], W1[g * 64:(g + 1) * 64, l_, :], rhs, l_ == 0, l_ == 31, r=["W1", "qk"], w=[BK[hb]])
                    act(htmp[:, 0, 0:127], banks[hb][:, 0:127], AF.Exp, r=[BK[hb], "csm"], w=["htmp"], scale=-1.0, bias=csm[:, 1:2])
                    ts("dve", htmp[:, 0, 0:127], htmp[:, 0, 0:127], 1.0, ALU.add, r=["htmp"], w=["htmp"])
                    recip(htmp[:, 0, 0:127], htmp[:, 0, 0:127], r=["htmp"], w=["htmp"])
                    stt("dve", hidS[:, 0:127], banks[hb][:, 0:127], csm[:, 0:1], htmp[:, 0, 0:127], ALU.add, ALU.mult,
                        r=[BK[hb], "csm", "htmp"], w=["hidS"])
                    ob_ = 6
                    mm(banks[ob_][0:127, 0:64], hidS[:, 0:127], w2d[:, 0:64], True, True, r=["hidS", "w2d"], w=[BK[ob_]])
                    if kind == 0:
                        srcp = banks[ob_][0:127, 0:64]
                        x1, x2 = srcp[:, 0:8], srcp[:, 8:16]
                        c_, s__ = cosc[0:127, :], sinc[0:127, :]
                        t_ = ctmp_
                        tt("dve", t_[0:127, 0, :], x1, c_, ALU.mult, r=[BK[ob_], "cf"], w=["ctmp_"])
                        tt("dve", t_[0:127, 1, :], x2, s__, ALU.mult, r=[BK[ob_], "cf"], w=["ctmp_"])
                        tt("dve", t_[0:127, 2, :], x2, c_, ALU.mult, r=[BK[ob_], "cf"], w=["ctmp_"])
                        tt("dve", t_[0:127, 3, :], x1, s__, ALU.mult, r=[BK[ob_], "cf"], w=["ctmp_"])
                        tt("dve", cktok[0:127, g * 64:g * 64 + 8], t_[0:127, 0, :], t_[0:127, 1, :], ALU.subtract, r=["ctmp_"], w=["cktok"])
                        tt("dve", cktok[0:127, g * 64 + 8:g * 64 + 16], t_[0:127, 2, :], t_[0:127, 3, :], ALU.add, r=["ctmp_"], w=["cktok"])
                        cp("dve", cktok[0:127, g * 64 + 16:g * 64 + 64], srcp[:, 16:64], r=[BK[ob_]], w=["cktok"])
                    else:
                        cp("dve", CVX[0:127, g, 36:100], banks[ob_][0:127, 0:64], r=[BK[ob_]], w=["CVX"])
                if kind == 0:
                    tbk = 7
                    bbv = banks[tbk][:].bitcast(BF16)
                    tp(bbv[:, 0:127], cktok[0:127, :], r=["cktok", "cb"], w=[BK[tbk]], idn=ident[0:127, 0:127])
                    cp("dve", ckT[:, 0:127], bbv[:, 0:127], r=[BK[tbk]], w=["ckT"])
            import os as _os
            nstop = _os.environ.get("NSASTOP", "")
            if nstop == "cmpr":
                return
            dump("ckT", ckT, [128, 128], r=["ckT"], dt=BF16)
            dump("CVX", CVX, [128, 2, 100], r=["CVX"], dt=BF16)

            it = 0
            nqb = int(_os.environ.get("NSAQB", "16"))
            qbl_ = [int(v) for v in _os.environ["NSAQBLIST"].split(",")] if _os.environ.get("NSAQBLIST") else list(range(nqb))
            for qb in qbl_:
                oi = qb % 2
                for g in range(2):
                    gp = g * 64
                    qrhs = qk[gp:gp + 64, 0:4, qb * 128:(qb + 1) * 128]
                    nv = 128
                    i2 = it % 2
                    it += 1
                    sbk = 0 + i2
                    mm(banks[sbk][0:nv, :], ckT[gp:gp + 64, 0:nv], qrhs, True, False, r=["ckT", "qk"], w=[BK[sbk]])
                    for h in range(4):
                        mm(banks[sbk][0:nv, h * 128:(h + 1) * 128], ident[0:nv, 0:nv], cmpmask[0:nv, qb, :], False, h == 3,
                           r=["cb"], w=[BK[sbk]])
                    act(ET[i2][0:nv, :], banks[sbk][0:nv, :], AF.Exp, r=[BK[sbk]], w=[f"ET{i2}"])
                    rbk = 2
                    for h in range(4):
                        mm(banks[rbk][:, h * 100:(h + 1) * 100], ET[i2][0:nv, h * 128:(h + 1) * 128], CVX[0:nv, g, :], h == 0, h == 3,
                           r=[f"ET{i2}", "CVX"], w=[BK[rbk]], sgc=True)
                    R = banks[rbk][:, 0:400].rearrange("p (h c) -> p h c", h=4)
                    rinv = nsm[:, 0:4]
                    gfac = nsm[:, 4:8]
                    ts("dve", rinv, R[:, :, 32], 1e-30, ALU.add, r=[BK[rbk]], w=["nsm"])
                    recip(rinv, rinv, r=["nsm"], w=["nsm"])
                    sg = sig[:, qb, g * 12:(g + 1) * 12].rearrange("p (h b) -> p h b", b=3)
                    tt("dve", gfac, rinv, sg[:, :, 0], ALU.mult, r=["nsm", "sig"], w=["nsm"])
                    imp = sc32[:, 0, :]
                    ts("dve", imp, R[:, 0, 0:32], rinv[:, 0:1], ALU.mult, r=[BK[rbk], "nsm"], w=["sc32"])
                    for h in range(1, 4):
                        stt("dve", imp, R[:, h, 0:32], rinv[:, h:h + 1], imp, ALU.mult, ALU.add, r=[BK[rbk], "nsm", "sc32"], w=["sc32"])
                    for h in range(4):
                        ts("dve", OACC[:, h, :], R[:, h, 36:100], gfac[:, h:h + 1], ALU.mult, r=[BK[rbk], "nsm"], w=["OACC"])
                    if nstop == "cmp" and qb == int(_os.environ.get("NSASTOPQB", "0")) and g == int(_os.environ.get("NSASTOPG", "0")):
                        return
                    score = sc32[:, 1, :]
                    sc2 = sc32[:, 2, :]
                    tt("dve", score, imp, A_t[:, qb, :], ALU.mult, r=["sc32", "cf"], w=["sc32"])
                    tt("dve", score, score, Bc_t[:, qb, :], ALU.add, r=["sc32", "cf"], w=["sc32"])
                    S.op("dve", lambda e: e.max(out=m8[:, 0:8], in_=score), r=["sc32"], w=["m8"])
                    S.op("dve", lambda e: e.match_replace(out=sc2, in_to_replace=m8[:, 0:8], in_values=score, imm_value=-1.0e9),
                         r=["sc32", "m8"], w=["sc32"])
                    S.op("dve", lambda e: e.max(out=m8[:, 8:16], in_=sc2), r=["sc32"], w=["m8"])
                    ts("dve", sc32[:, 3, :], score, m8[:, 15:16], ALU.is_ge, r=["sc32", "m8"], w=["sc32"], s2=-NEGB, op1=ALU.mult)
                    ts("dve", selb, sc32[:, 3, :], NEGB, ALU.add, r=["sc32"], w=["selb"])
                    tbk = 3
                    bbv = banks[tbk][:].bitcast(BF16)
                    tp(bbv[0:32, 0:128], selb, r=["selb", "cb"], w=[BK[tbk]])
                    cp("dve", selT[0:32, :, :], bbv[0:32, 0:128].unsqueeze(1).to_broadcast([32, 4, 128]), r=[BK[tbk]], w=["selT"])
                    if "sel" in dbg and qb == 9 and g == 1:
                        dump("selb", selb, [128, 32], r=["selb"], dt=BF16)
                        dump("imp", sc32, [128, 4, 32], r=["sc32"])
                    if nstop == "selc" and qb == int(_os.environ.get("NSASTOPQB", "0")) and g == int(_os.environ.get("NSASTOPG", "0")):
                        return
                    for br in (1, 2):
                        kslot = 4 if br == 1 else 5
                        vh = (0 if br == 1 else 2) + g
                        kb0 = 0 if br == 1 else max(0, qb - 4)
                        obk = 4 + (br - 1)
                        nsteps = []
                        for kb in range(kb0, qb + 1):
                            nsteps.append((kb, it % 2))
                            it += 1

                        def nA(p, br=br, kslot=kslot):
                            kb, i2 = p
                            sbk = 0 + i2
                            last_plain = not (br == 1 or kb == qb or (br == 2 and kb == qb - 4))
                            mm(banks[sbk][:, :], qk[gp:gp + 64, kslot, kb * 128:(kb + 1) * 128], qrhs, True, last_plain,
                               r=["qk"], w=[BK[sbk]])
                            if br == 1:
                                mm(banks[sbk][:, :], eblk[0:32, kb, :], selT[0:32, :, :], False, kb != qb, r=["cb", "selT"], w=[BK[sbk]])
                            if kb == qb:
                                for h in range(4):
                                    mm(banks[sbk][:, h * 128:(h + 1) * 128], ident, tri_b, False, h == 3, r=["cb"], w=[BK[sbk]])
                            if br == 2 and kb == qb - 4:
                                for h in range(4):
                                    mm(banks[sbk][:, h * 128:(h + 1) * 128], ident, antitri_b, False, h == 3, r=["cb"], w=[BK[sbk]])
                            act(PT[i2], banks[sbk][:, :], AF.Exp, r=[BK[sbk]], w=[f"PT{i2}"])

                        def nB(p, vh=vh, obk=obk, kb0=kb0):
                            kb, i2 = p
                            for h in range(4):
                                mm(banks[obk][:, h * 65:(h + 1) * 65], PT[i2][:, h * 128:(h + 1) * 128], V[:, kb, vh, 0:65],
                                   kb == kb0 and h == 0, kb == qb, r=[f"PT{i2}", "V"], w=[BK[obk]], sgc=True)
                        nA(nsteps[0])
                        for j in range(len(nsteps)):
                            if j + 1 < len(nsteps):
                                nA(nsteps[j + 1])
                            nB(nsteps[j])
                        O = banks[obk][:, 0:260].rearrange("p (h c) -> p h c", h=4)
                        rv = nsm[:, 8 + 8 * (br - 1):12 + 8 * (br - 1)]
                        gf = nsm[:, 12 + 8 * (br - 1):16 + 8 * (br - 1)]
                        cp("dve", rv, O[:, :, 64], r=[BK[obk]], w=["nsm"])
                        recip(rv, rv, r=["nsm"], w=["nsm"])
                        tt("dve", gf, rv, sg[:, :, br], ALU.mult, r=["nsm", "sig"], w=["nsm"])
                        for h in range(4):
                            stt("dve", OACC[:, h, :], O[:, h, 0:64], gf[:, h:h + 1], OACC[:, h, :], ALU.mult, ALU.add,
                                r=[BK[obk], "nsm", "OACC"], w=["OACC"])
                    if nstop == "br" and qb == int(_os.environ.get("NSASTOPQB", "0")) and g == int(_os.environ.get("NSASTOPG", "0")):
                        return
                    head_norm_store(lambda b: OACC[:, b, :], 4, 64, lambda b: otile[oi][:, (g * 4 + b) * 64:(g * 4 + b + 1) * 64],
                                    ["OACC"], f"otile{oi}", scl)
                if "otile" in dbg and qb == 9:
                    dump("otile", otile[oi], [128, 512], r=[f"otile{oi}"], dt=BF16)
                out_proj_block(qb, otile[oi], 4, wo, f"otile{oi}", res)

        def moe(l):
            AR.reset()
            W1e = [AR.alloc([KC, 512], BF16) for _ in range(2)]
            W3e = [AR.alloc([KC, 512], BF16) for _ in range(2)]
            W2e = [AR.alloc([4, D], BF16) for _ in range(2)]
            G = [AR.alloc([4, 512], BF16) for _ in range(2)]
            St = [AR.alloc([512], BF16) for _ in range(2)]
            wr_bf = AR.alloc([KC, 36], BF16)
            lg = AR.alloc([NB, 36], F32)
            Wg = AR.alloc([NB, 32], F32)
            elm = AR.alloc([NB, 32], F32)
            gtmp = AR.alloc([6, NB, 4], F32)
            m8 = AR.alloc([NB, 8], F32)
            ptmp = AR.alloc([8, NB], F32)
            eq = AR.alloc([NB, 32], F32)
            norm_to_hT(1)
            S.dma("pool", wr_bf, wr_d[l].rearrange("(kc p) n -> p kc n", p=128), w=["wr_bf"])
            for tb in range(NB):
                bk = tb % 2
                for kc in range(KC):
                    mm(banks[bk][:, 0:36], hT[:, kc, tb * 128:(tb + 1) * 128], wr_bf[:, kc, :], kc == 0, kc == KC - 1,
                       r=[HK[tb], "wr_bf"], w=[BK[bk]])
                tt("dve", lg[:, tb, :], banks[bk][:, 0:36], brb[:, l * 36:(l + 1) * 36], ALU.add, r=[BK[bk], "brb"], w=["lg"])
            gl = lg[:, :, 0:4]
            el = lg[:, :, 4:36]
            gmax = ptmp[:, 0, :]
            S.op("dve", lambda e: e.tensor_reduce(out=gmax, in_=gl, axis=AX.X, op=ALU.max), r=["lg"], w=["ptmp"])
            gmb = gmax.unsqueeze(2).to_broadcast([128, NB, 4])
            tt("dve", gtmp[:, 0, :, :], gl, gmb, ALU.is_ge, r=["lg", "ptmp"], w=["gtmp"])
            tt("dve", gtmp[:, 1, :, :], gl, gmb, ALU.subtract, r=["lg", "ptmp"], w=["gtmp"])
            act(gtmp[:, 1, :, :], gtmp[:, 1, :, :], AF.Exp, r=["gtmp"], w=["gtmp"])
            gs = ptmp[:, 1, :]
            S.op("dve", lambda e: e.tensor_reduce(out=gs, in_=gtmp[:, 1, :, :], axis=AX.X, op=ALU.add), r=["gtmp"], w=["ptmp"])
            pg = ptmp[:, 2, :]
            recip(pg, gs, r=["ptmp"], w=["ptmp"])
            ts("dve", gtmp[:, 2, :, :], gtmp[:, 0, :, :], 1.0e9, ALU.mult, r=["gtmp"], w=["gtmp"], s2=-1.0e9, op1=ALU.add)
            tt("dve", elm.rearrange("p a (g e) -> p a g e", g=4), el.rearrange("p a (g e) -> p a g e", g=4),
               gtmp[:, 2, :, :].unsqueeze(3).to_broadcast([128, NB, 4, 8]), ALU.add, r=["lg", "gtmp"], w=["elm"])
            for tb in range(NB):
                S.op("dve", lambda e, tb=tb: e.max(out=m8[:, tb, :], in_=elm[:, tb, :]), r=["elm"], w=["m8"])
            l1 = m8[:, :, 0]
            l2 = m8[:, :, 1]
            d21 = ptmp[:, 3, :]
            tt("dve", d21, l2, l1, ALU.subtract, r=["m8"], w=["ptmp"])
            act(d21, d21, AF.Exp, r=["ptmp"], w=["ptmp"])
            ts("dve", d21, d21, 1.0, ALU.add, r=["ptmp"], w=["ptmp"])
            p1 = ptmp[:, 4, :]
            recip(p1, d21, r=["ptmp"], w=["ptmp"])
            wA = ptmp[:, 5, :]
            wB = ptmp[:, 6, :]
            tt("dve", wA, p1, pg, ALU.mult, r=["ptmp"], w=["ptmp"])
            tt("dve", wB, pg, wA, ALU.subtract, r=["ptmp"], w=["ptmp"])
            tt("dve", eq[:], elm[:], l1.unsqueeze(2).to_broadcast([128, NB, 32]), ALU.is_equal, r=["elm", "m8"], w=["eq"])
            tt("dve", Wg[:], eq[:], wA.unsqueeze(2).to_broadcast([128, NB, 32]), ALU.mult, r=["eq", "ptmp"], w=["Wg"])
            tt("dve", eq[:], elm[:], l2.unsqueeze(2).to_broadcast([128, NB, 32]), ALU.is_equal, r=["elm", "m8"], w=["eq"])
            tt("dve", eq[:], eq[:], wB.unsqueeze(2).to_broadcast([128, NB, 32]), ALU.mult, r=["eq", "ptmp"], w=["eq"])
            tt("dve", Wg[:], Wg[:], eq[:], ALU.add, r=["eq", "Wg"], w=["Wg"])
            dump("Wg", Wg, [128, NB, 32], r=["Wg"])
            if upto == "router":
                return
            it = 0
            for e_ in range(32):
                i = e_ % 2
                S.dma("pool", W1e[i], ew1_d[l, e_].rearrange("(kc p) n -> p kc n", p=128), w=[f"W1e{i}"])
                S.dma("pool", W3e[i], ew3_d[l, e_].rearrange("(kc p) n -> p kc n", p=128), w=[f"W3e{i}"])
                S.dma("pool", W2e[i], ew2_d[l, e_].rearrange("(hc p) n -> p hc n", p=128), w=[f"W2e{i}"])
                for hc in range(4):
                    tt("pool", W2e[i][:, hc, :], W2e[i][:, hc, :], g2b[:], ALU.mult, r=[f"W2e{i}", "g2b"], w=[f"W2e{i}"])
                for t4 in range(4):
                    gi = it % 2
                    it += 1
                    for hc in range(4):
                        b1 = 0 + hc % 2
                        b3 = 2 + hc % 2
                        si = hc % 2
                        for kc in range(KC):
                            mm(banks[b1][:, :], W1e[i][:, kc, hc * 128:(hc + 1) * 128], hT[:, kc, t4 * 512:(t4 + 1) * 512],
                               kc == 0, kc == KC - 1, r=HK[4 * t4:4 * t4 + 4] + [f"W1e{i}"], w=[BK[b1]])
                        for kc in range(KC):
                            mm(banks[b3][:, :], W3e[i][:, kc, hc * 128:(hc + 1) * 128], hT[:, kc, t4 * 512:(t4 + 1) * 512],
                               kc == 0, kc == KC - 1, r=HK[4 * t4:4 * t4 + 4] + [f"W3e{i}"], w=[BK[b3]])
                        act(St[si], banks[b1][:, :], AF.Silu, r=[BK[b1]], w=[f"St{si}"])
                        tt("dve", G[gi][:, hc, :], St[si], banks[b3][:, :], ALU.mult, r=[f"St{si}", BK[b3]], w=[f"G{gi}"])
                    for tbl in range(4):
                        tb = 4 * t4 + tbl
                        for half in range(2):
                            yb = 4 + (2 * tbl + half) % 4
                            for hc in range(4):
                                mm(banks[yb][:, :], G[gi][:, hc, tbl * 128:(tbl + 1) * 128], W2e[i][:, hc, half * 512:(half + 1) * 512],
                                   hc == 0, hc == 3, r=[f"G{gi}", f"W2e{i}"], w=[BK[yb]])
                            xs = x_sb[:, tb, half * 512:(half + 1) * 512]
                            stt("dve", xs, banks[yb][:, :], Wg[:, tb, e_:e_ + 1], xs, ALU.mult, ALU.add,
                                r=[BK[yb], "Wg", XK[tb]], w=[XK[tb]])

        done = False
        for l in range(depth):
            layer_mod(l)
            if upto == "mod":
                dump("modT", modT, [128, 48], r=["modT"])
                dump("g1b", g1b, [128, D], r=["g1b"])
                dump("g2b", g2b, [128, D], r=["g2b"])
                dump("AB", AB, [128, 4, KC], r=["AB"])
                break
            S.barrier()
            AR.reset()
            fraw = AR.alloc([NB, 4], F32)
            sig = AR.alloc([NB, 24], F32)
            st_ = {"fraw": fraw, "sig": sig}
            mark = AR.off
            norm_to_hT(0)
            if upto == "norm":
                dump("hT", hT, [128, KC, S_LEN], r=HK, dt=BF16)
                break
            in_proj(l, st_)
            if upto.startswith("inproj"):
                S.barrier()
                if "ut" in dbg:
                    d = nc.dram_tensor("dbg_ut", [16, 128, S_LEN], BF16, kind="ExternalOutput").ap()
                    dbg_out["ut"] = d
                    for s_ in range(16):
                        S.dma("sp", d[s_], ut_d[s_], r=[f"ut{s_}"])
                    d2 = nc.dram_tensor("dbg_v", [3, 128, NB * 4 * VW], BF16, kind="ExternalOutput").ap()
                    dbg_out["v"] = d2
                    for s_ in range(3):
                        S.dma("sp", d2[s_], v_d[s_], r=[f"v{s_}"])
                dump("sig", sig, [128, NB, 24], r=["sig"])
                dump("fraw", fraw, [128, NB, 4], r=["fraw"])
                break
            S.barrier()
            AR.off = mark
            wo = AR.alloc([4, D], BF16)
            wstg = AR.alloc([D], F32)
            st_["qk"] = AR.alloc([8, S_LEN], BF16)
            st_["V"] = AR.alloc([NB, 4, VW], BF16)
            st_["onrm"] = AR.alloc([4, 64], F32)
            res = {"cnt": 0, "oT": [AR.alloc([4, 128], BF16) for _ in range(2)]}
            mark2 = AR.off
            load_wout(l, wo, wstg, 4, 2)
            fox_attention(l, st_, wo, res)
            if upto == "fox":
                break
            S.barrier()
            AR.off = mark2
            load_wout(l, wo, wstg, 6, 2)
            sb_attention(l, st_, wo, res)
            if upto == "sb":
                break
            S.barrier()
            AR.off = mark2
            load_wout(l, wo, wstg, 0, 4)
            nsa_attention(l, st_, wo, res)
            if upto == "nsa":
                break
            S.barrier()
            moe(l)
            if upto in ("router", "moe1"):
                break
            S.barrier()
        else:
            done = True

        if done:
            AR.reset()
            junk = AR.alloc([D], BF16)
            fgb = AR.alloc([D], F32)
            ob = [AR.alloc([D], F32) for _ in range(2)]
            S.dma("sp", fgb, fg_d, w=["fgb"])
            for tb in range(NB):
                act(junk, x_sb[:, tb, :], AF.Square, r=[XK[tb]], w=["junk", "ssq"], accum=ssq[:, tb:tb + 1])
            act(rstd[:], ssq[:], AF.Ln, r=["ssq"], w=["rstd"], scale=1.0 / D, bias=EPS)
            act(rstd[:], rstd[:], AF.Exp, r=["rstd"], w=["rstd"], scale=-0.5)
            for tb in range(NB):
                i = tb % 2
                stt("dve", ob[i], x_sb[:, tb, :], rstd[:, tb:tb + 1], fgb, ALU.mult, ALU.mult, r=[XK[tb], "rstd", "fgb"], w=[f"ob{i}"])
                S.dma("sp", out_d[tb * 128:(tb + 1) * 128, :], ob[i], r=[f"ob{i}"], w=["out"])
        else:
            S.barrier()
            for tb in range(NB):
                S.dma("sp", out_d[tb * 128:(tb + 1) * 128, :], x_sb[:, tb, :], r=[XK[tb]], w=["out"])
        S.barrier()
        S.emit_all()
    return nc, dbg_out, S


def prep_inputs(inputs):
    f = lambda a: np.ascontiguousarray(np.asarray(a, dtype=np.float32))
    L = DEPTH
    perm = _col_perm()
    cf, cb = _host_consts()
    shared = {
        "ada_w": f(inputs["ada_w"]),
        "ada_b": f(inputs["ada_b"]),
        "w_in_p": f(np.asarray(inputs["w_in"])[:, :, perm]),
        "cmp_w1_k": f(inputs["cmp_w1_k"]), "cmp_w1_v": f(inputs["cmp_w1_v"]),
        "cmp_w2_k": f(inputs["cmp_w2_k"]), "cmp_w2_v": f(inputs["cmp_w2_v"]),
        "pekT": f(np.asarray(inputs["cmp_pos_k"]).transpose(0, 2, 1)),
        "pevT": f(np.asarray(inputs["cmp_pos_v"]).transpose(0, 2, 1)),
        "w_out": f(inputs["w_out"]),
        "wr": f(np.concatenate([np.asarray(inputs["router_group_w"]), np.asarray(inputs["router_expert_w"])], axis=2)),
        "expert_w1": f(inputs["expert_w1"]), "expert_w3": f(inputs["expert_w3"]), "expert_w2": f(inputs["expert_w2"]),
        "cf": cf, "cb": cb,
    }
    g = np.stack([np.asarray(inputs["norm1_g"]), np.asarray(inputs["norm2_g"]), np.asarray(inputs["out_norm_g"])], axis=1)
    shared["gcols"] = f(g.reshape(L, 3, KC, 128).transpose(3, 0, 1, 2).reshape(128, L * 3 * KC))
    shared["bfb"] = f(np.broadcast_to(np.asarray(inputs["b_forget"]).reshape(1, L * 4), (128, L * 4)))
    br = np.concatenate([np.asarray(inputs["router_group_b"]), np.asarray(inputs["router_expert_b"])], axis=1)
    shared["brb"] = f(np.broadcast_to(br.reshape(1, L * 36), (128, L * 36)))
    shared["fgb"] = f(np.broadcast_to(np.asarray(inputs["final_g"]).reshape(1, D), (128, D)))
    xs = np.asarray(inputs["x"], dtype=np.float32)
    cs = np.asarray(inputs["c"], dtype=np.float32)
    in_maps = []
    for b in range(8):
        m = dict(shared)
        m["x"] = f(xs[b])
        m["c_fm"] = f(cs[b].reshape(KC, 128).T)
        in_maps.append(m)
    return in_maps


def kernel(**inputs):
    in_maps = prep_inputs(inputs)
    nc, _, _ = build()
    res = run_bass_kernel_spmd(nc, in_maps, core_ids=list(range(8)))
    return np.stack([np.asarray(r["out"], dtype=np.float32) for r in res.results], axis=0)
```

```python
import math
from contextlib import ExitStack
import numpy as np
import concourse.bass as bass
import concourse.mybir as mybir
from concourse.bass_utils import run_bass_kernel_spmd

F32 = mybir.dt.float32
BF16 = mybir.dt.bfloat16
AF = mybir.ActivationFunctionType
ALU = mybir.AluOpType
AX = mybir.AxisListType

S_LEN = 2048
D = 1024
NB = 16
KC = 8
DEPTH = 4
D_IN = 2844
NEGB = -30000.0
VW = 72
EPS = 1e-6

EPOCH = 30000
NSEM_ENG = 8
DMA_POOL = {"sp": 12, "act": 4, "pool": 12}
COMPUTE = ("pe", "dve", "act", "pool")
QUEUES = ("pe", "dve", "act", "pool", "sp")


class Sched:
    def __init__(self, nc, stack):
        self.nc = nc
        self.q = {e: [] for e in QUEUES}
        self.cnt = {e: 0 for e in COMPUTE}
        self.sems = {}
        for e in COMPUTE:
            self.sems[e] = [stack.enter_context(nc.semaphore(f"s_{e}{i}")) for i in range(NSEM_ENG)]
        self.dsems = {}
        self.dcnt = {}
        for qn, n in DMA_POOL.items():
            self.dsems[qn] = [stack.enter_context(nc.semaphore(f"d_{qn}{i}")) for i in range(n)]
            self.dcnt[qn] = 0
        self.seen = {e: {} for e in QUEUES}
        self.last_w = {}
        self.readers = {}
        self.all_dma_tokens = {}
        self.n_ops = 0

    def _need(self, eng, tok, same_ok):
        sem, val, prod = tok
        if prod == eng and same_ok:
            return None
        if self.seen[eng].get(sem.name, 0) >= val:
            return None
        return tok

    def _deps(self, eng, r, w, same_raw=True):
        toks = []
        for k in r:
            t = self.last_w.get(k)
            if t is not None:
                n = self._need(eng, t, same_ok=(not same_raw))
                if n:
                    toks.append(n)
            if isinstance(k, str) and k.startswith("bk"):
                for t in self.readers.get(k, {}).values():
                    n = self._need(eng, t, same_ok=True)
                    if n:
                        toks.append(n)
        for k in w:
            t = self.last_w.get(k)
            if t is not None:
                n = self._need(eng, t, same_ok=True)
                if n:
                    toks.append(n)
            for t in self.readers.get(k, {}).values():
                n = self._need(eng, t, same_ok=True)
                if n:
                    toks.append(n)
        best = {}
        for sem, val, prod in toks:
            if sem.name not in best or best[sem.name][1] < val:
                best[sem.name] = (sem, val, prod)
        return list(best.values())

    def _record(self, eng, tok, r, w):
        for k in w:
            self.last_w[k] = tok
            self.readers[k] = {}
        for k in r:
            if k in w:
                continue
            rk = eng if tok[2] in COMPUTE else tok[0].name
            self.readers.setdefault(k, {})[rk] = tok

    def op(self, eng, fn, r=(), w=(), same_raw=True):
        waits = self._deps(eng, r, w, same_raw)
        g = self.cnt[eng]
        self.cnt[eng] = g + 1
        sem = self.sems[eng][g // EPOCH]
        val = g % EPOCH + 1
        tok = (sem, val, eng)
        for (s, v, p) in waits:
            self.seen[eng][s.name] = max(self.seen[eng].get(s.name, 0), v)
        self.n_ops += 1

        def emit(e, waits=waits, fn=fn, sem=sem):
            for (s, v, p) in waits:
                e.wait_ge(s, v)
            fn(e).then_inc(sem, 1)
        self.q[eng].append(emit)
        self._record(eng, tok, r, w)
        return tok

    def dma(self, qn, out, in_, r=(), w=(), **kw):
        n = self.dcnt[qn]
        self.dcnt[qn] = n + 1
        pool = self.dsems[qn]
        sem = pool[n % len(pool)]
        val = 16 * (n // len(pool) + 1)
        waits = self._deps(qn, r, w, same_raw=True)
        if val > 16 and self.seen[qn].get(sem.name, 0) < val - 16:
            waits = [t for t in waits if t[0].name != sem.name] + [(sem, val - 16, "dma")]
        for (s, v, p) in waits:
            self.seen[qn][s.name] = max(self.seen[qn].get(s.name, 0), v)
        tok = (sem, val, "dma")
        self.n_ops += 1

        def emit(e, waits=waits, sem=sem, out=out, in_=in_, kw=kw):
            for (s, v, p) in waits:
                e.wait_ge(s, v)
            e.dma_start(out=out, in_=in_, **kw).then_inc(sem, 16)
        self.q[qn].append(emit)
        self._record(qn, tok, r, w)
        self.all_dma_tokens[sem.name] = (sem, val, "dma")
        return tok

    def barrier(self):
        toks = []
        for e in COMPUTE:
            g = self.cnt[e]
            if g > 0:
                toks.append((self.sems[e][(g - 1) // EPOCH], (g - 1) % EPOCH + 1, e))
        toks += list(self.all_dma_tokens.values())
        for e in QUEUES:
            ws = [t for t in toks if t[2] != e and self.seen[e].get(t[0].name, 0) < t[1]]
            for (s, v, p) in ws:
                self.seen[e][s.name] = v

            def emit(eo, ws=ws):
                for (s, v, p) in ws:
                    eo.wait_ge(s, v)
            self.q[e].append(emit)
        self.last_w = {}
        self.readers = {}

    def emit_all(self):
        nc = self.nc
        with nc.Block() as block:
            @block.tensor
            def _(e):
                for f in self.q["pe"]:
                    f(e)

            @block.vector
            def _(e):
                for f in self.q["dve"]:
                    f(e)

            @block.scalar
            def _(e):
                for f in self.q["act"]:
                    f(e)

            @block.gpsimd
            def _(e):
                for f in self.q["pool"]:
                    f(e)

            @block.sync
            def _(e):
                for f in self.q["sp"]:
                    f(e)


class Arena:
    def __init__(self, t, nbytes):
        self.t = t
        self.n = nbytes
        self.off = 0

    def reset(self):
        self.off = 0

    def alloc(self, free_shape, dt):
        esz = 4 if dt == F32 else 2
        n_el = int(np.prod(free_shape))
        size = n_el * esz
        off = (self.off + 63) // 64 * 64
        assert off + size <= self.n, f"arena overflow {off + size} > {self.n}"
        self.off = off + size
        v = self.t[:, off // 4:(off + size) // 4]
        if dt != F32:
            v = v.bitcast(dt)
        if len(free_shape) == 2:
            v = v.rearrange("p (a b) -> p a b", a=free_shape[0])
        elif len(free_shape) == 3:
            v = v.rearrange("p (a b c) -> p a b c", a=free_shape[0], b=free_shape[1])
        elif len(free_shape) == 4:
            v = v.rearrange("p (a b c d) -> p a b c d", a=free_shape[0], b=free_shape[1], c=free_shape[2])
        return v


def _col_perm():
    o = {}
    acc = 0
    sizes = [("qa", 512), ("kca", 128), ("vca", 128), ("ksa", 128), ("vsa", 128), ("kwa", 128), ("vwa", 128),
             ("ga", 24), ("qb", 256), ("kb", 256), ("vb", 256), ("fb", 4), ("qc", 256), ("kcc", 256), ("vcc", 256)]
    for n, s in sizes:
        o[n] = acc
        acc += s
    assert acc == D_IN
    perm = []
    for j in range(4):
        perm += list(range(o["qa"] + j * 64, o["qa"] + j * 64 + 64))
        perm += list(range(o["qa"] + (4 + j) * 64, o["qa"] + (4 + j) * 64 + 64))
    perm += list(range(o["ksa"], o["ksa"] + 128)) + list(range(o["kwa"], o["kwa"] + 128))
    perm += list(range(o["vsa"], o["vsa"] + 128)) + list(range(o["vwa"], o["vwa"] + 128))
    perm += list(range(o["kca"], o["kca"] + 128)) + list(range(o["vca"], o["vca"] + 128))
    perm += list(range(o["qb"], o["qb"] + 256)) + list(range(o["kb"], o["kb"] + 256))
    perm += list(range(o["qc"], o["qc"] + 256)) + list(range(o["kcc"], o["kcc"] + 256))
    perm += list(range(o["vb"], o["vb"] + 256)) + list(range(o["vcc"], o["vcc"] + 256))
    perm += list(range(o["fb"], o["fb"] + 4)) + list(range(o["ga"], o["ga"] + 24))
    assert len(perm) == D_IN and len(set(perm)) == D_IN
    return np.array(perm)


PIECES = [(0, 512), (512, 512), (1024, 256), (1280, 512), (1792, 512), (2304, 512), (2816, 28)]

CF = {}
_o = 0
for _n, _w in [("identf", 128), ("onesf", 128), ("triincl", 128), ("cos", 128), ("sin", 128), ("cosc", 8), ("sinc", 8),
               ("A", 512), ("Bc", 512)]:
    CF[_n] = (_o, _w)
    _o += _w
NCF = _o
CB = {}
_o = 0
for _n, _w in [("ident", 128), ("ones", 128), ("tri", 128), ("antitri", 128), ("strictpos", 128), ("trige", 128),
               ("cmpmask", 2048), ("eblk", 2048), ("cover1", 36)]:
    CB[_n] = (_o, _w)
    _o += _w
NCB = _o


def _host_consts():
    p = np.arange(128)
    cf = np.zeros((128, NCF), np.float32)
    cb = np.zeros((128, NCB), np.float32)

    def setf(n, a):
        o, w = CF[n]
        cf[:, o:o + w] = a.reshape(128, w)

    def setb(n, a):
        o, w = CB[n]
        cb[:, o:o + w] = a.reshape(128, w)
    eye = np.eye(128, dtype=np.float32)
    setf("identf", eye)
    setf("onesf", np.ones((128, 128), np.float32))
    setf("triincl", (p[:, None] <= p[None, :]).astype(np.float32))
    half = 8
    inv = np.exp(np.arange(half, dtype=np.float32) * np.float32(-2.0 * math.log(500000.0) / 16)).astype(np.float32)
    pos = (np.arange(NB)[None, :] * 128 + p[:, None]).astype(np.float32)
    ang = pos[:, :, None] * inv[None, None, :]
    setf("cos", np.cos(ang).astype(np.float32))
    setf("sin", np.sin(ang).astype(np.float32))
    posc = (16 * p + 31).astype(np.float32)
    angc = posc[:, None] * inv[None, :]
    setf("cosc", np.cos(angc).astype(np.float32))
    setf("sinc", np.sin(angc).astype(np.float32))
    q = np.arange(NB)[None, :] * 128 + p[:, None]
    cur = q // 64
    j = np.arange(32)[None, None, :]
    A = np.zeros((128, NB, 32), np.float32)
    Bc = np.zeros((128, NB, 32), np.float32)
    causal = (64 * j <= q[:, :, None])
    A[causal] = 1.0
    Bc[~causal] = -1.0
    f0 = (j == 0) & np.ones_like(causal)
    f1 = (j == cur[:, :, None])
    f2 = (j == cur[:, :, None] - 1)
    for fm, val in ((f0, 1.0e4), (f2, 1.0e4 + 64.0), (f1, 1.0e4 + 128.0)):
        A[fm] = 0.0
        Bc[fm] = val
    setf("A", A)
    setf("Bc", Bc)
    setb("ident", eye)
    setb("ones", np.ones((128, 128), np.float32))
    kk = p[:, None]
    qq = p[None, :]
    setb("tri", np.where(kk > qq, NEGB, 0.0).astype(np.float32))
    setb("antitri", np.where(kk <= qq, NEGB, 0.0).astype(np.float32))
    setb("strictpos", np.where(kk >= qq, -NEGB, 0.0).astype(np.float32))
    setb("trige", (p[:, None] >= p[None, :]).astype(np.float32))
    n = p[:, None, None]
    qabs = np.arange(NB)[None, :, None] * 128 + p[None, None, :]
    setb("cmpmask", np.where((16 * n + 31 > qabs) | (n >= 127), NEGB, 0.0).astype(np.float32))
    kb = np.arange(NB)[None, :, None]
    kp = p[None, None, :]
    jj = p[:, None, None]
    setb("eblk", ((jj == 2 * kb + kp // 64) & (jj < 32)).astype(np.float32))
    cover = np.zeros((128, 36), np.float32)
    nn = np.arange(127)[:, None]
    js = np.arange(32)[None, :]
    cover[:127, :32] = ((16 * nn < 64 * js + 64) & (16 * nn + 32 > 64 * js)).astype(np.float32)
    cover[:127, 32] = 1.0
    setb("cover1", cover)
    return cf, cb


def build(depth=DEPTH, upto="all", dbg=(), lite=False):
    nc = bass.Bass("TRN2", target_bir_lowering=False)
    dram = {}

    def din(name, shape, dt=F32):
        dram[name] = nc.dram_tensor(name, list(shape), dt, kind="ExternalInput").ap()
        return dram[name]

    NL = 1 if lite else DEPTH
    x_d = din("x", [S_LEN, D])
    c_d = din("c_fm", [128, KC])
    adaw_d = din("ada_w", [NL, D, 6 * D])
    adab_d = din("ada_b", [NL, 6 * D])
    gcol_d = din("gcols", [128, DEPTH * 3 * KC])
    win_d = din("w_in_p", [NL, D, D_IN])
    bfb_d = din("bfb", [128, DEPTH * 4])
    w1k_d = din("cmp_w1_k", [NL, 2048, 128])
    w1v_d = din("cmp_w1_v", [NL, 2048, 128])
    w2k_d = din("cmp_w2_k", [NL, 128, 64])
    w2v_d = din("cmp_w2_v", [NL, 128, 64])
    pek_d = din("pekT", [NL, 64, 32])
    pev_d = din("pevT", [NL, 64, 32])
    wout_d = din("w_out", [NL, D, D])
    wr_d = din("wr", [NL, D, 36])
    br_d = din("brb", [128, DEPTH * 36])
    ne_l, ne_e = (1, 1) if lite else (DEPTH, 32)
    ew1_d = din("expert_w1", [ne_l, ne_e, D, 512])
    ew3_d = din("expert_w3", [ne_l, ne_e, D, 512])
    ew2_d = din("expert_w2", [ne_l, ne_e, 512, D])
    fg_d = din("fgb", [128, D])
    cf_d = din("cf", [128, NCF])
    cb_d = din("cb", [128, NCB])
    out_d = nc.dram_tensor("out", [S_LEN, D], F32, kind="ExternalOutput").ap()
    ut_d = nc.dram_tensor("ut_scr", [16, 128, S_LEN], BF16, kind="Internal").ap()
    v_d = nc.dram_tensor("v_scr", [3, 128, NB * 4 * VW], BF16, kind="Internal").ap()
    dbg_out = {}

    with ExitStack() as st:
        S = Sched(nc, st)

        def sb(name, shape, dt):
            return st.enter_context(nc.sbuf_tensor(name, list(shape), dt))
        banks = [st.enter_context(nc.psum_tensor(f"bank{i}", [128, 512], F32)) for i in range(8)]
        BK = [f"bk{i}" for i in range(8)]

        x_sb = sb("x_sb", [128, NB, D], F32)
        hT = sb("hT", [128, KC, S_LEN], BF16)
        cf = sb("cf_sb", [128, NCF], F32)
        cbt = sb("cb_sb", [128, NCB], BF16)
        g1b = sb("g1b", [128, D], F32)
        g2b = sb("g2b", [128, D], F32)
        modT = sb("modT", [128, 48], F32)
        AB = sb("AB", [128, 4, KC], F32)
        gcol = sb("gcol", [128, DEPTH * 3 * KC], F32)
        bfb = sb("bfb_sb", [128, DEPTH * 4], F32)
        brb = sb("brb_sb", [128, DEPTH * 36], F32)
        cond_bf = sb("cond_bf", [128, KC], BF16)
        small = sb("small", [128, 256], F32)
        ssq = sb("ssq", [128, NB], F32)
        rstd = sb("rstd", [128, NB], F32)
        ones1 = sb("ones1", [1, 128], F32)
        one11 = sb("one11", [1, 1], F32)
        ARENA_BYTES = 84 * 1024
        arena_t = sb("arena", [128, ARENA_BYTES // 4], F32)
        AR = Arena(arena_t, ARENA_BYTES)

        def cfv(n):
            o, w = CF[n]
            return cf[:, o:o + w]

        def cbv(n):
            o, w = CB[n]
            return cbt[:, o:o + w]
        ident = cbv("ident")
        ones_bf = cbv("ones")
        tri_b = cbv("tri")
        antitri_b = cbv("antitri")
        strictpos_b = cbv("strictpos")
        trige_b = cbv("trige")
        cmpmask = cbv("cmpmask").rearrange("p (a b) -> p a b", a=NB)
        eblk = cbv("eblk").rearrange("p (a b) -> p a b", a=NB)
        cover1 = cbv("cover1")
        identf = cfv("identf")
        onesf = cfv("onesf")
        triincl = cfv("triincl")
        cos_t = cfv("cos").rearrange("p (a b) -> p a b", a=NB)
        sin_t = cfv("sin").rearrange("p (a b) -> p a b", a=NB)
        cosc = cfv("cosc")
        sinc = cfv("sinc")
        A_t = cfv("A").rearrange("p (a b) -> p a b", a=NB)
        Bc_t = cfv("Bc").rearrange("p (a b) -> p a b", a=NB)

        def mm(out, lhsT, rhs, start, stop, r, w, sgc=False):
            S.op("pe", lambda e: e.matmul(out, lhsT=lhsT, rhs=rhs, start=start, stop=stop, skip_group_check=sgc),
                 r=r, w=w, same_raw=False)

        def tp(out, in_, r, w, idn=None):
            idn_ = ident if idn is None else idn
            S.op("pe", lambda e: e.transpose(out=out, in_=in_, identity=idn_), r=r, w=w, same_raw=False)

        def act(out, in_, func, r, w, bias=None, scale=None, accum=None):
            kw = {}
            if bias is not None:
                kw["bias"] = bias
            if scale is not None:
                kw["scale"] = scale
            if accum is not None:
                kw["accum_out"] = accum
            S.op("act", lambda e: e.activation(out=out, in_=in_, func=func, **kw), r=r, w=w)

        def tt(eng, out, in0, in1, op, r, w):
            S.op(eng, lambda e: e.tensor_tensor(out=out, in0=in0, in1=in1, op=op), r=r, w=w)

        def ts(eng, out, in0, s1, op0, r, w, s2=None, op1=None):
            if op1 is None:
                S.op(eng, lambda e: e.tensor_scalar(out=out, in0=in0, scalar1=s1, scalar2=None, op0=op0), r=r, w=w)
            else:
                S.op(eng, lambda e: e.tensor_scalar(out=out, in0=in0, scalar1=s1, scalar2=s2, op0=op0, op1=op1), r=r, w=w)

        def stt(eng, out, in0, scalar, in1, op0, op1, r, w):
            S.op(eng, lambda e: e.scalar_tensor_tensor(out=out, in0=in0, scalar=scalar, in1=in1, op0=op0, op1=op1), r=r, w=w)

        def cp(eng, out, in_, r, w):
            if eng == "act":
                S.op("act", lambda e: e.copy(out=out, in_=in_), r=r, w=w)
            else:
                S.op(eng, lambda e: e.tensor_copy(out=out, in_=in_), r=r, w=w)

        def recip(out, in_, r, w):
            S.op("dve", lambda e: e.reciprocal(out=out, in_=in_), r=r, w=w)

        def memset(eng, ap, val, w):
            S.op(eng, lambda e: e.memset(ap, val), w=w)

        def dump(name, ap, shape, r, dt=F32):
            if name not in dbg:
                return
            if not isinstance(ap, bass.AP):
                ap = ap[:]
            d = nc.dram_tensor("dbg_" + name, list(shape), dt, kind="ExternalOutput").ap()
            dbg_out[name] = d
            S.dma("sp", d, ap, r=r)

        XK = [f"x{tb}" for tb in range(NB)]
        HK = [f"hT{tb}" for tb in range(NB)]

        S.dma("sp", cf[:], cf_d, w=["cf"])
        S.dma("pool", cbt[:], cb_d, w=["cb"])
        S.dma("sp", gcol[:], gcol_d, w=["gcol"])
        S.dma("sp", bfb[:], bfb_d, w=["bfb"])
        S.dma("sp", brb[:], br_d, w=["brb"])
        memset("pool", ones1[:], 1.0, ["ones1"])
        memset("pool", one11[:], 1.0, ["one11"])
        for tb in range(NB):
            S.dma("sp" if tb % 2 == 0 else "act", x_sb[:, tb, :], x_d[tb * 128:(tb + 1) * 128, :], w=[XK[tb]])
        ctmp = small[:, 0:8]
        ctmp2 = small[:, 8:16]
        S.dma("sp", ctmp, c_d, w=["ctmp"])
        act(ctmp2, ctmp, AF.Exp, r=["ctmp"], w=["ctmp2"], scale=-1.0)
        ts("dve", ctmp2, ctmp2, 1.0, ALU.add, r=["ctmp2"], w=["ctmp2"])
        recip(ctmp2, ctmp2, r=["ctmp2"], w=["ctmp2"])
        tt("dve", cond_bf[:], ctmp, ctmp2, ALU.mult, r=["ctmp", "ctmp2"], w=["cond"])

        def layer_mod(l):
            AR.reset()
            adab = [AR.alloc([KC, 512], BF16) for _ in range(2)]
            brow = [AR.alloc([512], F32) for _ in range(2)]
            rowp = [AR.alloc([512], F32) for _ in range(2)]
            for j in range(12):
                i = j % 2
                S.dma("pool", adab[i], adaw_d[l, :, j * 512:(j + 1) * 512].rearrange("(kc p) n -> p kc n", p=128),
                      w=[f"adab{i}"])
                S.dma("sp", brow[i][0:1, :], adab_d[l:l + 1, j * 512:(j + 1) * 512], w=[f"brow{i}"])
                bk = 0 + i
                for kc in range(KC):
                    mm(banks[bk][0:1, :], cond_bf[:, kc:kc + 1], adab[i][:, kc, :], kc == 0, kc == KC - 1,
                       r=["cond", f"adab{i}"], w=[BK[bk]])
                tt("dve", rowp[i][0:1, :], banks[bk][0:1, :], brow[i][0:1, :], ALU.add, r=[BK[bk], f"brow{i}"], w=[f"rowp{i}"])
                if j in (4, 5, 10, 11):
                    dst = g1b if j < 6 else g2b
                    off = (j % 2) * 512
                    mm(banks[2 + i][:, :], ones1[0:1, :], rowp[i][0:1, :], True, True, r=["ones1", f"rowp{i}"], w=[BK[2 + i]])
                    cp("act", dst[:, off:off + 512], banks[2 + i][:, :], r=[BK[2 + i]], w=["g1b" if j < 6 else "g2b"])
                for ii in range(4):
                    cidx = j * 4 + ii
                    mm(banks[4][:, cidx:cidx + 1], rowp[i][0:1, ii * 128:(ii + 1) * 128], one11[0:1, 0:1], True, True,
                       r=[f"rowp{i}", "one11"], w=[BK[4]])
            cp("dve", modT[:], banks[4][:, 0:48], r=[BK[4]], w=["modT"])
            g1c = gcol[:, (l * 3 + 0) * KC:(l * 3 + 1) * KC]
            g2c = gcol[:, (l * 3 + 1) * KC:(l * 3 + 2) * KC]
            stt("dve", AB[:, 0, :], modT[:, 8:16], 1.0, g1c, ALU.add, ALU.mult, r=["modT", "gcol"], w=["AB"])
            cp("dve", AB[:, 1, :], modT[:, 0:8], r=["modT"], w=["AB"])
            stt("dve", AB[:, 2, :], modT[:, 32:40], 1.0, g2c, ALU.add, ALU.mult, r=["modT", "gcol"], w=["AB"])
            cp("dve", AB[:, 3, :], modT[:, 24:32], r=["modT"], w=["AB"])

        def norm_to_hT(which):
            junk = AR.alloc([D], BF16)
            xn = [AR.alloc([D], BF16) for _ in range(2)]
            for tb in range(NB):
                act(junk, x_sb[:, tb, :], AF.Square, r=[XK[tb]], w=["junk", "ssq"], accum=ssq[:, tb:tb + 1])
            act(rstd[:], ssq[:], AF.Ln, r=["ssq"], w=["rstd"], scale=1.0 / D, bias=EPS)
            act(rstd[:], rstd[:], AF.Exp, r=["rstd"], w=["rstd"], scale=-0.5)
            for tb in range(NB):
                i = tb % 2
                ts("dve", xn[i], x_sb[:, tb, :], rstd[:, tb:tb + 1], ALU.mult, r=[XK[tb], "rstd"], w=[f"xn{i}"])
                bk = 6 + i
                bb = banks[bk][:].bitcast(BF16)
                for kc in range(KC):
                    tp(bb[:, kc * 128:(kc + 1) * 128], xn[i][:, kc * 128:(kc + 1) * 128], r=[f"xn{i}", "cb"], w=[BK[bk]])
                for kc in range(KC):
                    act(hT[:, kc, tb * 128:(tb + 1) * 128], bb[:, kc * 128:(kc + 1) * 128], AF.Identity,
                        r=[BK[bk], "AB"], w=[HK[tb]], scale=AB[:, 2 * which, kc:kc + 1], bias=AB[:, 2 * which + 1, kc:kc + 1])

        def in_proj(l, st_):
            wbuf = [AR.alloc([KC, 512], BF16) for _ in range(2)]
            ut32 = [AR.alloc([512], F32) for _ in range(2)]
            ub = [AR.alloc([512], BF16) for _ in range(2)]
            rtmp = [AR.alloc([4, 12, 8], F32) for _ in range(2)]
            stage = [AR.alloc([4, 512], BF16) for _ in range(2)]
            fmst = [AR.alloc([512], BF16) for _ in range(2)]
            vst = [AR.alloc([4, VW], BF16) for _ in range(4)]
            for i in range(4):
                memset("pool", vst[i], 1.0, [f"vst{i}"])
            fraw, sig = st_["fraw"], st_["sig"]
            wcnt = [0]

            def load_w(pi):
                c0, w = PIECES[pi]
                i = wcnt[0] % 2
                wcnt[0] += 1
                S.dma("pool", wbuf[i][:, :, 0:w], win_d[l, :, c0:c0 + w].rearrange("(kc p) n -> p kc n", p=128),
                      w=[f"wbuf{i}"])
                return i

            def rope(src32, dstb, nheads, tb, ri):
                sv = src32.rearrange("p (h d) -> p h d", h=nheads)
                dv = dstb.rearrange("p (h d) -> p h d", h=nheads)
                x1 = sv[:, :, 0:8]
                x2 = sv[:, :, 8:16]
                cs = cos_t[:, tb, :].unsqueeze(1).to_broadcast([128, nheads, 8])
                sn = sin_t[:, tb, :].unsqueeze(1).to_broadcast([128, nheads, 8])
                t = rtmp[ri]
                k = [f"rtmp{ri}"]
                rsk = []
                cp("act", dstb, src32, r=[f"ut32{ri}"], w=[f"ub{ri}"])
                if "0" not in rsk:
                    tt("dve", t[:, 0, 0:nheads, :], x1, cs, ALU.mult, r=[f"ut32{ri}", "cf"], w=k)
                    tt("dve", t[:, 1, 0:nheads, :], x2, sn, ALU.mult, r=[f"ut32{ri}", "cf"], w=k)
                    tt("dve", t[:, 2, 0:nheads, :], x2, cs, ALU.mult, r=[f"ut32{ri}", "cf"], w=k)
                    tt("dve", t[:, 3, 0:nheads, :], x1, sn, ALU.mult, r=[f"ut32{ri}", "cf"], w=k)
                if "1" not in rsk:
                    tt("dve", dv[:, :, 0:8], t[:, 0, 0:nheads, :], t[:, 1, 0:nheads, :], ALU.subtract, r=k, w=[f"ub{ri}"])
                    tt("dve", dv[:, :, 8:16], t[:, 2, 0:nheads, :], t[:, 3, 0:nheads, :], ALU.add, r=k, w=[f"ub{ri}"])

            def tm_piece(pi, handler):
                wi = load_w(pi)
                c0, w = PIECES[pi]
                for tb in range(NB):
                    bk = tb % 2
                    for kc in range(KC):
                        mm(banks[bk][:, 0:w], hT[:, kc, tb * 128:(tb + 1) * 128], wbuf[wi][:, kc, 0:w], kc == 0, kc == KC - 1,
                           r=[HK[tb], f"wbuf{wi}"], w=[BK[bk]])
                    handler(tb, bk)

            def fm_piece(pi, slot0, scales):
                wi = load_w(pi)
                c0, w = PIECES[pi]
                cnt = 0
                for m in range(w // 128):
                    for t4 in range(4):
                        bk = cnt % 2
                        si = cnt % 2
                        cnt += 1
                        for kc in range(KC):
                            mm(banks[bk][:, :], wbuf[wi][:, kc, m * 128:(m + 1) * 128], hT[:, kc, t4 * 512:(t4 + 1) * 512],
                               kc == 0, kc == KC - 1, r=HK[t4 * 4:t4 * 4 + 4] + [f"wbuf{wi}"], w=[BK[bk]])
                        if scales[m] == 1.0:
                            cp("act", fmst[si], banks[bk][:, :], r=[BK[bk]], w=[f"fmst{si}"])
                        else:
                            S.op("act", lambda e, si=si, bk=bk, sc=scales[m]: e.mul(out=fmst[si], in_=banks[bk][:, :], mul=sc),
                                 r=[BK[bk]], w=[f"fmst{si}"])
                        S.dma("sp", ut_d[slot0 + m, :, t4 * 512:(t4 + 1) * 512], fmst[si], r=[f"fmst{si}"], w=[f"ut{slot0 + m}"])

            def h_p1(tb, bk):
                ri = tb % 2
                S.op("act", lambda e: e.mul(out=ut32[ri], in_=banks[bk][:, :], mul=0.125), r=[BK[bk]], w=[f"ut32{ri}"])
                rope(ut32[ri], ub[ri], 8, tb, ri)
                tbk = 2 + ri
                bb = banks[tbk][:].bitcast(BF16)
                for j in range(4):
                    tp(bb[:, j * 128:(j + 1) * 128], ub[ri][:, j * 128:(j + 1) * 128], r=[f"ub{ri}", "cb"], w=[BK[tbk]])
                sgi = (tb // 4) % 2
                cp("act", stage[sgi][:, :, (tb % 4) * 128:(tb % 4 + 1) * 128],
                   bb[:, 0:512].rearrange("p (s t) -> p s t", s=4), r=[BK[tbk]], w=[f"stage{sgi}"])
                if tb % 4 == 3:
                    t4 = tb // 4
                    S.dma("sp", ut_d[0:4, :, t4 * 512:(t4 + 1) * 512].rearrange("s p t -> p s t"), stage[sgi],
                          r=[f"stage{sgi}"], w=["ut0", "ut1", "ut2", "ut3"])
            ipn = int(upto[6:]) if (upto.startswith("inproj") and len(upto) > 6) else 99
            tm_piece(0, h_p1)
            if ipn <= 1:
                return

            import os as _os
            _skip = _os.environ.get("P2SKIP", "").split(",")

            def h_p2(tb, bk):
                ri = tb % 2
                cp("act", ut32[ri][:, 0:256], banks[bk][:, 0:256], r=[BK[bk]], w=[f"ut32{ri}"])
                vi = tb % 4
                if "vcopy" not in _skip:
                    cp("dve", vst[vi][:, :, 0:64], banks[bk][:, 256:512].rearrange("p (h d) -> p h d", h=4), r=[BK[bk]], w=[f"vst{vi}"])
                if "vdma" not in _skip:
                    S.dma("sp", v_d[0, :, tb * 4 * VW:(tb + 1) * 4 * VW], vst[vi].rearrange("p h d -> p (h d)"), r=[f"vst{vi}"], w=["v0"])
                if "rope" not in _skip:
                    rope(ut32[ri][:, 0:256], ub[ri][:, 0:256], 4, tb, ri)
                else:
                    cp("dve", ub[ri][:, 0:256], ut32[ri][:, 0:256], r=[f"ut32{ri}"], w=[f"ub{ri}"])
                if "tp" in _skip:
                    return
                tbk = 2 + ri
                bb = banks[tbk][:].bitcast(BF16)
                for j in range(2):
                    tp(bb[:, j * 128:(j + 1) * 128], ub[ri][:, j * 128:(j + 1) * 128], r=[f"ub{ri}", "cb"], w=[BK[tbk]])
                sgi = (tb // 4) % 2
                cp("act", stage[sgi][:, 0:2, (tb % 4) * 128:(tb % 4 + 1) * 128],
                   bb[:, 0:256].rearrange("p (s t) -> p s t", s=2), r=[BK[tbk]], w=[f"stage{sgi}"])
                if tb % 4 == 3 and "sdma" not in _skip:
                    t4 = tb // 4
                    S.dma("sp", ut_d[4:6, :, t4 * 512:(t4 + 1) * 512].rearrange("s p t -> p s t"), stage[sgi][:, 0:2, :],
                          r=[f"stage{sgi}"], w=["ut4", "ut5"])
            tm_piece(1, h_p2)
            if ipn <= 2:
                return

            fm_piece(2, 6, [1.0, 1.0])
            if ipn <= 3:
                return
            fm_piece(3, 8, [0.125, 0.125, 1.0, 1.0])
            fm_piece(4, 12, [-0.125, -0.125, 1.0, 1.0])
            if ipn <= 5:
                return

            def h_p6(tb, bk):
                for gi in range(2):
                    vi = (2 * tb + gi) % 4
                    cp("dve" if gi == 0 else "act", vst[vi][:, :, 0:64],
                       banks[bk][:, gi * 256:(gi + 1) * 256].rearrange("p (h d) -> p h d", h=4), r=[BK[bk]], w=[f"vst{vi}"])
                    S.dma("sp", v_d[1 + gi, :, tb * 4 * VW:(tb + 1) * 4 * VW], vst[vi].rearrange("p h d -> p (h d)"),
                          r=[f"vst{vi}"], w=[f"v{1 + gi}"])
            tm_piece(5, h_p6)
            if ipn <= 6:
                return

            def h_p7(tb, bk):
                cp("dve", fraw[:, tb, :], banks[bk][:, 0:4], r=[BK[bk]], w=["fraw"])
                cp("dve", sig[:, tb, :], banks[bk][:, 4:28], r=[BK[bk]], w=["sig"])
            tm_piece(6, h_p7)
            act(sig[:], sig[:], AF.Exp, r=["sig"], w=["sig"], scale=-1.0)
            ts("dve", sig[:], sig[:], 1.0, ALU.add, r=["sig"], w=["sig"])
            recip(sig[:], sig[:], r=["sig"], w=["sig"])

        def load_wout(l, wo, stg, kc0, nkc):
            for k in range(nkc):
                kc = kc0 + k
                S.dma("act", stg, wout_d[l, kc * 128:(kc + 1) * 128, :], w=["wostg"])
                ong = gcol[:, (l * 3 + 2) * KC + kc:(l * 3 + 2) * KC + kc + 1]
                stt("dve", wo[:, k, :], stg, ong, g1b[:], ALU.mult, ALU.mult, r=["wostg", "gcol", "g1b"], w=["wo"])

        def out_proj_block(tb, otile_ap, nkc, wo, otk, res):
            i = res["cnt"] % 2
            res["cnt"] += 1
            oT = res["oT"][i]
            tbk = 5
            bb = banks[tbk][:].bitcast(BF16)
            for k in range(nkc):
                tp(bb[:, k * 128:(k + 1) * 128], otile_ap[:, k * 128:(k + 1) * 128], r=[otk, "cb"], w=[BK[tbk]])
            cp("act", oT[:, 0:nkc, :], bb[:, 0:nkc * 128].rearrange("p (k t) -> p k t", k=nkc), r=[BK[tbk]], w=[f"oT{i}"])
            for half in range(2):
                bk = 6 + half
                for k in range(nkc):
                    mm(banks[bk][:, :], oT[:, k, :], wo[:, k, half * 512:(half + 1) * 512], k == 0, k == nkc - 1,
                       r=[f"oT{i}", "wo"], w=[BK[bk]])
                tt("dve", x_sb[:, tb, half * 512:(half + 1) * 512], x_sb[:, tb, half * 512:(half + 1) * 512], banks[bk][:, :],
                   ALU.add, r=[BK[bk], XK[tb]], w=[XK[tb]])

        def head_norm_store(src_ap, nblk, width, dst_fn, rkeys, wkey, scl):
            jk = scl["junk64"]
            sq = scl["sq"]
            for b in range(nblk):
                act(jk, src_ap(b), AF.Square, r=rkeys, w=["junk64", "hn_sq"], accum=sq[:, b:b + 1])
            act(sq[:, nblk:2 * nblk], sq[:, 0:nblk], AF.Ln, r=["hn_sq"], w=["hn_sq"], scale=1.0 / 64, bias=EPS)
            act(sq[:, 2 * nblk:3 * nblk], sq[:, nblk:2 * nblk], AF.Exp, r=["hn_sq"], w=["hn_sq"], scale=-0.5)
            for b in range(nblk):
                ts("dve", dst_fn(b), src_ap(b), sq[:, 2 * nblk + b:2 * nblk + b + 1], ALU.mult, r=rkeys + ["hn_sq"], w=[wkey])

        def fox_attention(l, st_, wo, res):
            qk = st_["qk"]
            V = st_["V"]
            fraw = st_["fraw"]
            PT = [AR.alloc([512], BF16) for _ in range(2)]
            otile = [AR.alloc([4, 256], BF16) for _ in range(2)]
            cpos = AR.alloc([NB, 4], F32)
            cend = AR.alloc([4, NB], F32)
            FB = AR.alloc([4, NB, NB], F32)
            spf = AR.alloc([NB, 4], F32)
            scl = {"junk64": AR.alloc([64], F32), "sq": AR.alloc([16], F32)}
            for s_ in range(4):
                S.dma("sp", qk[:, s_, :], ut_d[8 + s_, :, :], r=[f"ut{8 + s_}"], w=["qk"])
            S.dma("act", V.rearrange("p a h d -> p (a h d)"), v_d[1, :, :], r=["v1"], w=["V"])
            bf_ = bfb[:, l * 4:(l + 1) * 4]
            tt("dve", spf[:], fraw[:], bf_.unsqueeze(1).to_broadcast([128, NB, 4]), ALU.add, r=["fraw", "bfb"], w=["spf"])
            act(spf[:], spf[:], AF.Exp, r=["spf"], w=["spf"], scale=-1.0)
            act(spf[:], spf[:], AF.Ln, r=["spf"], w=["spf"], bias=1.0)
            spf2 = spf.rearrange("p a h -> p (a h)")
            S.op("pe", lambda e: e.matmul(banks[4][:, 0:64], lhsT=triincl, rhs=spf2, start=True, stop=True),
                 r=["spf", "cf"], w=[BK[4]], same_raw=False)
            S.op("pe", lambda e: e.matmul(banks[4][:, 64:128], lhsT=onesf, rhs=spf2, start=True, stop=True),
                 r=["spf", "cf"], w=[BK[4]], same_raw=False)
            tot = banks[4][:, 64:128].rearrange("p (a h) -> p a h", a=NB)
            cp("dve", cend[:, :, 0], tot[:, 0, :], r=[BK[4]], w=["cend"])
            for tb in range(1, NB):
                tt("dve", cend[:, :, tb], cend[:, :, tb - 1], tot[:, tb, :], ALU.add, r=[BK[4], "cend"], w=["cend"])
            win_ = banks[4][:, 0:64].rearrange("p (a h) -> p a h", a=NB)
            cp("dve", cpos[:, 0, :], win_[:, 0, :], r=[BK[4]], w=["cpos"])
            for tb in range(1, NB):
                tt("dve", cpos[:, tb, :], win_[:, tb, :], cend[:, :, tb - 1], ALU.add, r=[BK[4], "cend"], w=["cpos"])
            for h in range(4):
                for kb in range(NB):
                    ts("dve", FB[:, h, kb, :], cend[:, h, :], cpos[:, kb, h:h + 1], ALU.subtract, r=["cend", "cpos"], w=["FB"],
                       s2=-1.0, op1=ALU.mult)
            dump("cpos", cpos, [128, NB, 4], r=["cpos"])
            it = 0
            for Q in range(4):
                oi = Q % 2
                for h in range(4):
                    hp = (h % 2) * 64
                    sq_, sk_ = h // 2, 2 + h // 2
                    obk = 2 + (it % 2)
                    nkb = 4 * Q + 4
                    steps = []
                    for kb in range(nkb):
                        c0b = max(0, kb - 4 * Q)
                        steps.append((kb, c0b, c0b * 128, 512 - c0b * 128, it % 2, kb >= 4 * Q))
                        it += 1

                    def stA(p):
                        kb, c0b, c0, n, sbk, diag = p
                        mm(banks[sbk][:, 0:n], qk[hp:hp + 64, sk_, kb * 128:(kb + 1) * 128],
                           qk[hp:hp + 64, sq_, Q * 512 + c0:(Q + 1) * 512], True, not diag, r=["qk"], w=[BK[sbk]])
                        if diag:
                            mm(banks[sbk][:, 0:128], ident, tri_b, False, True, r=["cb"], w=[BK[sbk]])
                        for qbl in range(c0b, 4):
                            qb = 4 * Q + qbl
                            act(PT[sbk][:, qbl * 128:(qbl + 1) * 128], banks[sbk][:, qbl * 128 - c0:(qbl + 1) * 128 - c0], AF.Exp,
                                r=[BK[sbk], "FB"], w=[f"PT{sbk}"], bias=FB[:, h, kb, qb:qb + 1])

                    def stB(p):
                        kb, c0b, c0, n, sbk, diag = p
                        for qbl in range(c0b, 4):
                            mm(banks[obk][:, qbl * 65:(qbl + 1) * 65], PT[sbk][:, qbl * 128:(qbl + 1) * 128], V[:, kb, h, 0:65],
                               (kb == 0 and qbl == 0), kb == 4 * Q + qbl, r=[f"PT{sbk}", "V"], w=[BK[obk]], sgc=True)
                    stA(steps[0])
                    for j in range(nkb):
                        if j + 1 < nkb:
                            stA(steps[j + 1])
                        stB(steps[j])
                    ob = banks[obk]
                    rs = scl["sq"][:, 12:16]
                    for qbl in range(4):
                        cp("dve", rs[:, qbl:qbl + 1], ob[:, qbl * 65 + 64:qbl * 65 + 65], r=[BK[obk]], w=["fx_rs"])
                    recip(rs, rs, r=["fx_rs"], w=["fx_rs"])
                    onrm = st_["onrm"]
                    for qbl in range(4):
                        ts("dve", onrm[:, qbl, :], ob[:, qbl * 65:qbl * 65 + 64], rs[:, qbl:qbl + 1], ALU.mult,
                           r=[BK[obk], "fx_rs"], w=["onrm"])
                    head_norm_store(lambda b: onrm[:, b, :], 4, 64, lambda b: otile[oi][:, b, h * 64:(h + 1) * 64],
                                    ["onrm"], f"otile{oi}", scl)
                for qbl in range(4):
                    out_proj_block(4 * Q + qbl, otile[oi][:, qbl, :], 2, wo, f"otile{oi}", res)

        def sb_attention(l, st_, wo, res):
            qk = st_["qk"]
            V = st_["V"]
            Et = [AR.alloc([512], F32) for _ in range(2)]
            SPt = [AR.alloc([512], BF16) for _ in range(2)]
            AT = [AR.alloc([512], BF16) for _ in range(2)]
            SPsum = AR.alloc([512], BF16)
            otile = [AR.alloc([4, 256], BF16) for _ in range(2)]
            scl = {"junk64": AR.alloc([64], F32), "sq": AR.alloc([16], F32)}
            for s_ in range(4):
                S.dma("sp", qk[:, s_, :], ut_d[12 + s_, :, :], r=[f"ut{12 + s_}"], w=["qk"])
            S.dma("act", V.rearrange("p a h d -> p (a h d)"), v_d[2, :, :], r=["v2"], w=["V"])
            it = 0
            for Q in range(4):
                oi = Q % 2
                for h in range(4):
                    hp = (h % 2) * 64
                    sq_, sk_ = h // 2, 2 + h // 2
                    obk = 4 + (it % 2)
                    memset("pool", SPsum, 0.0, ["SPsum"])
                    sst = []
                    for kb in range(4 * Q + 3, -1, -1):
                        c0b = max(0, kb - 4 * Q)
                        sst.append((kb, c0b, c0b * 128, 512 - c0b * 128, it % 2, kb >= 4 * Q, len(sst) == 0))
                        it += 1

                    def sA(p):
                        kb, c0b, c0, n, i2, diag, first = p
                        zbk = 0 + i2
                        kT = qk[hp:hp + 64, sk_, kb * 128:(kb + 1) * 128]
                        nq = qk[hp:hp + 64, sq_, Q * 512 + c0:(Q + 1) * 512]
                        mm(banks[zbk][:, 0:n], kT, nq, True, not diag, r=["qk"], w=[BK[zbk]])
                        if diag:
                            mm(banks[zbk][:, 0:128], ident, strictpos_b, False, True, r=["cb"], w=[BK[zbk]])
                        act(Et[i2][:, 0:n], banks[zbk][:, 0:n], AF.Exp, r=[BK[zbk]], w=[f"Et{i2}"], scale=-1.0)
                        act(SPt[i2][:, 0:n], Et[i2][:, 0:n], AF.Ln, r=[f"Et{i2}"], w=[f"SPt{i2}"], bias=1.0)

                    def sB(p):
                        kb, c0b, c0, n, i2, diag, first = p
                        cbk = 2 + i2
                        kT = qk[hp:hp + 64, sk_, kb * 128:(kb + 1) * 128]
                        nq = qk[hp:hp + 64, sq_, Q * 512 + c0:(Q + 1) * 512]
                        mm(banks[cbk][:, 0:n], trige_b, SPt[i2][:, 0:n], True, False, r=[f"SPt{i2}", "cb"], w=[BK[cbk]])
                        if not first:
                            mm(banks[cbk][:, 0:n], ones_bf, SPsum[:, c0:512], False, False, r=["SPsum", "cb"], w=[BK[cbk]])
                        mm(banks[cbk][:, 0:n], kT, nq, False, not diag, r=["qk"], w=[BK[cbk]])
                        if diag:
                            mm(banks[cbk][:, 0:128], ident, strictpos_b, False, True, r=["cb"], w=[BK[cbk]])
                        act(AT[i2][:, 0:n], banks[cbk][:, 0:n], AF.Exp, r=[BK[cbk]], w=[f"AT{i2}"], scale=-1.0)
                        tt("dve", SPsum[:, c0:512], SPsum[:, c0:512], SPt[i2][:, 0:n], ALU.add, r=["SPsum", f"SPt{i2}"], w=["SPsum"])

                    def sC(p):
                        kb, c0b, c0, n, i2, diag, first = p
                        for qbl in range(c0b, 4):
                            mm(banks[obk][:, qbl * 64:(qbl + 1) * 64], AT[i2][:, qbl * 128 - c0:(qbl + 1) * 128 - c0], V[:, kb, h, 0:64],
                               first and qbl == 3, kb == 0, r=[f"AT{i2}", "V"], w=[BK[obk]], sgc=True)
                    sA(sst[0])
                    for j in range(len(sst)):
                        if j + 1 < len(sst):
                            sA(sst[j + 1])
                        sB(sst[j])
                        if j >= 1:
                            sC(sst[j - 1])
                    sC(sst[-1])
                    ob = banks[obk]
                    head_norm_store(lambda b: ob[:, b * 64:(b + 1) * 64], 4, 64, lambda b: otile[oi][:, b, h * 64:(h + 1) * 64],
                                    [BK[obk]], f"otile{oi}", scl)
                for qbl in range(4):
                    out_proj_block(4 * Q + qbl, otile[oi][:, qbl, :], 2, wo, f"otile{oi}", res)

        def nsa_attention(l, st_, wo, res):
            qk = st_["qk"]
            V = st_["V"]
            sig = st_["sig"]
            W1 = AR.alloc([32, 128], BF16)
            w2d = AR.alloc([128], BF16)
            peT = AR.alloc([32], BF16)
            hidS = AR.alloc([128], BF16)
            cktok = AR.alloc([128], BF16)
            ckT = AR.alloc([128], BF16)
            CVX = AR.alloc([2, 100], BF16)
            csm = AR.alloc([64], F32)
            htmp = AR.alloc([3, 128], F32)
            ctmp_ = AR.alloc([6, 8], F32)
            ET = [AR.alloc([512], BF16) for _ in range(2)]
            PT = [AR.alloc([512], BF16) for _ in range(2)]
            selT = AR.alloc([4, 128], BF16)
            selb = AR.alloc([32], BF16)
            OACC = AR.alloc([4, 64], F32)
            sc32 = AR.alloc([4, 32], F32)
            m8 = AR.alloc([16], F32)
            nsm = AR.alloc([32], F32)
            otile = [AR.alloc([512], BF16) for _ in range(2)]
            scl = {"junk64": AR.alloc([64], F32), "sq": AR.alloc([16], F32)}
            for s_ in range(8):
                S.dma("sp", qk[:, s_, :], ut_d[s_, :, :], r=[f"ut{s_}"], w=["qk"])
            S.dma("act", V.rearrange("p a h d -> p (a h d)"), v_d[0, :, :], r=["v0"], w=["V"])
            memset("pool", CVX, 0.0, ["CVX"])
            memset("pool", ckT, 0.0, ["ckT"])
            for g in range(2):
                cp("pool", CVX[:, g, 0:36], cover1, r=["cb"], w=["CVX"])

            for kind in range(2):
                w1_d = (w1k_d, w1v_d)[kind]
                w2_d = (w2k_d, w2v_d)[kind]
                pe_d = (pek_d, pev_d)[kind]
                slot = 6 + kind
                for hf in range(2):
                    S.dma("pool", W1[hf * 64:(hf + 1) * 64, :, :], w1_d[l].rearrange("(l d) h -> d l h", d=64), w=["W1"])
                    S.dma("pool", w2d[:, hf * 64:(hf + 1) * 64], w2_d[l], w=["w2d"])
                S.dma("pool", peT[0:64, :], pe_d[l], w=["peT"])
                bb = 4
                for l_ in range(32):
                    mm(banks[bb][:, 0:1], W1[0:64, l_, :], peT[0:64, l_:l_ + 1], l_ == 0, l_ == 31, r=["W1", "peT"], w=[BK[bb]])
                cp("dve", csm[:, 0:1], banks[bb][:, 0:1], r=[BK[bb]], w=["csm"])
                ts("dve", csm[:, 1:2], csm[:, 0:1], -1.0, ALU.mult, r=["csm"], w=["csm"])
                for g in range(2):
                    hb = 5
                    src = qk[g * 64:(g + 1) * 64, slot, :].rearrange("p (n s) -> p n s", s=16)
                    for l_ in range(32):
                        rhs = src[:, 0:127, l_] if l_ < 16 else src[:, 1:128, l_ - 16]
                        mm(banks[hb][:, 0:127], W1[g * 64:(g + 1) * 64, l_, :], rhs, l_ == 0, l_ == 31, r=["W1", "qk"], w=[BK[hb]])
                    act(htmp[:, 0, 0:127], banks[hb][:, 0:127], AF.Exp, r=[BK[hb], "csm"], w=["htmp"], scale=-1.0, bias=csm[:, 1:2])
                    ts("dve", htmp[:, 0, 0:127], htmp[:, 0, 0:127], 1.0, ALU.add, r=["htmp"], w=["htmp"])
                    recip(htmp[:, 0, 0:127], htmp[:, 0, 0:127], r=["htmp"], w=["htmp"])
                    stt("dve", hidS[:, 0:127], banks[hb][:, 0:127], csm[:, 0:1], htmp[:, 0, 0:127], ALU.add, ALU.mult,
                        r=[BK[hb], "csm", "htmp"], w=["hidS"])
                    ob_ = 6
                    mm(banks[ob_][0:127, 0:64], hidS[:, 0:127], w2d[:, 0:64], True, True, r=["hidS", "w2d"], w=[BK[ob_]])
                    if kind == 0:
                        srcp = banks[ob_][0:127, 0:64]
                        x1, x2 = srcp[:, 0:8], srcp[:, 8:16]
                        c_, s__ = cosc[0:127, :], sinc[0:127, :]
                        t_ = ctmp_
                        tt("dve", t_[0:127, 0, :], x1, c_, ALU.mult, r=[BK[ob_], "cf"], w=["ctmp_"])
                        tt("dve", t_[0:127, 1, :], x2, s__, ALU.mult, r=[BK[ob_], "cf"], w=["ctmp_"])
                        tt("dve", t_[0:127, 2, :], x2, c_, ALU.mult, r=[BK[ob_], "cf"], w=["ctmp_"])
                        tt("dve", t_[0:127, 3, :], x1, s__, ALU.mult, r=[BK[ob_], "cf"], w=["ctmp_"])
                        tt("dve", cktok[0:127, g * 64:g * 64 + 8], t_[0:127, 0, :], t_[0:127, 1, :], ALU.subtract, r=["ctmp_"], w=["cktok"])
                        tt("dve", cktok[0:127, g * 64 + 8:g * 64 + 16], t_[0:127, 2, :], t_[0:127, 3, :], ALU.add, r=["ctmp_"], w=["cktok"])
                        cp("dve", cktok[0:127, g * 64 + 16:g * 64 + 64], srcp[:, 16:64], r=[BK[ob_]], w=["cktok"])
                    else:
                        cp("dve", CVX[0:127, g, 36:100], banks[ob_][0:127, 0:64], r=[BK[ob_]], w=["CVX"])
                if kind == 0:
                    tbk = 7
                    bbv = banks[tbk][:].bitcast(BF16)
                    tp(bbv[:, 0:127], cktok[0:127, :], r=["cktok", "cb"], w=[BK[tbk]], idn=ident[0:127, 0:127])
                    cp("dve", ckT[:, 0:127], bbv[:, 0:127], r=[BK[tbk]], w=["ckT"])
            import os as _os
            nstop = _os.environ.get("NSASTOP", "")
            if nstop == "cmpr":
                return
            dump("ckT", ckT, [128, 128], r=["ckT"], dt=BF16)
            dump("CVX", CVX, [128, 2, 100], r=["CVX"], dt=BF16)

            it = 0
            nqb = int(_os.environ.get("NSAQB", "16"))
            qbl_ = [int(v) for v in _os.environ["NSAQBLIST"].split(",")] if _os.environ.get("NSAQBLIST") else list(range(nqb))
            for qb in qbl_:
                oi = qb % 2
                for g in range(2):
                    gp = g * 64
                    qrhs = qk[gp:gp + 64, 0:4, qb * 128:(qb + 1) * 128]
                    nv = 128
                    i2 = it % 2
                    it += 1
                    sbk = 0 + i2
                    mm(banks[sbk][0:nv, :], ckT[gp:gp + 64, 0:nv], qrhs, True, False, r=["ckT", "qk"], w=[BK[sbk]])
                    for h in range(4):
                        mm(banks[sbk][0:nv, h * 128:(h + 1) * 128], ident[0:nv, 0:nv], cmpmask[0:nv, qb, :], False, h == 3,
                           r=["cb"], w=[BK[sbk]])
                    act(ET[i2][0:nv, :], banks[sbk][0:nv, :], AF.Exp, r=[BK[sbk]], w=[f"ET{i2}"])
                    rbk = 2
                    for h in range(4):
                        mm(banks[rbk][:, h * 100:(h + 1) * 100], ET[i2][0:nv, h * 128:(h + 1) * 128], CVX[0:nv, g, :], h == 0, h == 3,
                           r=[f"ET{i2}", "CVX"], w=[BK[rbk]], sgc=True)
                    R = banks[rbk][:, 0:400].rearrange("p (h c) -> p h c", h=4)
                    rinv = nsm[:, 0:4]
                    gfac = nsm[:, 4:8]
                    ts("dve", rinv, R[:, :, 32], 1e-30, ALU.add, r=[BK[rbk]], w=["nsm"])
                    recip(rinv, rinv, r=["nsm"], w=["nsm"])
                    sg = sig[:, qb, g * 12:(g + 1) * 12].rearrange("p (h b) -> p h b", b=3)
                    tt("dve", gfac, rinv, sg[:, :, 0], ALU.mult, r=["nsm", "sig"], w=["nsm"])
                    imp = sc32[:, 0, :]
                    ts("dve", imp, R[:, 0, 0:32], rinv[:, 0:1], ALU.mult, r=[BK[rbk], "nsm"], w=["sc32"])
                    for h in range(1, 4):
                        stt("dve", imp, R[:, h, 0:32], rinv[:, h:h + 1], imp, ALU.mult, ALU.add, r=[BK[rbk], "nsm", "sc32"], w=["sc32"])
                    for h in range(4):
                        ts("dve", OACC[:, h, :], R[:, h, 36:100], gfac[:, h:h + 1], ALU.mult, r=[BK[rbk], "nsm"], w=["OACC"])
                    if nstop == "cmp" and qb == int(_os.environ.get("NSASTOPQB", "0")) and g == int(_os.environ.get("NSASTOPG", "0")):
                        return
                    score = sc32[:, 1, :]
                    sc2 = sc32[:, 2, :]
                    tt("dve", score, imp, A_t[:, qb, :], ALU.mult, r=["sc32", "cf"], w=["sc32"])
                    tt("dve", score, score, Bc_t[:, qb, :], ALU.add, r=["sc32", "cf"], w=["sc32"])
                    S.op("dve", lambda e: e.max(out=m8[:, 0:8], in_=score), r=["sc32"], w=["m8"])
                    S.op("dve", lambda e: e.match_replace(out=sc2, in_to_replace=m8[:, 0:8], in_values=score, imm_value=-1.0e9),
                         r=["sc32", "m8"], w=["sc32"])
                    S.op("dve", lambda e: e.max(out=m8[:, 8:16], in_=sc2), r=["sc32"], w=["m8"])
                    ts("dve", sc32[:, 3, :], score, m8[:, 15:16], ALU.is_ge, r=["sc32", "m8"], w=["sc32"], s2=-NEGB, op1=ALU.mult)
                    ts("dve", selb, sc32[:, 3, :], NEGB, ALU.add, r=["sc32"], w=["selb"])
                    tbk = 3
                    bbv = banks[tbk][:].bitcast(BF16)
                    tp(bbv[0:32, 0:128], selb, r=["selb", "cb"], w=[BK[tbk]])
                    cp("dve", selT[0:32, :, :], bbv[0:32, 0:128].unsqueeze(1).to_broadcast([32, 4, 128]), r=[BK[tbk]], w=["selT"])
                    if "sel" in dbg and qb == 9 and g == 1:
                        dump("selb", selb, [128, 32], r=["selb"], dt=BF16)
                        dump("imp", sc32, [128, 4, 32], r=["sc32"])
                    if nstop == "selc" and qb == int(_os.environ.get("NSASTOPQB", "0")) and g == int(_os.environ.get("NSASTOPG", "0")):
                        return
                    for br in (1, 2):
                        kslot = 4 if br == 1 else 5
                        vh = (0 if br == 1 else 2) + g
                        kb0 = 0 if br == 1 else max(0, qb - 4)
                        obk = 4 + (br - 1)
                        nsteps = []
                        for kb in range(kb0, qb + 1):
                            nsteps.append((kb, it % 2))
                            it += 1

                        def nA(p, br=br, kslot=kslot):
                            kb, i2 = p
                            sbk = 0 + i2
                            last_plain = not (br == 1 or kb == qb or (br == 2 and kb == qb - 4))
                            mm(banks[sbk][:, :], qk[gp:gp + 64, kslot, kb * 128:(kb + 1) * 128], qrhs, True, last_plain,
                               r=["qk"], w=[BK[sbk]])
                            if br == 1:
                                mm(banks[sbk][:, :], eblk[0:32, kb, :], selT[0:32, :, :], False, kb != qb, r=["cb", "selT"], w=[BK[sbk]])
                            if kb == qb:
                                for h in range(4):
                                    mm(banks[sbk][:, h * 128:(h + 1) * 128], ident, tri_b, False, h == 3, r=["cb"], w=[BK[sbk]])
                            if br == 2 and kb == qb - 4:
                                for h in range(4):
                                    mm(banks[sbk][:, h * 128:(h + 1) * 128], ident, antitri_b, False, h == 3, r=["cb"], w=[BK[sbk]])
                            act(PT[i2], banks[sbk][:, :], AF.Exp, r=[BK[sbk]], w=[f"PT{i2}"])

                        def nB(p, vh=vh, obk=obk, kb0=kb0):
                            kb, i2 = p
                            for h in range(4):
                                mm(banks[obk][:, h * 65:(h + 1) * 65], PT[i2][:, h * 128:(h + 1) * 128], V[:, kb, vh, 0:65],
                                   kb == kb0 and h == 0, kb == qb, r=[f"PT{i2}", "V"], w=[BK[obk]], sgc=True)
                        nA(nsteps[0])
                        for j in range(len(nsteps)):
                            if j + 1 < len(nsteps):
                                nA(nsteps[j + 1])
                            nB(nsteps[j])
                        O = banks[obk][:, 0:260].rearrange("p (h c) -> p h c", h=4)
                        rv = nsm[:, 8 + 8 * (br - 1):12 + 8 * (br - 1)]
                        gf = nsm[:, 12 + 8 * (br - 1):16 + 8 * (br - 1)]
                        cp("dve", rv, O[:, :, 64], r=[BK[obk]], w=["nsm"])
                        recip(rv, rv, r=["nsm"], w=["nsm"])
                        tt("dve", gf, rv, sg[:, :, br], ALU.mult, r=["nsm", "sig"], w=["nsm"])
                        for h in range(4):
                            stt("dve", OACC[:, h, :], O[:, h, 0:64], gf[:, h:h + 1], OACC[:, h, :], ALU.mult, ALU.add,
                                r=[BK[obk], "nsm", "OACC"], w=["OACC"])
                    if nstop == "br" and qb == int(_os.environ.get("NSASTOPQB", "0")) and g == int(_os.environ.get("NSASTOPG", "0")):
                        return
                    head_norm_store(lambda b: OACC[:, b, :], 4, 64, lambda b: otile[oi][:, (g * 4 + b) * 64:(g * 4 + b + 1) * 64],
                                    ["OACC"], f"otile{oi}", scl)
                if "otile" in dbg and qb == 9:
                    dump("otile", otile[oi], [128, 512], r=[f"otile{oi}"], dt=BF16)
                out_proj_block(qb, otile[oi], 4, wo, f"otile{oi}", res)

        def moe(l):
            AR.reset()
            W1e = [AR.alloc([KC, 512], BF16) for _ in range(2)]
            W3e = [AR.alloc([KC, 512], BF16) for _ in range(2)]
            W2e = [AR.alloc([4, D], BF16) for _ in range(2)]
            G = [AR.alloc([4, 512], BF16) for _ in range(2)]
            St = [AR.alloc([512], BF16) for _ in range(2)]
            wr_bf = AR.alloc([KC, 36], BF16)
            lg = AR.alloc([NB, 36], F32)
            Wg = AR.alloc([NB, 32], F32)
            elm = AR.alloc([NB, 32], F32)
            gtmp = AR.alloc([6, NB, 4], F32)
            m8 = AR.alloc([NB, 8], F32)
            ptmp = AR.alloc([8, NB], F32)
            eq = AR.alloc([NB, 32], F32)
            norm_to_hT(1)
            S.dma("pool", wr_bf, wr_d[l].rearrange("(kc p) n -> p kc n", p=128), w=["wr_bf"])
            for tb in range(NB):
                bk = tb % 2
                for kc in range(KC):
                    mm(banks[bk][:, 0:36], hT[:, kc, tb * 128:(tb + 1) * 128], wr_bf[:, kc, :], kc == 0, kc == KC - 1,
                       r=[HK[tb], "wr_bf"], w=[BK[bk]])
                tt("dve", lg[:, tb, :], banks[bk][:, 0:36], brb[:, l * 36:(l + 1) * 36], ALU.add, r=[BK[bk], "brb"], w=["lg"])
            gl = lg[:, :, 0:4]
            el = lg[:, :, 4:36]
            gmax = ptmp[:, 0, :]
            S.op("dve", lambda e: e.tensor_reduce(out=gmax, in_=gl, axis=AX.X, op=ALU.max), r=["lg"], w=["ptmp"])
            gmb = gmax.unsqueeze(2).to_broadcast([128, NB, 4])
            tt("dve", gtmp[:, 0, :, :], gl, gmb, ALU.is_ge, r=["lg", "ptmp"], w=["gtmp"])
            tt("dve", gtmp[:, 1, :, :], gl, gmb, ALU.subtract, r=["lg", "ptmp"], w=["gtmp"])
            act(gtmp[:, 1, :, :], gtmp[:, 1, :, :], AF.Exp, r=["gtmp"], w=["gtmp"])
            gs = ptmp[:, 1, :]
            S.op("dve", lambda e: e.tensor_reduce(out=gs, in_=gtmp[:, 1, :, :], axis=AX.X, op=ALU.add), r=["gtmp"], w=["ptmp"])
            pg = ptmp[:, 2, :]
            recip(pg, gs, r=["ptmp"], w=["ptmp"])
            ts("dve", gtmp[:, 2, :, :], gtmp[:, 0, :, :], 1.0e9, ALU.mult, r=["gtmp"], w=["gtmp"], s2=-1.0e9, op1=ALU.add)
            tt("dve", elm.rearrange("p a (g e) -> p a g e", g=4), el.rearrange("p a (g e) -> p a g e", g=4),
               gtmp[:, 2, :, :].unsqueeze(3).to_broadcast([128, NB, 4, 8]), ALU.add, r=["lg", "gtmp"], w=["elm"])
            for tb in range(NB):
                S.op("dve", lambda e, tb=tb: e.max(out=m8[:, tb, :], in_=elm[:, tb, :]), r=["elm"], w=["m8"])
            l1 = m8[:, :, 0]
            l2 = m8[:, :, 1]
            d21 = ptmp[:, 3, :]
            tt("dve", d21, l2, l1, ALU.subtract, r=["m8"], w=["ptmp"])
            act(d21, d21, AF.Exp, r=["ptmp"], w=["ptmp"])
            ts("dve", d21, d21, 1.0, ALU.add, r=["ptmp"], w=["ptmp"])
            p1 = ptmp[:, 4, :]
            recip(p1, d21, r=["ptmp"], w=["ptmp"])
            wA = ptmp[:, 5, :]
            wB = ptmp[:, 6, :]
            tt("dve", wA, p1, pg, ALU.mult, r=["ptmp"], w=["ptmp"])
            tt("dve", wB, pg, wA, ALU.subtract, r=["ptmp"], w=["ptmp"])
            tt("dve", eq[:], elm[:], l1.unsqueeze(2).to_broadcast([128, NB, 32]), ALU.is_equal, r=["elm", "m8"], w=["eq"])
            tt("dve", Wg[:], eq[:], wA.unsqueeze(2).to_broadcast([128, NB, 32]), ALU.mult, r=["eq", "ptmp"], w=["Wg"])
            tt("dve", eq[:], elm[:], l2.unsqueeze(2).to_broadcast([128, NB, 32]), ALU.is_equal, r=["elm", "m8"], w=["eq"])
            tt("dve", eq[:], eq[:], wB.unsqueeze(2).to_broadcast([128, NB, 32]), ALU.mult, r=["eq", "ptmp"], w=["eq"])
            tt("dve", Wg[:], Wg[:], eq[:], ALU.add, r=["eq", "Wg"], w=["Wg"])
            dump("Wg", Wg, [128, NB, 32], r=["Wg"])
            if upto == "router":
                return
            it = 0
            for e_ in range(32):
                i = e_ % 2
                S.dma("pool", W1e[i], ew1_d[l, e_].rearrange("(kc p) n -> p kc n", p=128), w=[f"W1e{i}"])
                S.dma("pool", W3e[i], ew3_d[l, e_].rearrange("(kc p) n -> p kc n", p=128), w=[f"W3e{i}"])
                S.dma("pool", W2e[i], ew2_d[l, e_].rearrange("(hc p) n -> p hc n", p=128), w=[f"W2e{i}"])
                for hc in range(4):
                    tt("pool", W2e[i][:, hc, :], W2e[i][:, hc, :], g2b[:], ALU.mult, r=[f"W2e{i}", "g2b"], w=[f"W2e{i}"])
                for t4 in range(4):
                    gi = it % 2
                    it += 1
                    for hc in range(4):
                        b1 = 0 + hc % 2
                        b3 = 2 + hc % 2
                        si = hc % 2
                        for kc in range(KC):
                            mm(banks[b1][:, :], W1e[i][:, kc, hc * 128:(hc + 1) * 128], hT[:, kc, t4 * 512:(t4 + 1) * 512],
                               kc == 0, kc == KC - 1, r=HK[4 * t4:4 * t4 + 4] + [f"W1e{i}"], w=[BK[b1]])
                        for kc in range(KC):
                            mm(banks[b3][:, :], W3e[i][:, kc, hc * 128:(hc + 1) * 128], hT[:, kc, t4 * 512:(t4 + 1) * 512],
                               kc == 0, kc == KC - 1, r=HK[4 * t4:4 * t4 + 4] + [f"W3e{i}"], w=[BK[b3]])
                        act(St[si], banks[b1][:, :], AF.Silu, r=[BK[b1]], w=[f"St{si}"])
                        tt("dve", G[gi][:, hc, :], St[si], banks[b3][:, :], ALU.mult, r=[f"St{si}", BK[b3]], w=[f"G{gi}"])
                    for tbl in range(4):
                        tb = 4 * t4 + tbl
                        for half in range(2):
                            yb = 4 + (2 * tbl + half) % 4
                            for hc in range(4):
                                mm(banks[yb][:, :], G[gi][:, hc, tbl * 128:(tbl + 1) * 128], W2e[i][:, hc, half * 512:(half + 1) * 512],
                                   hc == 0, hc == 3, r=[f"G{gi}", f"W2e{i}"], w=[BK[yb]])
                            xs = x_sb[:, tb, half * 512:(half + 1) * 512]
                            stt("dve", xs, banks[yb][:, :], Wg[:, tb, e_:e_ + 1], xs, ALU.mult, ALU.add,
                                r=[BK[yb], "Wg", XK[tb]], w=[XK[tb]])

        done = False
        for l in range(depth):
            layer_mod(l)
            if upto == "mod":
                dump("modT", modT, [128, 48], r=["modT"])
                dump("g1b", g1b, [128, D], r=["g1b"])
                dump("g2b", g2b, [128, D], r=["g2b"])
                dump("AB", AB, [128, 4, KC], r=["AB"])
                break
            S.barrier()
            AR.reset()
            fraw = AR.alloc([NB, 4], F32)
            sig = AR.alloc([NB, 24], F32)
            st_ = {"fraw": fraw, "sig": sig}
            mark = AR.off
            norm_to_hT(0)
            if upto == "norm":
                dump("hT", hT, [128, KC, S_LEN], r=HK, dt=BF16)
                break
            in_proj(l, st_)
            if upto.startswith("inproj"):
                S.barrier()
                if "ut" in dbg:
                    d = nc.dram_tensor("dbg_ut", [16, 128, S_LEN], BF16, kind="ExternalOutput").ap()
                    dbg_out["ut"] = d
                    for s_ in range(16):
                        S.dma("sp", d[s_], ut_d[s_], r=[f"ut{s_}"])
                    d2 = nc.dram_tensor("dbg_v", [3, 128, NB * 4 * VW], BF16, kind="ExternalOutput").ap()
                    dbg_out["v"] = d2
                    for s_ in range(3):
                        S.dma("sp", d2[s_], v_d[s_], r=[f"v{s_}"])
                dump("sig", sig, [128, NB, 24], r=["sig"])
                dump("fraw", fraw, [128, NB, 4], r=["fraw"])
                break
            S.barrier()
            AR.off = mark
            wo = AR.alloc([4, D], BF16)
            wstg = AR.alloc([D], F32)
            st_["qk"] = AR.alloc([8, S_LEN], BF16)
            st_["V"] = AR.alloc([NB, 4, VW], BF16)
            st_["onrm"] = AR.alloc([4, 64], F32)
            res = {"cnt": 0, "oT": [AR.alloc([4, 128], BF16) for _ in range(2)]}
            mark2 = AR.off
            load_wout(l, wo, wstg, 4, 2)
            fox_attention(l, st_, wo, res)
            if upto == "fox":
                break
            S.barrier()
            AR.off = mark2
            load_wout(l, wo, wstg, 6, 2)
            sb_attention(l, st_, wo, res)
            if upto == "sb":
                break
            S.barrier()
            AR.off = mark2
            load_wout(l, wo, wstg, 0, 4)
            nsa_attention(l, st_, wo, res)
            if upto == "nsa":
                break
            S.barrier()
            moe(l)
            if upto in ("router", "moe1"):
                break
            S.barrier()
        else:
            done = True

        if done:
            AR.reset()
            junk = AR.alloc([D], BF16)
            fgb = AR.alloc([D], F32)
            ob = [AR.alloc([D], F32) for _ in range(2)]
            S.dma("sp", fgb, fg_d, w=["fgb"])
            for tb in range(NB):
                act(junk, x_sb[:, tb, :], AF.Square, r=[XK[tb]], w=["junk", "ssq"], accum=ssq[:, tb:tb + 1])
            act(rstd[:], ssq[:], AF.Ln, r=["ssq"], w=["rstd"], scale=1.0 / D, bias=EPS)
            act(rstd[:], rstd[:], AF.Exp, r=["rstd"], w=["rstd"], scale=-0.5)
            for tb in range(NB):
                i = tb % 2
                stt("dve", ob[i], x_sb[:, tb, :], rstd[:, tb:tb + 1], fgb, ALU.mult, ALU.mult, r=[XK[tb], "rstd", "fgb"], w=[f"ob{i}"])
                S.dma("sp", out_d[tb * 128:(tb + 1) * 128, :], ob[i], r=[f"ob{i}"], w=["out"])
        else:
            S.barrier()
            for tb in range(NB):
                S.dma("sp", out_d[tb * 128:(tb + 1) * 128, :], x_sb[:, tb, :], r=[XK[tb]], w=["out"])
        S.barrier()
        S.emit_all()
    return nc, dbg_out, S


def prep_inputs(inputs):
    f = lambda a: np.ascontiguousarray(np.asarray(a, dtype=np.float32))
    L = DEPTH
    perm = _col_perm()
    cf, cb = _host_consts()
    shared = {
        "ada_w": f(inputs["ada_w"]),
        "ada_b": f(inputs["ada_b"]),
        "w_in_p": f(np.asarray(inputs["w_in"])[:, :, perm]),
        "cmp_w1_k": f(inputs["cmp_w1_k"]), "cmp_w1_v": f(inputs["cmp_w1_v"]),
        "cmp_w2_k": f(inputs["cmp_w2_k"]), "cmp_w2_v": f(inputs["cmp_w2_v"]),
        "pekT": f(np.asarray(inputs["cmp_pos_k"]).transpose(0, 2, 1)),
        "pevT": f(np.asarray(inputs["cmp_pos_v"]).transpose(0, 2, 1)),
        "w_out": f(inputs["w_out"]),
        "wr": f(np.concatenate([np.asarray(inputs["router_group_w"]), np.asarray(inputs["router_expert_w"])], axis=2)),
        "expert_w1": f(inputs["expert_w1"]), "expert_w3": f(inputs["expert_w3"]), "expert_w2": f(inputs["expert_w2"]),
        "cf": cf, "cb": cb,
    }
    g = np.stack([np.asarray(inputs["norm1_g"]), np.asarray(inputs["norm2_g"]), np.asarray(inputs["out_norm_g"])], axis=1)
    shared["gcols"] = f(g.reshape(L, 3, KC, 128).transpose(3, 0, 1, 2).reshape(128, L * 3 * KC))
    shared["bfb"] = f(np.broadcast_to(np.asarray(inputs["b_forget"]).reshape(1, L * 4), (128, L * 4)))
    br = np.concatenate([np.asarray(inputs["router_group_b"]), np.asarray(inputs["router_expert_b"])], axis=1)
    shared["brb"] = f(np.broadcast_to(br.reshape(1, L * 36), (128, L * 36)))
    shared["fgb"] = f(np.broadcast_to(np.asarray(inputs["final_g"]).reshape(1, D), (128, D)))
    xs = np.asarray(inputs["x"], dtype=np.float32)
    cs = np.asarray(inputs["c"], dtype=np.float32)
    in_maps = []
    for b in range(8):
        m = dict(shared)
        m["x"] = f(xs[b])
        m["c_fm"] = f(cs[b].reshape(KC, 128).T)
        in_maps.append(m)
    return in_maps


def kernel(**inputs):
    in_maps = prep_inputs(inputs)
    nc, _, _ = build()
    res = run_bass_kernel_spmd(nc, in_maps, core_ids=list(range(8)))
    return np.stack([np.asarray(r["out"], dtype=np.float32) for r in res.results], axis=0)
```
